# Optimizing a Trainium2 kernel written in Bass

```python
import math
import jax, jax.numpy as jnp
from jax import lax
import numpy as np

D_MODEL = 2048
BATCH = 4
SEQ = 4096
DEPTH = 1

NORM_EPS = 1e-6
SSD_D_INNER = D_MODEL
SSD_HEAD_DIM = 64
SSD_N_HEADS = SSD_D_INNER // SSD_HEAD_DIM
SSD_N_GROUPS = 8
SSD_D_STATE = 128
SSD_CONV_WIDTH = 4
SSD_CHUNK = 128
SSD_CONV_DIM = SSD_D_INNER + 2 * SSD_N_GROUPS * SSD_D_STATE
ATTN_HEAD_DIM = 128
DILATION_PATTERNS = ((128, 1), (512, 4), (2048, 16))
ATTN_HEADS_PER_GROUP = 4
ATTN_N_HEADS = ATTN_HEADS_PER_GROUP * len(DILATION_PATTERNS)
ATTN_WIDTH = ATTN_N_HEADS * ATTN_HEAD_DIM
ATTN_OUT_WIDTH = ATTN_HEADS_PER_GROUP * ATTN_HEAD_DIM
ATTN_BLOCK = 128
N_BRANCHES = 2
IN_COLS = SSD_D_INNER + SSD_CONV_DIM + SSD_N_HEADS + 3 * ATTN_WIDTH + N_BRANCHES * D_MODEL
N_EXPERT_GROUPS = 4
EXPERTS_PER_GROUP = 8
N_EXPERTS = N_EXPERT_GROUPS * EXPERTS_PER_GROUP
EXPERT_TOP_K = 2
EXPERT_D_FF = 1024

kernel_name = "hybrid_ssd_dilated_attn_hmoe_block"


def rms_norm(x, w):
    xf = x.astype(jnp.float32)
    y = xf * lax.rsqrt(jnp.mean(xf * xf, axis=-1, keepdims=True) + NORM_EPS)
    return (y * w.astype(jnp.float32)).astype(x.dtype)


def alibi_slopes(n):
    return jnp.asarray(2.0 ** (-8.0 * np.arange(1, n + 1) / n), dtype=jnp.float32)


def causal_depthwise_conv(u, w, bias):
    width, ch = w.shape
    out = lax.conv_general_dilated(
        u, w[:, None, :].astype(u.dtype), window_strides=(1,), padding=[(width - 1, 0)],
        dimension_numbers=("NWC", "WIO", "NWC"), feature_group_count=ch)
    return out + bias.astype(u.dtype)


def ssd_chunked_scan(xs, dt, a, bm, cm):
    b, s, h, p = xs.shape
    g, n = bm.shape[2], bm.shape[3]
    r = h // g
    l = SSD_CHUNK
    c = s // l
    x_dt = (xs * dt[..., None]).reshape(b, c, l, g, r, p)
    a_cum = jnp.cumsum((dt * a[None, None, :]).reshape(b, c, l, g, r), axis=2)
    bm = bm.reshape(b, c, l, g, n)
    cm = cm.reshape(b, c, l, g, n)
    pos = jnp.arange(l)
    causal = (pos[:, None] >= pos[None, :])[None, None, :, :, None, None]
    seg = a_cum[:, :, :, None] - a_cum[:, :, None, :]
    decay = jnp.exp(jnp.where(causal, seg, -jnp.inf))
    cb = jnp.einsum("bclgn,bcsgn->bclsg", cm, bm)
    y_diag = jnp.einsum("bclsgr,bcsgrp->bclgrp", cb[..., None] * decay, x_dt)
    decay_to_end = jnp.exp(a_cum[:, :, -1:] - a_cum)
    states = jnp.einsum("bclgn,bclgr,bclgrp->bcgrpn", bm, decay_to_end, x_dt)
    chunk_decay = jnp.exp(a_cum[:, :, -1])

    def step(carry, inp):
        st, dec = inp
        return carry * dec[..., None, None] + st, carry

    init = jnp.zeros((b, g, r, p, n), jnp.float32)
    _, states_in = lax.scan(step, init, (jnp.moveaxis(states, 1, 0), jnp.moveaxis(chunk_decay, 1, 0)))
    states_in = jnp.moveaxis(states_in, 0, 1)
    y_off = jnp.einsum("bclgn,bcgrpn,bclgr->bclgrp", cm, states_in, jnp.exp(a_cum))
    return (y_diag + y_off).reshape(b, s, h, p)


def dilated_window_attention(q, k, v, slopes, window, dilation):
    b, s, h, e = q.shape
    hops = window // dilation
    span = dilation * ATTN_BLOCK
    s_pad = -(-s // span) * span
    nb = s_pad // span
    pad = ((0, 0), (0, s_pad - s), (0, 0), (0, 0))

    def to_blocks(t):
        return jnp.pad(t, pad).reshape(b, nb, ATTN_BLOCK, dilation, h, e)

    qb, kb, vb = to_blocks(q), to_blocks(k), to_blocks(v)
    shift = ((0, 0), (1, 0), (0, 0), (0, 0), (0, 0), (0, 0))
    kk = jnp.concatenate([jnp.pad(kb, shift)[:, :-1], kb], axis=2)
    vv = jnp.concatenate([jnp.pad(vb, shift)[:, :-1], vb], axis=2)
    scores = jnp.einsum("bnqrhe,bnkrhe->brhnqk", qb, kk, preferred_element_type=jnp.float32)
    rel = (jnp.arange(ATTN_BLOCK)[:, None] + ATTN_BLOCK) - jnp.arange(2 * ATTN_BLOCK)[None, :]
    key_ok = (jnp.arange(nb)[:, None] > 0) | (jnp.arange(2 * ATTN_BLOCK)[None, :] >= ATTN_BLOCK)
    valid = ((rel >= 0) & (rel <= hops))[None] & key_ok[:, None, :]
    bias = -slopes.astype(jnp.float32)[:, None, None] * (rel * dilation).astype(jnp.float32)[None]
    logits = scores * (ATTN_HEAD_DIM ** -0.5) + bias[:, None]
    logits = jnp.where(valid, logits, -jnp.inf)
    lse = jax.nn.logsumexp(logits, axis=-1)
    probs = jnp.exp(logits - lse[..., None])
    out = jnp.einsum("brhnqk,bnkrhe->bnqrhe", probs, vv.astype(jnp.float32))
    out = out.reshape(b, s_pad, h, e)[:, :s]
    lse = jnp.transpose(lse, (0, 3, 4, 1, 2)).reshape(b, s_pad, h)[:, :s]
    return out, lse


def hierarchical_moe(hn, w_gr, b_gr, w_er, b_er, w_g, w_u, w_d):
    b, s, d = hn.shape
    t = hn.reshape(b * s, d)
    n_tok = t.shape[0]
    group_logits = jnp.dot(t, w_gr, preferred_element_type=jnp.float32) + b_gr.astype(jnp.float32)
    group_prob = jax.nn.softmax(group_logits, axis=-1)
    group_idx = jnp.argmax(group_logits, axis=-1)
    group_gate = jnp.take_along_axis(group_prob, group_idx[:, None], axis=-1)
    expert_logits = (jnp.dot(t, w_er, preferred_element_type=jnp.float32)
                     + b_er.astype(jnp.float32)).reshape(n_tok, N_EXPERT_GROUPS, EXPERTS_PER_GROUP)
    in_group = jnp.take_along_axis(expert_logits, group_idx[:, None, None], axis=1)[:, 0]
    top_vals, top_idx = lax.top_k(in_group, EXPERT_TOP_K)
    gate = jax.nn.softmax(top_vals, axis=-1) * group_gate
    expert_id = (group_idx[:, None] * EXPERTS_PER_GROUP + top_idx).reshape(-1)
    order = jnp.argsort(expert_id)
    token_of = order // EXPERT_TOP_K
    xs = t[token_of]
    sizes = jnp.bincount(expert_id, length=N_EXPERTS).astype(jnp.int32)
    hmid = jax.nn.silu(lax.ragged_dot(xs, w_g, sizes)) * lax.ragged_dot(xs, w_u, sizes)
    ys = lax.ragged_dot(hmid, w_d, sizes) * gate.reshape(-1)[order][:, None].astype(t.dtype)
    out = jnp.zeros_like(t).at[token_of].add(ys)
    return out.reshape(b, s, d)


def setup_inputs(seed: int = 0) -> dict:
    key = jax.random.key(seed)
    ks = jax.random.split(key, 24)
    f32 = jnp.float32
    L = DEPTH

    def nrm(k, shape, scale):
        return jax.random.normal(k, shape, f32) * scale

    dt0 = jnp.exp(jax.random.uniform(ks[6], (L, SSD_N_HEADS), f32, math.log(1e-3), math.log(1e-1)))
    return {
        "x": nrm(ks[0], (BATCH, SEQ, D_MODEL), 1.0),
        "attn_norm_w": 1.0 + nrm(ks[1], (L, D_MODEL), 0.02),
        "w_in": nrm(ks[2], (L, D_MODEL, IN_COLS), D_MODEL ** -0.5),
        "b_gate": nrm(ks[3], (L, N_BRANCHES * D_MODEL), 0.02),
        "conv_w": nrm(ks[4], (L, SSD_CONV_WIDTH, SSD_CONV_DIM), SSD_CONV_WIDTH ** -0.5),
        "conv_b": nrm(ks[5], (L, SSD_CONV_DIM), 0.02),
        "dt_bias": dt0 + jnp.log(-jnp.expm1(-dt0)),
        "a_log": jnp.log(jax.random.uniform(ks[7], (L, SSD_N_HEADS), f32, 1.0, 16.0)),
        "d_skip": 1.0 + nrm(ks[8], (L, SSD_N_HEADS), 0.1),
        "ssd_norm_w": 1.0 + nrm(ks[9], (L, SSD_D_INNER), 0.02),
        "w_ssd_out": nrm(ks[10], (L, SSD_D_INNER, D_MODEL), SSD_D_INNER ** -0.5),
        "w_attn_out": nrm(ks[11], (L, ATTN_OUT_WIDTH, D_MODEL), ATTN_OUT_WIDTH ** -0.5),
        "w_out": nrm(ks[12], (L, D_MODEL, D_MODEL), D_MODEL ** -0.5),
        "ffn_norm_w": 1.0 + nrm(ks[13], (L, D_MODEL), 0.02),
        "w_group_router": nrm(ks[14], (L, D_MODEL, N_EXPERT_GROUPS), D_MODEL ** -0.5),
        "b_group_router": nrm(ks[15], (L, N_EXPERT_GROUPS), 0.01),
        "w_expert_router": nrm(ks[16], (L, D_MODEL, N_EXPERTS), D_MODEL ** -0.5),
        "b_expert_router": nrm(ks[17], (L, N_EXPERTS), 0.01),
        "w_exp_gate": nrm(ks[18], (L, N_EXPERTS, D_MODEL, EXPERT_D_FF), D_MODEL ** -0.5),
        "w_exp_up": nrm(ks[19], (L, N_EXPERTS, D_MODEL, EXPERT_D_FF), D_MODEL ** -0.5),
        "w_exp_down": nrm(ks[20], (L, N_EXPERTS, EXPERT_D_FF, D_MODEL), EXPERT_D_FF ** -0.5),
        "final_norm_w": 1.0 + nrm(ks[21], (D_MODEL,), 0.02),
    }


def reference(x, attn_norm_w, w_in, b_gate, conv_w, conv_b, dt_bias, a_log, d_skip, ssd_norm_w,
              w_ssd_out, w_attn_out, w_out, ffn_norm_w, w_group_router, b_group_router,
              w_expert_router, b_expert_router, w_exp_gate, w_exp_up, w_exp_down, final_norm_w):
    b, s, _ = x.shape
    f32 = jnp.float32
    slopes = alibi_slopes(ATTN_N_HEADS)
    cuts = [int(c) for c in np.cumsum([SSD_D_INNER, SSD_CONV_DIM, SSD_N_HEADS,
                                       ATTN_WIDTH, ATTN_WIDTH, ATTN_WIDTH])]
    bc_cuts = [SSD_D_INNER, SSD_D_INNER + SSD_N_GROUPS * SSD_D_STATE]
    for layer in range(DEPTH):
        h = rms_norm(x, attn_norm_w[layer])
        proj = jnp.einsum("bsd,dc->bsc", h, w_in[layer])
        z, xbc, dt_raw, q, k, v, gate_raw = jnp.split(proj, cuts, axis=-1)

        xbc = jax.nn.silu(causal_depthwise_conv(xbc, conv_w[layer], conv_b[layer]))
        xs, bm, cm = jnp.split(xbc, bc_cuts, axis=-1)
        xs = xs.astype(f32).reshape(b, s, SSD_N_HEADS, SSD_HEAD_DIM)
        bm = bm.astype(f32).reshape(b, s, SSD_N_GROUPS, SSD_D_STATE)
        cm = cm.astype(f32).reshape(b, s, SSD_N_GROUPS, SSD_D_STATE)
        dt = jax.nn.softplus(dt_raw.astype(f32) + dt_bias[layer].astype(f32))
        a = -jnp.exp(a_log[layer].astype(f32))
        y = ssd_chunked_scan(xs, dt, a, bm, cm)
        y = y + d_skip[layer].astype(f32)[:, None] * xs
        y = y.reshape(b, s, SSD_D_INNER) * jax.nn.silu(z.astype(f32))
        y = rms_norm(y, ssd_norm_w[layer]).astype(x.dtype)
        y_ssd = y @ w_ssd_out[layer]

        q = q.reshape(b, s, ATTN_N_HEADS, ATTN_HEAD_DIM)
        k = k.reshape(b, s, ATTN_N_HEADS, ATTN_HEAD_DIM)
        v = v.reshape(b, s, ATTN_N_HEADS, ATTN_HEAD_DIM)
        outs, lses = [], []
        for gi, (window, dilation) in enumerate(DILATION_PATTERNS):
            hs = slice(gi * ATTN_HEADS_PER_GROUP, (gi + 1) * ATTN_HEADS_PER_GROUP)
            o_g, lse_g = dilated_window_attention(q[:, :, hs], k[:, :, hs], v[:, :, hs],
                                                  slopes[hs], window, dilation)
            outs.append(o_g)
            lses.append(lse_g)
        weights = jax.nn.softmax(jnp.stack(lses), axis=0)
        o = jnp.sum(weights[..., None] * jnp.stack(outs), axis=0)
        o = o.reshape(b, s, ATTN_OUT_WIDTH).astype(x.dtype)
        y_attn = o @ w_attn_out[layer]

        gates = jax.nn.sigmoid(gate_raw.astype(f32) + b_gate[layer].astype(f32))
        gates = gates.reshape(b, s, N_BRANCHES, D_MODEL)
        merged = (gates[:, :, 0] * y_ssd.astype(f32) + gates[:, :, 1] * y_attn.astype(f32)).astype(x.dtype)
        x = x + merged @ w_out[layer]

        x = x + hierarchical_moe(rms_norm(x, ffn_norm_w[layer]), w_group_router[layer],
                                 b_group_router[layer], w_expert_router[layer], b_expert_router[layer],
                                 w_exp_gate[layer], w_exp_up[layer], w_exp_down[layer])
    return rms_norm(x, final_norm_w)
```

```python
import math
from contextlib import ExitStack

import numpy as np
import concourse.bass as bass
import concourse.mybir as mybir
from concourse.bass_utils import run_bass_kernel_spmd

F32 = mybir.dt.float32
BF16 = mybir.dt.bfloat16
AF = mybir.ActivationFunctionType
ALU = mybir.AluOpType
AX = mybir.AxisListType

D = 2048
NTOK = 2048
NPREV = 2048
NDC = D // 128
DI = 2048
NH = 32
HP = 64
NG = 8
DS = 128
CONV_DIM = DI + 2 * NG * DS
AH = 12
AE = 128
IN_COLS = 14880
C_Z, C_XBC, C_DT, C_Q, C_K, C_V, C_G = 0, 2048, 6144, 6176, 7712, 9248, 10784
DIL = (1, 4, 16)
NE = 32
DFF = 1024
EPS = 1e-6
NEG = -30000.0
NSLOT = 64
I32 = mybir.dt.int32


class Tok:
    __slots__ = ("sem", "val")

    def __init__(self, sem, val):
        self.sem = sem
        self.val = val


class Buf:
    def __init__(self, name=""):
        self.name = name
        self.w = []
        self.r = {}


class Tile(Buf):
    def __init__(self, name, t):
        super().__init__(name)
        self.t = t

    def __getitem__(self, k):
        return self.t[k]


def _flat(bs):
    out = []
    for b in bs:
        if b is None:
            continue
        if isinstance(b, (list, tuple)):
            out.extend(_flat(b))
        else:
            out.append(b)
    return out


class Prog:
    ENG = ("pe", "act", "dve", "pool", "sp")

    def __init__(self, nc, es):
        self.nc = nc
        self.es = es
        self.eng = {"pe": nc.tensor, "act": nc.scalar, "dve": nc.vector, "pool": nc.gpsimd, "sp": nc.sync}
        self.sem = {e: es.enter_context(nc.semaphore("s_" + e)) for e in self.ENG}
        self.cnt = {e: 0 for e in self.ENG}
        self.waited = {e: {} for e in self.ENG}
        nslots = {"sp": 36, "pool": 32, "act": 2, "bg": 16}
        self.slots = {q: [[es.enter_context(nc.semaphore(f"d_{q}{i}")), 0] for i in range(n)]
                      for q, n in nslots.items()}
        self.rr = {q: 0 for q in nslots}
        self.n_inst = 0

    def _wait(self, e, tok):
        if tok is None:
            return
        key = tok.sem.name
        if e == "pe" and tok.sem is self.sem["pe"]:
            return
        if self.waited[e].get(key, 0) >= tok.val:
            return
        self.eng[e].wait_ge(tok.sem, tok.val)
        self.waited[e][key] = tok.val
        self.n_inst += 1

    def _deps(self, e, reads, writes):
        for b in reads:
            for t in b.w:
                self._wait(e, t)
        for b in writes:
            for t in b.w:
                self._wait(e, t)
            for t in b.r.values():
                self._wait(e, t)

    def _commit(self, e, tok, reads, writes):
        for b in writes:
            b.w = [tok]
            b.r = {}
        for b in reads:
            if b not in writes:
                b.r[e] = tok

    def op(self, e, fn, reads=(), writes=(), mark=True):
        reads = _flat(reads)
        writes = _flat(writes)
        ex = [b for b in reads if getattr(b, "excl", False)]
        if ex:
            reads = [b for b in reads if b not in ex]
            writes = writes + [b for b in ex if b not in writes]
        self._deps(e, reads, writes)
        ins = fn()
        self.n_inst += 1
        if mark:
            self.cnt[e] += 1
            ins.then_inc(self.sem[e], 1)
            tok = Tok(self.sem[e], self.cnt[e])
        else:
            assert e == "pe"
            tok = Tok(self.sem[e], self.cnt[e] + 1)
        self._commit(e, tok, reads, writes)
        return tok

    def dma(self, q, out, in_, reads=(), writes=(), bg=False):
        reads = _flat(reads)
        writes = _flat(writes)
        sk = "bg" if bg else q
        slot = self.slots[sk][self.rr[sk]]
        self.rr[sk] = (self.rr[sk] + 1) % len(self.slots[sk])
        if slot[1] > 0:
            self._wait(q, Tok(slot[0], slot[1]))
        self._deps(q, reads, writes)
        ins = self.eng[q].dma_start(out=out, in_=in_)
        self.n_inst += 1
        slot[1] += 16
        ins.then_inc(slot[0], 16)
        tok = Tok(slot[0], slot[1])
        self._commit(q, tok, reads, writes)
        return tok

    def idma(self, out, out_off, in_, in_off, reads=(), writes=()):
        q = "pool"
        reads = _flat(reads)
        writes = _flat(writes)
        slot = self.slots[q][self.rr[q]]
        self.rr[q] = (self.rr[q] + 1) % len(self.slots[q])
        if slot[1] > 0:
            self._wait(q, Tok(slot[0], slot[1]))
        self._deps(q, reads, writes)
        ins = self.nc.gpsimd.indirect_dma_start(out, out_off, in_, in_off)
        self.n_inst += 1
        slot[1] += 16
        ins.then_inc(slot[0], 16)
        tok = Tok(slot[0], slot[1])
        self._commit(q, tok, reads, writes)
        return tok

    def barrier(self):
        toks = [Tok(self.sem[e], self.cnt[e]) for e in self.ENG if self.cnt[e] > 0]
        for q, sl in self.slots.items():
            if q == "bg":
                continue
            for s, c in sl:
                if c > 0:
                    toks.append(Tok(s, c))
        for e in self.ENG:
            for t in toks:
                if t.sem is self.sem[e]:
                    continue
                self._wait(e, t)

    def tile(self, es, name, shape, dtype):
        self.n_tiles = getattr(self, "n_tiles", 0) + 1
        t = es.enter_context(self.nc.sbuf_tensor(f"sb{self.n_tiles}_{name}", list(shape), dtype))
        return Tile(name, t)


class Rot:
    def __init__(self, tiles):
        self.tiles = tiles
        self.i = 0

    def next(self):
        t = self.tiles[self.i]
        self.i = (self.i + 1) % len(self.tiles)
        return t


def tile_w(w2d, cw):
    K, C = w2d.shape
    assert C % cw == 0 and K % 128 == 0
    kc = K // 128
    a = w2d.reshape(kc, 128, C // cw, cw).transpose(2, 1, 0, 3)
    return np.ascontiguousarray(a).reshape(C // cw, 128, kc * cw)


class Ctx:
    pass


def declare_io(nc, debug=False, phases="abcdeE"):
    c = Ctx()
    di = lambda name, shape, dt=F32: nc.dram_tensor(name, list(shape), dt, kind="ExternalInput").ap()
    c.x_own = di("x_own", [NTOK, D])
    c.x_prev = di("x_prev", [NPREV, D])
    c.w_fm = di("w_fm", [88, 128, NDC * 128])
    c.w_tm = di("w_tm", [7, 128, NDC * 512])
    c.w_dt = di("w_dt", [128, NDC * 32])
    c.attn_norm_w = di("attn_norm_w", [1, D])
    c.b_gate = di("b_gate", [128, 32])
    c.ident = di("ident", [128, 128])
    c.cstB = di("cstB", [128, 1024])
    c.conv_wb = di("conv_wb", [128, 32 * 5])
    c.vec32 = di("vec32", [3, NH])
    c.ssd_norm_w = di("ssd_norm_w", [1, DI])
    c.flag = di("flag", [128, 2])
    c.attn_bias = di("attn_bias", [3, 128, 1024])
    c.w_ssd_fm = di("w_ssd_fm", [16, 128, NDC * 128])
    c.w_attn_fm = di("w_attn_fm", [16, 128, 4 * 128])
    c.w_out_tm = di("w_out_tm", [4, 128, NDC * 512])
    c.ffn_norm_w = di("ffn_norm_w", [1, D])
    c.w_router = di("w_router", [128, NDC * 36])
    c.b_router = di("b_router", [1, 36])
    if "E" in phases:
        c.wg_r = di("wg_r", [4, NE * 128, 4096])
        c.wu_r = di("wu_r", [4, NE * 128, 4096])
        c.wd_r = di("wd_r", [4, NE * 128, 4096])
        c.WB_s = [nc.dram_tensor(f"WB_s{fc}", [NE * 128, 3 * 4096], BF16).ap() for fc in range(4)]
        c.precast = [(fc, k, src, r0) for fc in range(4) for r0 in range(0, NE * 128, 512) for (k, src) in ((0, c.wg_r), (1, c.wu_r), (2, c.wd_r))]
        c.precast_toks = []
    c.cstE = di("cstE", [128, 128 + 128 + 64 + 8])
    c.final_norm_w = di("final_norm_w", [1, D])
    c.out = nc.dram_tensor("out", [NTOK, D], F32, kind="ExternalOutput").ap()
    kind = "ExternalOutput" if debug else "Internal"
    ds = lambda name, shape, dt=F32: nc.dram_tensor(name, list(shape), dt, kind=kind).ap()
    c.XBC_s = ds("XBC_s", [CONV_DIM, 3 + NPREV + NTOK])
    c.Z_s = ds("Z_s", [NTOK, DI])
    c.DT_s = ds("DT_s", [NPREV + NTOK, NH])
    c.QT_s = ds("QT_s", [AH, 128, NTOK], BF16)
    c.KT_s = ds("KT_s", [AH, 128, NPREV + NTOK], BF16)
    c.V_s = ds("V_s", [3, 32, 128, 512], BF16)
    c.G_s = ds("G_s", [2 * D, NTOK])
    c.YN_s = ds("YN_s", [NDC, 128, NTOK], BF16)
    c.O_s = ds("O_s", [3, NTOK, 4 * 130])
    c.OT_s = ds("OT_s", [4, 128, NTOK], BF16)
    c.M_s = ds("M_s", [NDC, 128, NTOK], BF16)
    c.X2_s = ds("X2_s", [NTOK, D])
    c.HN_s = ds("HN_s", [NDC, 128, NTOK], BF16)
    c.GATE_s = ds("GATE_s", [NTOK, NE])
    c.HNTM_s = ds("HNTM_s", [NTOK, D], BF16)
    c.XS_s = ds("XS_s", [NSLOT * 128, D], BF16)
    c.YP_s = ds("YP_s", [NSLOT * 128, D])
    return c


def precast_step(p, c, n=1):
    for _ in range(n):
        if not getattr(c, "precast", None):
            return
        fc, k, src, r0 = c.precast.pop(0)
        dst = c.WB_s[fc][r0:r0 + 512, k * 4096:(k + 1) * 4096].rearrange("r (a b) -> r a b", a=2)
        c.precast_toks.append(p.dma("pool", dst, src[fc, r0:r0 + 512, :].rearrange("r (a b) -> r a b", a=2), bg=True))


def cls_view(ap2d, d):
    if d == 1:
        return ap2d.unsqueeze(1)
    return ap2d.rearrange("p (j r) -> p r j", r=d)


def phase_a(p, c, ps):
    nc = p.nc
    with ExitStack() as es:
        hT = p.tile(es, "hT", [128, NDC, 2048], BF16)
        nw = p.tile(es, "nw_bc", [128, D], F32)
        bg = p.tile(es, "bg", [128, 32], F32)
        idb = p.tile(es, "idb", [128, 128], BF16)
        zero = p.tile(es, "zero", [128, 4], F32)
        xts = Rot([p.tile(es, f"xt{i}", [128, D], F32) for i in range(2)])
        hbs = Rot([p.tile(es, f"hb{i}", [128, D], BF16) for i in range(2)])
        junk = p.tile(es, "junk", [128, D], BF16)
        sts = Rot([p.tile(es, f"st{i}", [128, 4], F32) for i in range(2)])
        wfm = Rot([p.tile(es, f"wfm{i}", [128, NDC, 128], BF16) for i in range(3)])
        wtm = Rot([p.tile(es, f"wtm{i}", [128, NDC, 512], BF16) for i in range(2)])
        wdt = p.tile(es, "wdt", [128, NDC, 32], BF16)
        so32 = Rot([p.tile(es, f"so32_{i}", [128, 512], F32) for i in range(4)])
        so16 = Rot([p.tile(es, f"so16_{i}", [128, 512], BF16) for i in range(4)])
        psr = Rot(ps)
        ev = [0]

        p.dma("sp", nw[:], c.attn_norm_w.partition_broadcast(128), writes=[nw])
        p.dma("sp", bg[:], c.b_gate, writes=[bg])
        p.dma("pool", idb[:], c.ident, writes=[idb])
        p.dma("pool", wdt[:].rearrange("p a b -> p (a b)"), c.w_dt, writes=[wdt])
        p.op("dve", lambda: nc.vector.memset(zero[:], 0.0), writes=[zero])
        for r0 in range(0, CONV_DIM, 128):
            p.dma("sp", c.XBC_s[r0:r0 + 128, 0:3], zero[:, 0:3], reads=[zero])

        def evac_copy(dst_ap, src_ap, reads, writes, scale=None):
            e = "act" if ev[0] % 2 == 0 else "dve"
            ev[0] += 1
            if e == "act":
                if scale is None:
                    return p.op("act", lambda: nc.scalar.copy(dst_ap, src_ap), reads=reads, writes=writes)
                return p.op("act", lambda: nc.scalar.activation(out=dst_ap, in_=src_ap, func=AF.Copy, scale=scale),
                            reads=reads, writes=writes)
            if scale is None:
                return p.op("dve", lambda: nc.vector.tensor_copy(dst_ap, src_ap), reads=reads, writes=writes)
            return p.op("dve", lambda: nc.vector.tensor_scalar(dst_ap, src_ap, scale, None, ALU.mult),
                        reads=reads, writes=writes)

        def build_hT(xsrc):
            for tt in range(16):
                xt = xts.next(); hb = hbs.next(); st = sts.next()
                p.dma("sp", xt[:], xsrc[tt * 128:(tt + 1) * 128, :], writes=[xt])
                p.op("act", lambda: nc.scalar.activation(out=junk[:], in_=xt[:], func=AF.Square, accum_out=st[:, 0:1]),
                     reads=[xt], writes=[junk, st])
                p.op("dve", lambda: nc.vector.tensor_scalar(st[:, 1:2], st[:, 0:1], 1.0 / D, EPS, ALU.mult, ALU.add),
                     reads=[st], writes=[st])
                p.op("act", lambda: nc.scalar.activation(out=st[:, 2:3], in_=st[:, 1:2], func=AF.Ln), reads=[st], writes=[st])
                p.op("act", lambda: nc.scalar.activation(out=st[:, 3:4], in_=st[:, 2:3], func=AF.Exp, scale=-0.5),
                     reads=[st], writes=[st])
                p.op("dve", lambda: nc.vector.scalar_tensor_tensor(hb[:], xt[:], st[:, 3:4], nw[:], ALU.mult, ALU.mult),
                     reads=[xt, st, nw], writes=[hb])
                for half in range(2):
                    bank = psr.next()
                    pb = bank[:].bitcast(BF16)
                    for k in range(8):
                        dc = half * 8 + k
                        p.op("pe", lambda: nc.tensor.transpose(pb[:, k * 128:(k + 1) * 128], hb[:, dc * 128:(dc + 1) * 128], idb[:]),
                             reads=[hb, idb], writes=[bank], mark=(k == 7))
                    evac_copy(hT[:, half * 8:half * 8 + 8, tt * 128:(tt + 1) * 128],
                              pb.rearrange("p (a b) -> p a b", a=8), [bank], [hT])

        def mm_group(bank, out_ap, lhs_fn, rhs_fn, reads):
            for dc in range(NDC):
                p.op("pe", lambda: nc.tensor.matmul(out_ap, lhs_fn(dc), rhs_fn(dc), start=(dc == 0), stop=(dc == NDC - 1)),
                     reads=reads, writes=[bank], mark=(dc == NDC - 1))

        def fm_chunk(cc, jobs):
            wb = wfm.next()
            p.dma("pool", wb[:].rearrange("p a b -> p (a b)"), c.w_fm[cc], writes=[wb])
            for rhs_fn, n, epi in jobs:
                bank = psr.next()
                mm_group(bank, bank[:, 0:n], lambda dc: wb[:, dc, :], rhs_fn, [wb, hT])
                epi(bank, n)

        def nat_rhs(st):
            return lambda dc: hT[:, dc, st * 512:(st + 1) * 512]

        def epi_store32(dst_ap):
            def f(bank, n):
                so = so32.next()
                evac_copy(so[:, 0:n], bank[:, 0:n], [bank], [so])
                p.dma("sp", dst_ap, so[:, 0:n], reads=[so])
            return f

        def epi_store16(dst_ap, scale=None, n3=None):
            def f(bank, n):
                so = so16.next()
                evac_copy(so[:, 0:n], bank[:, 0:n], [bank], [so], scale=scale)
                src = so[:, 0:n]
                if n3 is not None:
                    src = src.rearrange("p (a b) -> p a b", a=n3)
                p.dma("sp", dst_ap, src, reads=[so])
            return f

        def epi_gate(cc, dst_ap):
            def f(bank, n):
                so = so32.next()
                p.op("act", lambda: nc.scalar.activation(out=so[:, 0:n], in_=bank[:, 0:n], func=AF.Sigmoid, bias=bg[:, cc:cc + 1]),
                     reads=[bank, bg], writes=[so])
                p.dma("sp", dst_ap, so[:, 0:n], reads=[so])
            return f

        def kt_view(h, d):
            return c.KT_s[h].rearrange("e (r n j) -> e r n j", r=d, j=128)

        def tm_tile(wt_ap_fn, ncols, lhs_list, dst_list, dtype32=True):
            for lhs_fn, dst in zip(lhs_list, dst_list):
                bank = psr.next()
                mm_group(bank, bank[:, 0:ncols], lhs_fn, wt_ap_fn, [hT] + tm_reads[0])
                if dtype32:
                    so = so32.next()
                else:
                    so = so16.next()
                evac_copy(so[:, 0:ncols], bank[:, 0:ncols], [bank], [so])
                p.dma("sp", dst, so[:, 0:ncols], reads=[so])

        tm_reads = [[]]

        def v_blocks(g, prev):
            d = DIL[g]
            span = 128 * d
            nprevb = NPREV // span
            ncls = 4096 // span
            lhs, dst = [], []
            if prev:
                blocks = [(r, nprevb - 1) for r in range(d)]
            else:
                blocks = [(r, n) for r in range(d) for n in range(NTOK // span)]
            for r, n in blocks:
                def lf(dc, r=r, n=n):
                    return cls_view(hT[:, dc, :], d)[:, r, n * 128:(n + 1) * 128]
                lhs.append(lf)
                ng = n if prev else nprevb + n
                dst.append(c.V_s[g, r * ncls + ng])
            return lhs, dst

        build_hT(c.x_prev)
        for cc in range(24):
            fm_chunk(cc, [(nat_rhs(st), 512, epi_store32(c.XBC_s[cc * 128:(cc + 1) * 128, 3 + st * 512: 3 + (st + 1) * 512]))
                          for st in range(4)])
        for cc in range(24, 32):
            fm_chunk(cc, [(nat_rhs(3), 512, epi_store32(c.XBC_s[cc * 128:(cc + 1) * 128, 3 + 3 * 512: 3 + 4 * 512]))])
        for h in range(AH):
            g = h // 4
            d = DIL[g]
            cc = 44 + h
            if g == 0:
                jobs = [(lambda dc: hT[:, dc, 1920:2048], 128, epi_store16(c.KT_s[h][:, 15 * 128:16 * 128]))]
            elif g == 1:
                jobs = [(lambda dc: cls_view(hT[:, dc, :], 4)[:, :, 384:512], 512,
                         epi_store16(kt_view(h, 4)[:, :, 3, :], n3=4))]
            else:
                jobs = [((lambda dc, st=st: cls_view(hT[:, dc, :], 16)[:, 4 * st:4 * st + 4, :]), 512,
                         epi_store16(kt_view(h, 16)[:, 4 * st:4 * st + 4, 0, :], n3=4)) for st in range(4)]
            fm_chunk(cc, jobs)
        tm_reads[0] = [wdt]
        tm_tile(lambda dc: wdt[:, dc, :], 32,
                [(lambda dc, tt=tt: hT[:, dc, tt * 128:(tt + 1) * 128]) for tt in range(16)],
                [c.DT_s[tt * 128:(tt + 1) * 128, :] for tt in range(16)])
        for g in range(3):
            wt = wtm.next()
            p.dma("pool", wt[:].rearrange("p a b -> p (a b)"), c.w_tm[4 + g], writes=[wt])
            tm_reads[0] = [wt]
            lhs, dst = v_blocks(g, True)
            tm_tile(lambda dc: wt[:, dc, :], 512, lhs, dst, dtype32=False)

        build_hT(c.x_own)
        for cc in range(32):
            fm_chunk(cc, [(nat_rhs(st), 512,
                           epi_store32(c.XBC_s[cc * 128:(cc + 1) * 128, 3 + NPREV + st * 512: 3 + NPREV + (st + 1) * 512]))
                          for st in range(4)])
        inv = 1.0 / math.sqrt(AE)
        for h in range(AH):
            g = h // 4
            d = DIL[g]
            if g == 0:
                jobs = [(nat_rhs(st), 512, epi_store16(c.QT_s[h][:, st * 512:(st + 1) * 512], scale=inv)) for st in range(4)]
            elif g == 1:
                jobs = [((lambda dc, r=r: cls_view(hT[:, dc, :], 4)[:, r, :]), 512,
                         epi_store16(c.QT_s[h][:, r * 512:(r + 1) * 512], scale=inv)) for r in range(4)]
            else:
                jobs = [((lambda dc, st=st: cls_view(hT[:, dc, :], 16)[:, 4 * st:4 * st + 4, :]), 512,
                         epi_store16(c.QT_s[h][:, st * 512:(st + 1) * 512], scale=inv)) for st in range(4)]
            fm_chunk(32 + h, jobs)
            if g == 0:
                jobs = [(nat_rhs(st), 512, epi_store16(c.KT_s[h][:, (16 + 4 * st) * 128:(20 + 4 * st) * 128])) for st in range(4)]
            elif g == 1:
                jobs = [((lambda dc, r=r: cls_view(hT[:, dc, :], 4)[:, r, :]), 512,
                         epi_store16(c.KT_s[h][:, r * 1024 + 512:(r + 1) * 1024])) for r in range(4)]
            else:
                jobs = [((lambda dc, st=st: cls_view(hT[:, dc, :], 16)[:, 4 * st:4 * st + 4, :]), 512,
                         epi_store16(kt_view(h, 16)[:, 4 * st:4 * st + 4, 1, :], n3=4)) for st in range(4)]
            fm_chunk(44 + h, jobs)
        for cc in range(32):
            fm_chunk(56 + cc, [(nat_rhs(st), 512, epi_gate(cc, c.G_s[cc * 128:(cc + 1) * 128, st * 512:(st + 1) * 512]))
                               for st in range(4)])
        for ct in range(4):
            wt = wtm.next()
            p.dma("pool", wt[:].rearrange("p a b -> p (a b)"), c.w_tm[ct], writes=[wt])
            tm_reads[0] = [wt]
            tm_tile(lambda dc: wt[:, dc, :], 512,
                    [(lambda dc, tt=tt: hT[:, dc, tt * 128:(tt + 1) * 128]) for tt in range(16)],
                    [c.Z_s[tt * 128:(tt + 1) * 128, ct * 512:(ct + 1) * 512] for tt in range(16)])
        tm_reads[0] = [wdt]
        tm_tile(lambda dc: wdt[:, dc, :], 32,
                [(lambda dc, tt=tt: hT[:, dc, tt * 128:(tt + 1) * 128]) for tt in range(16)],
                [c.DT_s[NPREV + tt * 128:NPREV + (tt + 1) * 128, :] for tt in range(16)])
        for g in range(3):
            wt = wtm.next()
            p.dma("pool", wt[:].rearrange("p a b -> p (a b)"), c.w_tm[4 + g], writes=[wt])
            tm_reads[0] = [wt]
            lhs, dst = v_blocks(g, False)
            tm_tile(lambda dc: wt[:, dc, :], 512, lhs, dst, dtype32=False)
        p.barrier()


def host_common(inp):
    w_in = np.asarray(inp["w_in"])[0]
    fm_cols = np.concatenate([w_in[:, C_XBC:C_DT], w_in[:, C_Q:C_K], w_in[:, C_K:C_V], w_in[:, C_G:IN_COLS]], axis=1)
    tm_cols = np.concatenate([w_in[:, C_Z:C_XBC], w_in[:, C_V:C_G]], axis=1)
    m = {}
    m["w_fm"] = tile_w(fm_cols, 128)
    m["w_tm"] = tile_w(tm_cols, 512)
    m["w_dt"] = tile_w(w_in[:, C_DT:C_Q], 32)[0]
    m["attn_norm_w"] = np.ascontiguousarray(np.asarray(inp["attn_norm_w"]).reshape(1, D))
    m["b_gate"] = np.ascontiguousarray(np.asarray(inp["b_gate"]).reshape(32, 128).T)
    m["ident"] = np.eye(128, dtype=np.float32)
    return m


def phase_b(p, c, ps):
    nc = p.nc
    with ExitStack() as es:
        T = lambda name, shape, dt=F32: p.tile(es, name, shape, dt)
        cst = T("cstB", [128, 1024])
        TRI, SU, ONES, IDF, NEGM = (cst[:, 0:128], cst[:, 128:256], cst[:, 256:384], cst[:, 384:512], cst[:, 512:1024])
        idb = T("idbB", [128, 128], BF16)
        cwb = T("cwb", [128, 32, 5])
        v32 = T("v32", [128, 3, NH])
        A_bc = T("A_bc", [128, NH])
        nws = T("nws", [128, DI])
        flag = T("flagB", [128, 2])
        us = Rot([T(f"u{i}", [128, 515]) for i in range(3)])
        accs = Rot([T(f"acc{i}", [128, 512]) for i in range(2)])
        xcs = Rot([T(f"xc{i}", [128, 512]) for i in range(2)])
        xs_tm = T("xs_tm", [128, 4, DI])
        B_tm = T("B_tm", [128, 4, NG * DS], BF16)
        BT = T("BT", [128, NG, 512], BF16)
        CT = T("CT", [128, NG, 512], BF16)
        dtr = Rot([T(f"dtr{i}", [128, NH]) for i in range(2)])
        sm = Rot([T(f"sm{i}", [128, 8, NH]) for i in range(2)])
        Ex = Rot([T(f"Ex{i}", [128, 3, NH]) for i in range(2)])
        L_all = T("L_all", [128, NH, 128])
        x_dt = T("x_dt", [128, DI], BF16)
        xw = T("xw", [128, DI], BF16)
        decs = Rot([T(f"dec{i}", [128, 512]) for i in range(2)])
        MTs = Rot([T(f"MT{i}", [128, 512], BF16) for i in range(2)])
        y = T("y", [128, DI])
        tmp = T("tmpB", [128, DI])
        zts = Rot([T(f"zt{i}", [128, DI]) for i in range(2)])
        yn = T("yn", [128, DI], BF16)
        junk = T("junkB", [128, DI], BF16)
        st4 = Rot([T(f"st4_{i}", [128, 4]) for i in range(2)])
        S = T("S", [128, DI])
        S_bf = T("S_bf", [128, DI], BF16)
        ynT = Rot([T(f"ynT{i}", [128, NDC, 128], BF16) for i in range(2)])
        psr = Rot(ps)

        p.dma("sp", cst[:], c.cstB, writes=[cst])
        p.dma("pool", idb[:], c.ident, writes=[idb])
        p.dma("sp", cwb[:].rearrange("p a b -> p (a b)"), c.conv_wb, writes=[cwb])
        p.dma("sp", v32[:].rearrange("p a b -> p (a b)"), c.vec32.rearrange("a b -> (a b)").partition_broadcast(128), writes=[v32])
        p.dma("sp", nws[:], c.ssd_norm_w.partition_broadcast(128), writes=[nws])
        p.dma("sp", flag[:], c.flag, writes=[flag])
        p.op("act", lambda: nc.scalar.activation(out=A_bc[:], in_=v32[:, 1, :], func=AF.Exp), reads=[v32], writes=[A_bc])
        p.op("dve", lambda: nc.vector.tensor_scalar(A_bc[:], A_bc[:], -1.0, None, ALU.mult), reads=[A_bc], writes=[A_bc])
        p.op("dve", lambda: nc.vector.memset(S[:], 0.0), writes=[S])
        p.op("dve", lambda: nc.vector.memset(S_bf[:], 0.0), writes=[S_bf])
        DTB = v32[:, 0, :]
        D_bc = v32[:, 2, :]
        bc = lambda ap, n: ap.unsqueeze(2).broadcast_to([128, ap.shape[1], n])

        def conv_supertile(t0, own):
            ncc = 32 if own else 24
            for cc in range(ncc):
                u = us.next(); acc = accs.next(); xc = xcs.next()
                p.dma("sp", u[:], c.XBC_s[cc * 128:(cc + 1) * 128, t0:t0 + 515], writes=[u])
                p.op("dve", lambda: nc.vector.tensor_scalar(acc[:], u[:, 0:512], cwb[:, cc, 0:1], cwb[:, cc, 4:5], ALU.mult, ALU.add),
                     reads=[u, cwb], writes=[acc])
                for w in range(1, 4):
                    p.op("dve", lambda: nc.vector.scalar_tensor_tensor(acc[:], u[:, w:w + 512], cwb[:, cc, w:w + 1], acc[:], ALU.mult, ALU.add),
                         reads=[u, cwb, acc], writes=[acc])
                if cc < 16:
                    p.op("act", lambda: nc.scalar.activation(out=xc[:], in_=acc[:], func=AF.Silu), reads=[acc], writes=[xc])
                    bank = psr.next()
                    for k in range(4):
                        p.op("pe", lambda: nc.tensor.transpose(bank[:, k * 128:(k + 1) * 128], xc[:, k * 128:(k + 1) * 128], IDF),
                             reads=[xc, cst], writes=[bank], mark=(k == 3))
                    p.op("dve", lambda: nc.vector.tensor_copy(xs_tm[:, :, cc * 128:(cc + 1) * 128], bank[:].rearrange("p (a b) -> p a b", a=4)),
                         reads=[bank], writes=[xs_tm])
                elif cc < 24:
                    g = cc - 16
                    p.op("act", lambda: nc.scalar.activation(out=BT[:, g, :], in_=acc[:], func=AF.Silu), reads=[acc], writes=[BT])
                    bank = psr.next()
                    pb = bank[:].bitcast(BF16)
                    for k in range(4):
                        p.op("pe", lambda: nc.tensor.transpose(pb[:, k * 128:(k + 1) * 128], BT[:, g, k * 128:(k + 1) * 128], idb[:]),
                             reads=[BT, idb], writes=[bank], mark=(k == 3))
                    p.op("dve", lambda: nc.vector.tensor_copy(B_tm[:, :, g * 128:(g + 1) * 128], pb[:, 0:512].rearrange("p (a b) -> p a b", a=4)),
                         reads=[bank], writes=[B_tm])
                else:
                    g = cc - 24
                    p.op("act", lambda: nc.scalar.activation(out=CT[:, g, :], in_=acc[:], func=AF.Silu), reads=[acc], writes=[CT])

        def chunk(ci, k, own):
            precast_step(p, c)
            dr = dtr.next(); s = sm.next(); E = Ex.next()
            p.dma("sp", dr[:], c.DT_s[ci * 128:(ci + 1) * 128, :], writes=[dr])
            p.op("dve", lambda: nc.vector.tensor_tensor(s[:, 0, :], dr[:], DTB, ALU.add), reads=[dr, v32], writes=[s])
            p.op("act", lambda: nc.scalar.activation(out=s[:, 1, :], in_=s[:, 0, :], func=AF.Exp), reads=[s], writes=[s])
            p.op("act", lambda: nc.scalar.activation(out=s[:, 2, :], in_=s[:, 1, :], func=AF.Ln, bias=1.0), reads=[s], writes=[s])
            p.op("dve", lambda: nc.vector.tensor_tensor(s[:, 3, :], s[:, 2, :], A_bc[:], ALU.mult), reads=[s, A_bc], writes=[s])
            bk = psr.next()
            p.op("pe", lambda: nc.tensor.matmul(bk[:, 0:NH], TRI, s[:, 3, :], start=True, stop=True), reads=[cst, s], writes=[bk], mark=False)
            p.op("pe", lambda: nc.tensor.matmul(bk[:, NH:2 * NH], ONES, s[:, 3, :], start=True, stop=True), reads=[cst, s], writes=[bk])
            p.op("act", lambda: nc.scalar.copy(s[:, 4:6, :].rearrange("p a b -> p (a b)"), bk[:, 0:2 * NH]), reads=[bk], writes=[s])
            p.op("dve", lambda: nc.vector.tensor_tensor(s[:, 6, :], s[:, 5, :], s[:, 4, :], ALU.subtract), reads=[s], writes=[s])
            p.op("act", lambda: nc.scalar.activation(out=E[:].rearrange("p a b -> p (a b)"), in_=s[:, 4:7, :].rearrange("p a b -> p (a b)"), func=AF.Exp),
                 reads=[s], writes=[E])
            p.op("dve", lambda: nc.vector.tensor_tensor(s[:, 7, :], s[:, 2, :], E[:, 2, :], ALU.mult), reads=[s, E], writes=[s])
            xs3 = xs_tm[:, k, :].rearrange("p (h q) -> p h q", h=NH)
            p.op("dve", lambda: nc.vector.tensor_tensor(xw[:].rearrange("p (h q) -> p h q", h=NH), xs3, bc(s[:, 7, :], HP), ALU.mult),
                 reads=[xs_tm, s], writes=[xw])
            if own:
                p.op("dve", lambda: nc.vector.tensor_tensor(x_dt[:].rearrange("p (h q) -> p h q", h=NH), xs3, bc(s[:, 2, :], HP), ALU.mult),
                     reads=[xs_tm, s], writes=[x_dt])
                p.op("dve", lambda: nc.vector.tensor_tensor(L_all[:], SU.unsqueeze(1).broadcast_to([128, NH, 128]), bc(s[:, 3, :], 128), ALU.mult),
                     reads=[cst, s], writes=[L_all])
            for g in range(NG):
                hs = slice(g * 4, g * 4 + 4)
                cs = slice(g * 256, (g + 1) * 256)
                if own:
                    bseg = psr.next()
                    for hh in range(4):
                        p.op("pe", lambda: nc.tensor.matmul(bseg[:, hh * 128:(hh + 1) * 128], L_all[:, g * 4 + hh, :], TRI, start=True, stop=False),
                             reads=[L_all, cst], writes=[bseg], mark=False)
                        p.op("pe", lambda: nc.tensor.matmul(bseg[:, hh * 128:(hh + 1) * 128], IDF, NEGM[:, 0:128], start=False, stop=True),
                             reads=[cst], writes=[bseg], mark=(hh == 3))
                    dec = decs.next(); MT = MTs.next()
                    p.op("act", lambda: nc.scalar.activation(out=dec[:], in_=bseg[:], func=AF.Exp), reads=[bseg], writes=[dec])
                    bcb = psr.next()
                    p.op("pe", lambda: nc.tensor.matmul(bcb[:, 0:128], BT[:, g, k * 128:(k + 1) * 128], CT[:, g, k * 128:(k + 1) * 128], start=True, stop=True),
                         reads=[BT, CT], writes=[bcb])
                    p.op("dve", lambda: nc.vector.tensor_tensor(MT[:].rearrange("p (h l) -> p h l", h=4), dec[:].rearrange("p (h l) -> p h l", h=4),
                                                                bcb[:, 0:128].unsqueeze(1).broadcast_to([128, 4, 128]), ALU.mult),
                         reads=[dec, bcb], writes=[MT])
                    by = psr.next()
                    for hh in range(4):
                        h = g * 4 + hh
                        p.op("pe", lambda: nc.tensor.matmul(by[:, hh * 64:(hh + 1) * 64], MT[:, hh * 128:(hh + 1) * 128], x_dt[:, h * 64:(h + 1) * 64], start=True, stop=True),
                             reads=[MT, x_dt], writes=[by], mark=False)
                    p.op("pe", lambda: nc.tensor.matmul(by[:, 256:512], CT[:, g, k * 128:(k + 1) * 128], S_bf[:, cs], start=True, stop=True),
                         reads=[CT, S_bf], writes=[by])
                    p.op("dve", lambda: nc.vector.tensor_tensor(tmp[:, cs].rearrange("p (h q) -> p h q", h=4), by[:, 256:512].rearrange("p (h q) -> p h q", h=4),
                                                                bc(E[:, 0, hs], HP), ALU.mult), reads=[by, E], writes=[tmp])
                    p.op("dve", lambda: nc.vector.tensor_tensor(y[:, cs], tmp[:, cs], by[:, 0:256], ALU.add), reads=[tmp, by], writes=[y])
                bs_ = psr.next()
                p.op("pe", lambda: nc.tensor.matmul(bs_[:, 0:256], B_tm[:, k, g * 128:(g + 1) * 128], xw[:, cs], start=True, stop=True),
                     reads=[B_tm, xw], writes=[bs_])
                p.op("dve", lambda: nc.vector.tensor_tensor(S[:, cs].rearrange("p (h q) -> p h q", h=4), S[:, cs].rearrange("p (h q) -> p h q", h=4),
                                                            bc(E[:, 1, hs], HP), ALU.mult), reads=[S, E], writes=[S])
                p.op("dve", lambda: nc.vector.tensor_tensor(S[:, cs], S[:, cs], bs_[:, 0:256], ALU.add), reads=[S, bs_], writes=[S])
                p.op("act", lambda: nc.scalar.copy(S_bf[:, cs], S[:, cs]), reads=[S], writes=[S_bf])
            if not own:
                return
            tt = ci - 16
            p.op("dve", lambda: nc.vector.tensor_tensor(tmp[:].rearrange("p (h q) -> p h q", h=NH), xs3, bc(D_bc, HP), ALU.mult),
                 reads=[xs_tm, v32], writes=[tmp])
            p.op("dve", lambda: nc.vector.tensor_tensor(y[:], y[:], tmp[:], ALU.add), reads=[y, tmp], writes=[y])
            zt = zts.next(); s4 = st4.next()
            p.dma("sp", zt[:], c.Z_s[tt * 128:(tt + 1) * 128, :], writes=[zt])
            p.op("act", lambda: nc.scalar.activation(out=zt[:], in_=zt[:], func=AF.Silu), reads=[zt], writes=[zt])
            p.op("dve", lambda: nc.vector.tensor_tensor(y[:], y[:], zt[:], ALU.mult), reads=[y, zt], writes=[y])
            p.op("act", lambda: nc.scalar.activation(out=junk[:], in_=y[:], func=AF.Square, accum_out=s4[:, 0:1]), reads=[y], writes=[junk, s4])
            p.op("dve", lambda: nc.vector.tensor_scalar(s4[:, 1:2], s4[:, 0:1], 1.0 / DI, EPS, ALU.mult, ALU.add), reads=[s4], writes=[s4])
            p.op("act", lambda: nc.scalar.activation(out=s4[:, 2:3], in_=s4[:, 1:2], func=AF.Ln), reads=[s4], writes=[s4])
            p.op("act", lambda: nc.scalar.activation(out=s4[:, 3:4], in_=s4[:, 2:3], func=AF.Exp, scale=-0.5), reads=[s4], writes=[s4])
            p.op("dve", lambda: nc.vector.scalar_tensor_tensor(yn[:], y[:], s4[:, 3:4], nws[:], ALU.mult, ALU.mult), reads=[y, s4, nws], writes=[yn])
            yT = ynT.next()
            for half in range(2):
                bank = psr.next()
                pb = bank[:].bitcast(BF16)
                for kk in range(8):
                    dc = half * 8 + kk
                    p.op("pe", lambda: nc.tensor.transpose(pb[:, kk * 128:(kk + 1) * 128], yn[:, dc * 128:(dc + 1) * 128], idb[:]),
                         reads=[yn, idb], writes=[bank], mark=(kk == 7))
                if half == 0:
                    p.op("act", lambda: nc.scalar.copy(yT[:, 0:8, :], pb.rearrange("p (a b) -> p a b", a=8)), reads=[bank], writes=[yT])
                else:
                    p.op("dve", lambda: nc.vector.tensor_copy(yT[:, 8:16, :], pb.rearrange("p (a b) -> p a b", a=8)), reads=[bank], writes=[yT])
            p.dma("sp", c.YN_s[:, :, tt * 128:(tt + 1) * 128].rearrange("a p t -> p a t"), yT[:], reads=[yT])

        for sti in range(8):
            own = sti >= 4
            conv_supertile(sti * 512, own)
            for k in range(4):
                chunk(sti * 4 + k, k, own)
            if sti == 3:
                p.op("dve", lambda: nc.vector.tensor_scalar(S[:], S[:], flag[:, 0:1], None, ALU.mult), reads=[S, flag], writes=[S])
                p.op("act", lambda: nc.scalar.copy(S_bf[:], S[:]), reads=[S], writes=[S_bf])
        p.barrier()


def host_common_b(inp, m):
    l = np.arange(128)
    tri = (l[:, None] <= l[None, :]).astype(np.float32)
    su = (l[:, None] > l[None, :]).astype(np.float32)
    ones = np.ones((128, 128), np.float32)
    idf = np.eye(128, dtype=np.float32)
    negm = np.where(l[None, :] < l[:, None], NEG, 0.0).astype(np.float32)
    m["cstB"] = np.ascontiguousarray(np.concatenate([tri, su, ones, idf, np.tile(negm, (1, 4))], axis=1))
    cw = np.asarray(inp["conv_w"])[0]
    cb = np.asarray(inp["conv_b"])[0]
    wb = np.concatenate([cw, cb[None, :]], axis=0)
    m["conv_wb"] = np.ascontiguousarray(wb.reshape(5, 32, 128).transpose(2, 1, 0)).reshape(128, 160)
    m["vec32"] = np.ascontiguousarray(np.stack([np.asarray(inp["dt_bias"])[0], np.asarray(inp["a_log"])[0], np.asarray(inp["d_skip"])[0]]))
    m["ssd_norm_w"] = np.ascontiguousarray(np.asarray(inp["ssd_norm_w"]).reshape(1, DI))
    return m


def phase_c(p, c, ps):
    nc = p.nc
    with ExitStack() as es:
        T = lambda name, shape, dt=F32: p.tile(es, name, shape, dt)
        idb = T("idbC", [128, 128], BF16)
        flag = T("flagC", [128, 2])
        bmid = T("bmid", [128, 4, 256])
        bfirst = T("bfirst", [128, 4, 256])
        qts = Rot([T(f"qt{i}", [128, 4, 128], BF16) for i in range(2)])
        kts = Rot([T(f"kt{i}", [128, 4, 256], BF16) for i in range(2)])
        vts = Rot([T(f"vt{i}", [128, 2, 512], BF16) for i in range(2)])
        scs = Rot([T(f"sc{i}", [128, 4, 256]) for i in range(2)])
        Ps = Rot([T(f"P{i}", [128, 4, 256], BF16) for i in range(2)])
        PTs = Rot([T(f"PT{i}", [128, 8, 128], BF16) for i in range(2)])
        mss = Rot([T(f"ms{i}", [128, 2, 4]) for i in range(2)])
        stg = Rot([T(f"stg{i}", [128, 4, 130]) for i in range(2)])
        psr = Rot(ps)
        p.dma("pool", idb[:], c.ident, writes=[idb])
        p.dma("sp", flag[:], c.flag, writes=[flag])

        for g in range(3):
            d = DIL[g]
            span = 128 * d
            nprevb = NPREV // span
            ncls = 4096 // span
            nown = NTOK // span
            p.dma("sp", bmid[:].rearrange("p a b -> p (a b)"), c.attn_bias[g], writes=[bmid])
            p.op("dve", lambda: nc.vector.tensor_copy(bfirst[:], bmid[:]), reads=[bmid], writes=[bfirst])
            p.op("dve", lambda: nc.vector.tensor_scalar(bfirst[:, :, 0:128], bfirst[:, :, 0:128], flag[:, 1:2], None, ALU.add),
                 reads=[bfirst, flag], writes=[bfirst])
            og = c.O_s[g].rearrange("(j r) c -> r j c", r=d)
            for r in range(d):
                for n in range(nown):
                    precast_step(p, c)
                    qt = qts.next(); kt = kts.next(); vt = vts.next(); sc = scs.next(); P = Ps.next(); PT = PTs.next()
                    ms = mss.next(); sg = stg.next()
                    pos0 = r * (NTOK // d) + n * 128
                    kpos = r * (4096 // d) + (nprevb + n - 1) * 128
                    p.dma("sp", qt[:], c.QT_s[4 * g:4 * g + 4, :, pos0:pos0 + 128].rearrange("h e q -> e h q"), writes=[qt])
                    p.dma("sp", kt[:], c.KT_s[4 * g:4 * g + 4, :, kpos:kpos + 256].rearrange("h e k -> e h k"), writes=[kt])
                    vb = r * ncls + nprevb + n - 1
                    p.dma("sp", vt[:], c.V_s[g, vb:vb + 2].rearrange("b k e -> k b e"), writes=[vt])
                    bias = bfirst if n == 0 else bmid
                    banks = [psr.next(), psr.next()]
                    for hh in range(4):
                        bk = banks[hh // 2]
                        p.op("pe", lambda: nc.tensor.matmul(bk[:, (hh % 2) * 256:(hh % 2 + 1) * 256], qt[:, hh, :], kt[:, hh, :], start=True, stop=True),
                             reads=[qt, kt], writes=[bk], mark=(hh % 2 == 1))
                    for i in range(2):
                        p.op("dve", lambda: nc.vector.tensor_tensor(sc[:, 2 * i:2 * i + 2, :], banks[i][:].rearrange("p (a b) -> p a b", a=2),
                                                                    bias[:, 2 * i:2 * i + 2, :], ALU.add), reads=[banks[i], bias], writes=[sc])
                    p.op("dve", lambda: nc.vector.reduce_max(ms[:, 0, :], sc[:], axis=AX.X), reads=[sc], writes=[ms])
                    p.op("dve", lambda: nc.vector.tensor_tensor(sc[:], sc[:], ms[:, 0, :].unsqueeze(2).broadcast_to([128, 4, 256]), ALU.subtract),
                         reads=[sc, ms], writes=[sc])
                    p.op("act", lambda: nc.scalar.activation(out=P[:], in_=sc[:], func=AF.Exp), reads=[sc], writes=[P])
                    p.op("dve", lambda: nc.vector.reduce_sum(ms[:, 1, :], P[:], axis=AX.X), reads=[P], writes=[ms])
                    bt = psr.next()
                    pb = bt[:].bitcast(BF16)
                    for hh in range(4):
                        for kb in range(2):
                            i = hh * 2 + kb
                            p.op("pe", lambda: nc.tensor.transpose(pb[:, i * 128:(i + 1) * 128], P[:, hh, kb * 128:(kb + 1) * 128], idb[:]),
                                 reads=[P, idb], writes=[bt], mark=(i == 7))
                    p.op("act", lambda: nc.scalar.copy(PT[:], pb.rearrange("p (a b) -> p a b", a=8)), reads=[bt], writes=[PT])
                    bu = psr.next()
                    for hh in range(4):
                        for kb in range(2):
                            p.op("pe", lambda: nc.tensor.matmul(bu[:, hh * 128:(hh + 1) * 128], PT[:, hh * 2 + kb, :], vt[:, kb, hh * 128:(hh + 1) * 128],
                                                                start=(kb == 0), stop=(kb == 1)), reads=[PT, vt], writes=[bu], mark=(hh == 3 and kb == 1))
                    p.op("act", lambda: nc.scalar.copy(sg[:, :, 0:128], bu[:].rearrange("p (a b) -> p a b", a=4)), reads=[bu], writes=[sg])
                    p.op("dve", lambda: nc.vector.tensor_copy(sg[:, :, 128:130], ms[:].rearrange("p a b -> p b a")), reads=[ms], writes=[sg])
                    p.dma("sp", og[r, n * 128:(n + 1) * 128, :], sg[:].rearrange("p a b -> p (a b)"), reads=[sg])
        p.barrier()
        lds = Rot([T(f"ld{i}", [128, 3, 4, 130]) for i in range(2)])
        wk = Rot([T(f"wk{i}", [128, 8, 12]) for i in range(2)])
        ob = Rot([T(f"ob{i}", [128, 4, 128]) for i in range(2)])
        o16 = Rot([T(f"o16_{i}", [128, 4, 128], BF16) for i in range(2)])
        oT = Rot([T(f"oT{i}", [128, 4, 128], BF16) for i in range(2)])
        for tt in range(16):
            ld = lds.next(); w = wk.next(); o = ob.next(); ob16 = o16.next(); ot = oT.next()
            p.dma("sp", ld[:].rearrange("p g h c -> p g (h c)"), c.O_s[:, tt * 128:(tt + 1) * 128, :].rearrange("g t c -> t g c"), writes=[ld])
            mg = ld[:, :, :, 128]
            sgm = ld[:, :, :, 129]
            M = w[:, 0, 0:4]
            p.op("dve", lambda: nc.vector.tensor_tensor(M, mg[:, 0, :], mg[:, 1, :], ALU.max), reads=[ld], writes=[w])
            p.op("dve", lambda: nc.vector.tensor_tensor(M, M, mg[:, 2, :], ALU.max), reads=[ld, w], writes=[w])
            wg3 = w[:, 1, :].rearrange("p (g h) -> p g h", g=3)
            p.op("dve", lambda: nc.vector.tensor_tensor(wg3, mg, M.unsqueeze(1).broadcast_to([128, 3, 4]), ALU.subtract), reads=[ld, w], writes=[w])
            p.op("act", lambda: nc.scalar.activation(out=w[:, 2, :], in_=w[:, 1, :], func=AF.Exp), reads=[w], writes=[w])
            e3 = w[:, 2, :].rearrange("p (g h) -> p g h", g=3)
            ws3 = w[:, 3, :].rearrange("p (g h) -> p g h", g=3)
            p.op("dve", lambda: nc.vector.tensor_tensor(ws3, e3, sgm, ALU.mult), reads=[ld, w], writes=[w])
            den = w[:, 4, 0:4]
            p.op("dve", lambda: nc.vector.tensor_tensor(den, ws3[:, 0, :], ws3[:, 1, :], ALU.add), reads=[w], writes=[w])
            p.op("dve", lambda: nc.vector.tensor_tensor(den, den, ws3[:, 2, :], ALU.add), reads=[w], writes=[w])
            p.op("dve", lambda: nc.vector.reciprocal(w[:, 5, 0:4], den), reads=[w], writes=[w])
            wn3 = w[:, 6, :].rearrange("p (g h) -> p g h", g=3)
            p.op("dve", lambda: nc.vector.tensor_tensor(wn3, e3, w[:, 5, 0:4].unsqueeze(1).broadcast_to([128, 3, 4]), ALU.mult), reads=[w], writes=[w])
            bcw = lambda gi: wn3[:, gi, :].unsqueeze(2).broadcast_to([128, 4, 128])
            p.op("dve", lambda: nc.vector.tensor_tensor(o[:], ld[:, 0, :, 0:128], bcw(0), ALU.mult), reads=[ld, w], writes=[o])
            for gi in (1, 2):
                p.op("dve", lambda: nc.vector.tensor_tensor(ld[:, gi, :, 0:128], ld[:, gi, :, 0:128], bcw(gi), ALU.mult), reads=[ld, w], writes=[ld])
                dst = o[:] if gi == 1 else ob16[:]
                p.op("dve", lambda: nc.vector.tensor_tensor(dst, o[:], ld[:, gi, :, 0:128], ALU.add), reads=[ld, o], writes=[o, ob16])
            bt = psr.next()
            pb = bt[:].bitcast(BF16)
            for hh in range(4):
                p.op("pe", lambda: nc.tensor.transpose(pb[:, hh * 128:(hh + 1) * 128], ob16[:, hh, :], idb[:]), reads=[ob16, idb], writes=[bt], mark=(hh == 3))
            p.op("act", lambda: nc.scalar.copy(ot[:], pb[:, 0:512].rearrange("p (a b) -> p a b", a=4)), reads=[bt], writes=[ot])
            p.dma("sp", c.OT_s[:, :, tt * 128:(tt + 1) * 128].rearrange("h e t -> e h t"), ot[:], reads=[ot])
        p.barrier()


def host_common_c(inp, m):
    slopes = (2.0 ** (-8.0 * np.arange(1, AH + 1) / AH)).astype(np.float32)
    q = np.arange(128)[:, None]
    k = np.arange(256)[None, :]
    rel = (q + 128) - k
    valid = (rel >= 0) & (rel <= 128)
    ab = np.zeros((3, 128, 4, 256), np.float32)
    for g in range(3):
        for hh in range(4):
            bias = -slopes[4 * g + hh] * (rel * DIL[g]).astype(np.float32)
            ab[g, :, hh, :] = np.where(valid, bias, NEG)
    m["attn_bias"] = np.ascontiguousarray(ab.reshape(3, 128, 1024))
    return m


def phase_d(p, c, ps):
    nc = p.nc
    with ExitStack() as es:
        T = lambda name, shape, dt=F32: p.tile(es, name, shape, dt)
        ynT = T("ynT_d", [128, NDC, NTOK], BF16)
        oT = T("oT_d", [128, 4, NTOK], BF16)
        wss = Rot([T(f"wss{i}", [128, NDC, 128], BF16) for i in range(2)])
        was = Rot([T(f"was{i}", [128, 4, 128], BF16) for i in range(2)])
        g0s = Rot([T(f"g0_{i}", [128, 512]) for i in range(2)])
        g1s = Rot([T(f"g1_{i}", [128, 512]) for i in range(2)])
        t1s = Rot([T(f"t1_{i}", [128, 512]) for i in range(2)])
        mgs = Rot([T(f"mg{i}", [128, 512], BF16) for i in range(2)])
        psr = Rot(ps)
        for kc in range(NDC):
            p.dma("sp", ynT[:, kc, :], c.YN_s[kc], writes=[ynT])
        for kc in range(4):
            p.dma("sp", oT[:, kc, :], c.OT_s[kc], writes=[oT])
        for dmc in range(16):
            precast_step(p, c)
            ws = wss.next(); wa = was.next()
            p.dma("pool", ws[:].rearrange("p a b -> p (a b)"), c.w_ssd_fm[dmc], writes=[ws])
            p.dma("pool", wa[:].rearrange("p a b -> p (a b)"), c.w_attn_fm[dmc], writes=[wa])
            for st in range(4):
                ts = slice(st * 512, (st + 1) * 512)
                g0 = g0s.next(); g1 = g1s.next(); t1 = t1s.next(); mg = mgs.next()
                p.dma("sp", g0[:], c.G_s[dmc * 128:(dmc + 1) * 128, ts], writes=[g0])
                p.dma("sp", g1[:], c.G_s[D + dmc * 128:D + (dmc + 1) * 128, ts], writes=[g1])
                b1 = psr.next()
                for kc in range(NDC):
                    p.op("pe", lambda: nc.tensor.matmul(b1[:], ws[:, kc, :], ynT[:, kc, ts], start=(kc == 0), stop=(kc == NDC - 1)),
                         reads=[ws, ynT], writes=[b1], mark=(kc == NDC - 1))
                b2 = psr.next()
                for kc in range(4):
                    p.op("pe", lambda: nc.tensor.matmul(b2[:], wa[:, kc, :], oT[:, kc, ts], start=(kc == 0), stop=(kc == 3)),
                         reads=[wa, oT], writes=[b2], mark=(kc == 3))
                p.op("dve", lambda: nc.vector.tensor_tensor(t1[:], g0[:], b1[:], ALU.mult), reads=[g0, b1], writes=[t1])
                p.op("dve", lambda: nc.vector.tensor_tensor(g1[:], g1[:], b2[:], ALU.mult), reads=[g1, b2], writes=[g1])
                p.op("dve", lambda: nc.vector.tensor_tensor(mg[:], t1[:], g1[:], ALU.add), reads=[t1, g1], writes=[mg])
                p.dma("sp", c.M_s[dmc][:, ts], mg[:], reads=[mg])
        p.barrier()
    with ExitStack() as es:
        T = lambda name, shape, dt=F32: p.tile(es, name, shape, dt)
        mT = T("mT_d", [128, NDC, NTOK], BF16)
        wos = Rot([T(f"wo{i}", [128, NDC, 512], BF16) for i in range(2)])
        xss = Rot([T(f"xs_d{i}", [128, 512]) for i in range(3)])
        psr = Rot(ps)
        for kc in range(NDC):
            p.dma("sp", mT[:, kc, :], c.M_s[kc], writes=[mT])
        for ct in range(4):
            wo = wos.next()
            p.dma("pool", wo[:].rearrange("p a b -> p (a b)"), c.w_out_tm[ct], writes=[wo])
            for tt in range(16):
                xs_ = xss.next()
                p.dma("sp", xs_[:], c.x_own[tt * 128:(tt + 1) * 128, ct * 512:(ct + 1) * 512], writes=[xs_])
                bk = psr.next()
                for kc in range(NDC):
                    p.op("pe", lambda: nc.tensor.matmul(bk[:], mT[:, kc, tt * 128:(tt + 1) * 128], wo[:, kc, :], start=(kc == 0), stop=(kc == NDC - 1)),
                         reads=[mT, wo], writes=[bk], mark=(kc == NDC - 1))
                p.op("dve", lambda: nc.vector.tensor_tensor(xs_[:], xs_[:], bk[:], ALU.add), reads=[xs_, bk], writes=[xs_])
                p.dma("sp", c.X2_s[tt * 128:(tt + 1) * 128, ct * 512:(ct + 1) * 512], xs_[:], reads=[xs_])
        p.barrier()


def phase_e0(p, c, ps):
    nc = p.nc
    with ExitStack() as es:
        T = lambda name, shape, dt=F32: p.tile(es, name, shape, dt)
        idf = T("idf_e", [128, 128])
        fw = T("fw_e", [128, D])
        wr = T("wr_e", [128, NDC, 36])
        br = T("br_e", [128, 36])
        x2s = Rot([T(f"x2_{i}", [128, D]) for i in range(2)])
        hns = Rot([T(f"hn_{i}", [128, D]) for i in range(2)])
        junk = T("junk_e", [128, D], BF16)
        st4 = Rot([T(f"st4e_{i}", [128, 4]) for i in range(2)])
        h32 = Rot([T(f"h32_{i}", [128, NDC, 128]) for i in range(2)])
        h16 = Rot([T(f"h16_{i}", [128, NDC, 128], BF16) for i in range(2)])
        rt = Rot([T(f"rt{i}", [128, 8, 36]) for i in range(2)])
        sc = Rot([T(f"rs{i}", [128, 16]) for i in range(2)])
        gt = Rot([T(f"gt{i}", [128, NE]) for i in range(2)])
        psr = Rot(ps)
        p.dma("sp", idf[:], c.ident, writes=[idf])
        p.dma("sp", fw[:], c.ffn_norm_w.partition_broadcast(128), writes=[fw])
        p.dma("sp", wr[:].rearrange("p a b -> p (a b)"), c.w_router, writes=[wr])
        p.dma("sp", br[:], c.b_router.partition_broadcast(128), writes=[br])
        for tt in range(16):
            x2 = x2s.next(); hn = hns.next(); s4 = st4.next(); a32 = h32.next(); a16 = h16.next()
            R = rt.next(); S = sc.next(); G = gt.next()
            p.dma("sp", x2[:], c.X2_s[tt * 128:(tt + 1) * 128, :], writes=[x2])
            p.op("act", lambda: nc.scalar.activation(out=junk[:], in_=x2[:], func=AF.Square, accum_out=s4[:, 0:1]), reads=[x2], writes=[junk, s4])
            p.op("dve", lambda: nc.vector.tensor_scalar(s4[:, 1:2], s4[:, 0:1], 1.0 / D, EPS, ALU.mult, ALU.add), reads=[s4], writes=[s4])
            p.op("act", lambda: nc.scalar.activation(out=s4[:, 2:3], in_=s4[:, 1:2], func=AF.Ln), reads=[s4], writes=[s4])
            p.op("act", lambda: nc.scalar.activation(out=s4[:, 3:4], in_=s4[:, 2:3], func=AF.Exp, scale=-0.5), reads=[s4], writes=[s4])
            p.op("dve", lambda: nc.vector.scalar_tensor_tensor(hn[:], x2[:], s4[:, 3:4], fw[:], ALU.mult, ALU.mult), reads=[x2, s4, fw], writes=[hn])
            for q4 in range(4):
                bk = psr.next()
                for k in range(4):
                    dc = q4 * 4 + k
                    p.op("pe", lambda: nc.tensor.transpose(bk[:, k * 128:(k + 1) * 128], hn[:, dc * 128:(dc + 1) * 128], idf[:]),
                         reads=[hn, idf], writes=[bk], mark=(k == 3))
                p.op("act", lambda: nc.scalar.copy(a32[:, q4 * 4:q4 * 4 + 4, :], bk[:].rearrange("p (a b) -> p a b", a=4)), reads=[bk], writes=[a32])
                p.op("dve", lambda: nc.vector.tensor_copy(a16[:, q4 * 4:q4 * 4 + 4, :], bk[:].rearrange("p (a b) -> p a b", a=4)), reads=[bk], writes=[a16])
            p.dma("sp", c.HN_s[:, :, tt * 128:(tt + 1) * 128].rearrange("a p t -> p a t"), a16[:], reads=[a16])
            bk = psr.next()
            for kc in range(NDC):
                p.op("pe", lambda: nc.tensor.matmul(bk[:, 0:36], a32[:, kc, :], wr[:, kc, :], start=(kc == 0), stop=(kc == NDC - 1)),
                     reads=[a32, wr], writes=[bk], mark=(kc == NDC - 1))
            L = R[:, 0, :]
            dv = lambda fn, reads, writes: p.op("dve", fn, reads=reads, writes=writes)
            dv(lambda: nc.vector.tensor_tensor(L, bk[:, 0:36], br[:], ALU.add), [bk, br], [R])
            gl = R[:, 0, 0:4]
            el = R[:, 0, 4:36]
            dv(lambda: nc.vector.reduce_max(S[:, 0:1], gl, axis=AX.X), [R], [S])
            dv(lambda: nc.vector.tensor_scalar(R[:, 1, 0:4], gl, S[:, 0:1], None, ALU.subtract), [R, S], [R])
            p.op("act", lambda: nc.scalar.activation(out=R[:, 1, 4:8], in_=R[:, 1, 0:4], func=AF.Exp, accum_out=S[:, 1:2]), reads=[R], writes=[R, S])
            dv(lambda: nc.vector.reciprocal(S[:, 2:3], S[:, 1:2]), [S], [S])
            dv(lambda: nc.vector.tensor_scalar(R[:, 1, 8:12], gl, S[:, 0:1], None, ALU.is_equal), [R, S], [R])
            dv(lambda: nc.vector.tensor_scalar(R[:, 1, 12:16], R[:, 1, 8:12], -NEG, NEG, ALU.mult, ALU.add), [R], [R])
            elm = R[:, 2, 0:32]
            dv(lambda: nc.vector.tensor_tensor(elm.rearrange("p (g e) -> p g e", g=4), el.rearrange("p (g e) -> p g e", g=4),
                                               R[:, 1, 12:16].unsqueeze(2).broadcast_to([128, 4, 8]), ALU.add), [R], [R])
            dv(lambda: nc.vector.reduce_max(S[:, 3:4], elm, axis=AX.X), [R], [S])
            oh1 = R[:, 3, 0:32]
            dv(lambda: nc.vector.tensor_scalar(oh1, elm, S[:, 3:4], None, ALU.is_equal), [R, S], [R])
            elm2 = R[:, 4, 0:32]
            dv(lambda: nc.vector.scalar_tensor_tensor(elm2, oh1, NEG, elm, ALU.mult, ALU.add), [R], [R])
            dv(lambda: nc.vector.reduce_max(S[:, 4:5], elm2, axis=AX.X), [R], [S])
            oh2 = R[:, 5, 0:32]
            dv(lambda: nc.vector.tensor_scalar(oh2, elm2, S[:, 4:5], None, ALU.is_equal), [R, S], [R])
            dv(lambda: nc.vector.tensor_tensor(S[:, 5:6], S[:, 4:5], S[:, 3:4], ALU.subtract), [S], [S])
            p.op("act", lambda: nc.scalar.activation(out=S[:, 6:7], in_=S[:, 5:6], func=AF.Exp), reads=[S], writes=[S])
            dv(lambda: nc.vector.tensor_scalar(S[:, 7:8], S[:, 6:7], 1.0, None, ALU.add), [S], [S])
            dv(lambda: nc.vector.reciprocal(S[:, 8:9], S[:, 7:8]), [S], [S])
            dv(lambda: nc.vector.tensor_tensor(S[:, 9:10], S[:, 8:9], S[:, 2:3], ALU.mult), [S], [S])
            dv(lambda: nc.vector.tensor_tensor(S[:, 10:11], S[:, 9:10], S[:, 6:7], ALU.mult), [S], [S])
            dv(lambda: nc.vector.tensor_scalar(G[:], oh1, S[:, 9:10], None, ALU.mult), [R, S], [G])
            dv(lambda: nc.vector.scalar_tensor_tensor(G[:], oh2, S[:, 10:11], G[:], ALU.mult, ALU.add), [R, S, G], [G])
            p.dma("sp", c.GATE_s[tt * 128:(tt + 1) * 128, :], G[:], reads=[G])
        p.barrier()


def phase_e(p, c, ps):
    nc = p.nc
    NP = 1024
    with ExitStack() as es:
        T = lambda name, shape, dt=F32: p.tile(es, name, shape, dt)
        fnw = T("fnw", [128, D])
        gates = T("gates", [128, 16, NE])
        hnT = T("hnT_e", [128, NDC, NP], BF16)
        yacc = T("yacc", [128, NP // 128, D])
        wgs = Rot([T(f"wg{i}", [128, NDC, 256], BF16) for i in range(2)])
        wus = Rot([T(f"wu{i}", [128, NDC, 256], BF16) for i in range(2)])
        wds = Rot([T(f"wd{i}", [128, 2, D], BF16) for i in range(2)])
        sils = Rot([T(f"sil{i}", [128, 512]) for i in range(2)])
        hms = Rot([T(f"hm{i}", [128, 2, NP], BF16) for i in range(2)])
        junk = T("junk_m", [128, D], BF16)
        st4 = Rot([T(f"st4m_{i}", [128, 4]) for i in range(2)])
        psr = Rot(ps)
        p.dma("sp", fnw[:], c.final_norm_w.partition_broadcast(128), writes=[fnw])
        p.dma("sp", gates[:], c.GATE_s.rearrange("(t p) e -> p t e", p=128), writes=[gates])
        for ps_i in range(NTOK // NP):
            t0 = ps_i * NP
            for kc in range(NDC):
                p.dma("sp", hnT[:, kc, :], c.HN_s[kc][:, t0:t0 + NP], writes=[hnT])
            for t in range(NP // 128):
                p.dma("sp", yacc[:, t, :], c.X2_s[t0 + t * 128:t0 + (t + 1) * 128, :], writes=[yacc])
            for e in range(NE):
                for fc in range(4):
                    wg = wgs.next(); wu = wus.next(); wd = wds.next(); hm = hms.next()
                    p.dma("pool", wg[:].rearrange("p a b -> p (a b)"), c.wg_t[e, fc], writes=[wg])
                    p.dma("pool", wu[:].rearrange("p a b -> p (a b)"), c.wu_t[e, fc], writes=[wu])
                    p.dma("pool", wd[:].rearrange("p a b -> p (a b)"), c.wd_t[e, fc], writes=[wd])
                    for j in range(2):
                        for ts in range(NP // 512):
                            tsl = slice(ts * 512, (ts + 1) * 512)
                            bg = psr.next(); bu = psr.next(); sil = sils.next()
                            for kc in range(NDC):
                                p.op("pe", lambda: nc.tensor.matmul(bg[:], wg[:, kc, j * 128:(j + 1) * 128], hnT[:, kc, tsl], start=(kc == 0), stop=(kc == NDC - 1)),
                                     reads=[wg, hnT], writes=[bg], mark=(kc == NDC - 1))
                            for kc in range(NDC):
                                p.op("pe", lambda: nc.tensor.matmul(bu[:], wu[:, kc, j * 128:(j + 1) * 128], hnT[:, kc, tsl], start=(kc == 0), stop=(kc == NDC - 1)),
                                     reads=[wu, hnT], writes=[bu], mark=(kc == NDC - 1))
                            p.op("act", lambda: nc.scalar.activation(out=sil[:], in_=bg[:], func=AF.Silu), reads=[bg], writes=[sil])
                            p.op("dve", lambda: nc.vector.tensor_tensor(hm[:, j, tsl], sil[:], bu[:], ALU.mult), reads=[sil, bu], writes=[hm])
                    for t in range(NP // 128):
                        gcol = gates[:, ps_i * (NP // 128) + t, e:e + 1]
                        for dtile in range(4):
                            dsl = slice(dtile * 512, (dtile + 1) * 512)
                            bk = psr.next()
                            for j in range(2):
                                p.op("pe", lambda: nc.tensor.matmul(bk[:], hm[:, j, t * 128:(t + 1) * 128], wd[:, j, dsl], start=(j == 0), stop=(j == 1)),
                                     reads=[hm, wd], writes=[bk], mark=(j == 1))
                            p.op("dve", lambda: nc.vector.scalar_tensor_tensor(yacc[:, t, dsl], bk[:], gcol, yacc[:, t, dsl], ALU.mult, ALU.add),
                                 reads=[bk, gates, yacc], writes=[yacc])
            for t in range(NP // 128):
                s4 = st4.next()
                p.op("act", lambda: nc.scalar.activation(out=junk[:], in_=yacc[:, t, :], func=AF.Square, accum_out=s4[:, 0:1]), reads=[yacc], writes=[junk, s4])
                p.op("dve", lambda: nc.vector.tensor_scalar(s4[:, 1:2], s4[:, 0:1], 1.0 / D, EPS, ALU.mult, ALU.add), reads=[s4], writes=[s4])
                p.op("act", lambda: nc.scalar.activation(out=s4[:, 2:3], in_=s4[:, 1:2], func=AF.Ln), reads=[s4], writes=[s4])
                p.op("act", lambda: nc.scalar.activation(out=s4[:, 3:4], in_=s4[:, 2:3], func=AF.Exp, scale=-0.5), reads=[s4], writes=[s4])
                p.op("dve", lambda: nc.vector.scalar_tensor_tensor(yacc[:, t, :], yacc[:, t, :], s4[:, 3:4], fnw[:], ALU.mult, ALU.mult),
                     reads=[yacc, s4, fnw], writes=[yacc])
                p.dma("sp", c.out[t0 + t * 128:t0 + (t + 1) * 128, :], yacc[:, t, :], reads=[yacc])
        p.barrier()


def host_common_de(inp, m):
    m["w_ssd_fm"] = tile_w(np.asarray(inp["w_ssd_out"])[0], 128)
    m["w_attn_fm"] = tile_w(np.asarray(inp["w_attn_out"])[0], 128)
    m["w_out_tm"] = tile_w(np.asarray(inp["w_out"])[0], 512)
    m["ffn_norm_w"] = np.ascontiguousarray(np.asarray(inp["ffn_norm_w"]).reshape(1, D))
    wr = np.concatenate([np.asarray(inp["w_group_router"])[0], np.asarray(inp["w_expert_router"])[0]], axis=1)
    m["w_router"] = tile_w(wr, 36)[0]
    m["b_router"] = np.ascontiguousarray(np.concatenate([np.asarray(inp["b_group_router"])[0], np.asarray(inp["b_expert_router"])[0]]).reshape(1, 36))
    m["final_norm_w"] = np.ascontiguousarray(np.asarray(inp["final_norm_w"]).reshape(1, D))
    return m


def build_program(debug=False, phases="abcdeE"):
    nc = bass.Bass("TRN2", target_bir_lowering=False)
    c = declare_io(nc, debug=debug, phases=phases)
    with ExitStack() as es:
        p = Prog(nc, es)
        ps = [Tile(f"ps{i}", es.enter_context(nc.psum_tensor(f"psum{i}", [128, 512], F32))) for i in range(8)]
        for b in ps:
            b.excl = True
        if "a" in phases:
            phase_a(p, c, ps)
        if "b" in phases:
            phase_b(p, c, ps)
        if "c" in phases:
            phase_c(p, c, ps)
        if "d" in phases:
            phase_d(p, c, ps)
        rt_ = route_tiles(p, es)
        if "e" in phases:
            phase_e0s(p, c, ps, rt_)
            phase_e1(p, c, ps, rt_)
        if "E" in phases:
            phase_es(p, c, ps, rt_)
            phase_ec(p, c, ps, rt_)
        p.barrier()
    return nc, p


def host_maps(inp):
    m = host_common(inp)
    host_common_b(inp, m)
    host_common_c(inp, m)
    host_common_de(inp, m)
    host_common_s(inp, m)
    x = np.asarray(inp["x"])
    maps = []
    zeros = np.zeros((NPREV, D), np.float32)
    for core in range(8):
        b, half = core // 2, core % 2
        mm = dict(m)
        mm["x_own"] = np.ascontiguousarray(x[b, half * NTOK:(half + 1) * NTOK])
        mm["x_prev"] = np.ascontiguousarray(x[b, 0:NPREV]) if half == 1 else zeros
        fl = np.array([[1.0, 0.0]], np.float32) if half == 1 else np.array([[0.0, NEG]], np.float32)
        mm["flag"] = np.ascontiguousarray(np.tile(fl, (128, 1)))
        maps.append(mm)
    return maps


_PROG = {}


def kernel(**inputs):
    if "nc" not in _PROG:
        _PROG["nc"], _ = build_program()
    nc = _PROG["nc"]
    maps = host_maps(inputs)
    res = run_bass_kernel_spmd(nc, maps, core_ids=list(range(8)))
    x = np.asarray(inputs["x"])
    out = np.empty(x.shape, np.float32)
    for core in range(8):
        b, half = core // 2, core % 2
        out[b, half * NTOK:(half + 1) * NTOK] = np.asarray(res.results[core]["out"], np.float32)
    return out


def route_tiles(p, es):
    r = Ctx()
    T = lambda name, shape, dt=F32: p.tile(es, name, shape, dt)
    r.OH1 = T("OH1", [128, 16, NE])
    r.OH2 = T("OH2", [128, 16, NE])
    r.PG = T("PG", [128, 16, 2])
    r.POSI = T("POSI", [128, 2, 16], I32)
    r.IDXI = T("IDXI", [128, NSLOT], I32)
    return r


def phase_e0s(p, c, ps, rt_):
    nc = p.nc
    with ExitStack() as es:
        T = lambda name, shape, dt=F32: p.tile(es, name, shape, dt)
        idf = T("idf_e", [128, 128])
        fw = T("fw_e", [128, D])
        wr = T("wr_e", [128, NDC, 36])
        br = T("br_e", [128, 36])
        zt = T("zt_e", [128, D], BF16)
        x2s = Rot([T(f"x2_{i}", [128, D]) for i in range(2)])
        hns = Rot([T(f"hn_{i}", [128, D]) for i in range(2)])
        hbs = Rot([T(f"hb_{i}", [128, D], BF16) for i in range(2)])
        junk = T("junk_e", [128, D], BF16)
        st4 = Rot([T(f"st4e_{i}", [128, 4]) for i in range(2)])
        h32 = Rot([T(f"h32_{i}", [128, NDC, 128]) for i in range(2)])
        rt = Rot([T(f"rt{i}", [128, 8, 36]) for i in range(2)])
        sc = Rot([T(f"rs{i}", [128, 16]) for i in range(2)])
        psr = Rot(ps)
        p.dma("sp", idf[:], c.ident, writes=[idf])
        p.dma("sp", fw[:], c.ffn_norm_w.partition_broadcast(128), writes=[fw])
        p.dma("sp", wr[:].rearrange("p a b -> p (a b)"), c.w_router, writes=[wr])
        p.dma("sp", br[:], c.b_router.partition_broadcast(128), writes=[br])
        p.op("dve", lambda: nc.vector.memset(zt[:], 0.0), writes=[zt])
        for i in range(NSLOT):
            p.dma("sp", c.XS_s[i * 128:(i + 1) * 128, :], zt[:], reads=[zt])
        for tt in range(16):
            x2 = x2s.next(); hn = hns.next(); hb = hbs.next(); s4 = st4.next(); a32 = h32.next()
            R = rt.next(); S = sc.next()
            p.dma("sp", x2[:], c.X2_s[tt * 128:(tt + 1) * 128, :], writes=[x2])
            p.op("act", lambda: nc.scalar.activation(out=junk[:], in_=x2[:], func=AF.Square, accum_out=s4[:, 0:1]), reads=[x2], writes=[junk, s4])
            p.op("dve", lambda: nc.vector.tensor_scalar(s4[:, 1:2], s4[:, 0:1], 1.0 / D, EPS, ALU.mult, ALU.add), reads=[s4], writes=[s4])
            p.op("act", lambda: nc.scalar.activation(out=s4[:, 2:3], in_=s4[:, 1:2], func=AF.Ln), reads=[s4], writes=[s4])
            p.op("act", lambda: nc.scalar.activation(out=s4[:, 3:4], in_=s4[:, 2:3], func=AF.Exp, scale=-0.5), reads=[s4], writes=[s4])
            p.op("dve", lambda: nc.vector.scalar_tensor_tensor(hn[:], x2[:], s4[:, 3:4], fw[:], ALU.mult, ALU.mult), reads=[x2, s4, fw], writes=[hn])
            p.op("act", lambda: nc.scalar.copy(hb[:], hn[:]), reads=[hn], writes=[hb])
            p.dma("sp", c.HNTM_s[tt * 128:(tt + 1) * 128, :], hb[:], reads=[hb])
            for q4 in range(4):
                bk = psr.next()
                for k in range(4):
                    dc = q4 * 4 + k
                    p.op("pe", lambda: nc.tensor.transpose(bk[:, k * 128:(k + 1) * 128], hn[:, dc * 128:(dc + 1) * 128], idf[:]),
                         reads=[hn, idf], writes=[bk], mark=(k == 3))
                if q4 % 2 == 0:
                    p.op("act", lambda: nc.scalar.copy(a32[:, q4 * 4:q4 * 4 + 4, :], bk[:].rearrange("p (a b) -> p a b", a=4)), reads=[bk], writes=[a32])
                else:
                    p.op("dve", lambda: nc.vector.tensor_copy(a32[:, q4 * 4:q4 * 4 + 4, :], bk[:].rearrange("p (a b) -> p a b", a=4)), reads=[bk], writes=[a32])
            bk = psr.next()
            for kc in range(NDC):
                p.op("pe", lambda: nc.tensor.matmul(bk[:, 0:36], a32[:, kc, :], wr[:, kc, :], start=(kc == 0), stop=(kc == NDC - 1)),
                     reads=[a32, wr], writes=[bk], mark=(kc == NDC - 1))
            L = R[:, 0, :]
            dv = lambda fn, reads, writes: p.op("dve", fn, reads=reads, writes=writes)
            dv(lambda: nc.vector.tensor_tensor(L, bk[:, 0:36], br[:], ALU.add), [bk, br], [R])
            gl = R[:, 0, 0:4]
            el = R[:, 0, 4:36]
            dv(lambda: nc.vector.reduce_max(S[:, 0:1], gl, axis=AX.X), [R], [S])
            dv(lambda: nc.vector.tensor_scalar(R[:, 1, 0:4], gl, S[:, 0:1], None, ALU.subtract), [R, S], [R])
            p.op("act", lambda: nc.scalar.activation(out=R[:, 1, 4:8], in_=R[:, 1, 0:4], func=AF.Exp, accum_out=S[:, 1:2]), reads=[R], writes=[R, S])
            dv(lambda: nc.vector.reciprocal(S[:, 2:3], S[:, 1:2]), [S], [S])
            dv(lambda: nc.vector.tensor_scalar(R[:, 1, 8:12], gl, S[:, 0:1], None, ALU.is_equal), [R, S], [R])
            dv(lambda: nc.vector.tensor_scalar(R[:, 1, 12:16], R[:, 1, 8:12], -NEG, NEG, ALU.mult, ALU.add), [R], [R])
            elm = R[:, 2, 0:32]
            dv(lambda: nc.vector.tensor_tensor(elm.rearrange("p (g e) -> p g e", g=4), el.rearrange("p (g e) -> p g e", g=4),
                                               R[:, 1, 12:16].unsqueeze(2).broadcast_to([128, 4, 8]), ALU.add), [R], [R])
            dv(lambda: nc.vector.reduce_max(S[:, 3:4], elm, axis=AX.X), [R], [S])
            oh1 = rt_.OH1[:, tt, :]
            dv(lambda: nc.vector.tensor_scalar(oh1, elm, S[:, 3:4], None, ALU.is_equal), [R, S], [rt_.OH1])
            elm2 = R[:, 4, 0:32]
            dv(lambda: nc.vector.scalar_tensor_tensor(elm2, oh1, NEG, elm, ALU.mult, ALU.add), [R, rt_.OH1], [R])
            dv(lambda: nc.vector.reduce_max(S[:, 4:5], elm2, axis=AX.X), [R], [S])
            oh2 = rt_.OH2[:, tt, :]
            dv(lambda: nc.vector.tensor_scalar(oh2, elm2, S[:, 4:5], None, ALU.is_equal), [R, S], [rt_.OH2])
            dv(lambda: nc.vector.tensor_tensor(S[:, 5:6], S[:, 4:5], S[:, 3:4], ALU.subtract), [S], [S])
            p.op("act", lambda: nc.scalar.activation(out=S[:, 6:7], in_=S[:, 5:6], func=AF.Exp), reads=[S], writes=[S])
            dv(lambda: nc.vector.tensor_scalar(S[:, 7:8], S[:, 6:7], 1.0, None, ALU.add), [S], [S])
            dv(lambda: nc.vector.reciprocal(S[:, 8:9], S[:, 7:8]), [S], [S])
            dv(lambda: nc.vector.tensor_tensor(rt_.PG[:, tt, 0:1], S[:, 8:9], S[:, 2:3], ALU.mult), [S], [rt_.PG])
            dv(lambda: nc.vector.tensor_tensor(rt_.PG[:, tt, 1:2], rt_.PG[:, tt, 0:1], S[:, 6:7], ALU.mult), [S, rt_.PG], [rt_.PG])
        p.barrier()


def phase_e1(p, c, ps, rt_):
    nc = p.nc
    with ExitStack() as es:
        T = lambda name, shape, dt=F32: p.tile(es, name, shape, dt)
        cst = T("cstE", [128, 328])
        SUTR, ONES, THR, C8 = cst[:, 0:128], cst[:, 128:256], cst[:, 256:320], cst[:, 320:328]
        OHS = T("OHS", [128, 16, NE])
        CUM = T("CUM", [128, 17, NE])
        RK = T("RK", [128, 16, NE])
        W = T("W_e1", [128, 12, NE])
        CMP = T("CMP", [128, NSLOT, NE])
        ET = T("ET", [128, 2, NSLOT])
        IDXF = T("IDXF", [128, NSLOT])
        TT = T("TT_e1", [128, 16, NE])
        MM = T("MM_e1", [128, 16, NE])
        POSF = T("POSF", [128, 2, 16])
        hbs = Rot([T(f"hb1_{i}", [128, D], BF16) for i in range(3)])
        psr = Rot(ps)
        dv = lambda fn, reads, writes: p.op("dve", fn, reads=reads, writes=writes)
        p.dma("sp", cst[:], c.cstE, writes=[cst])
        dv(lambda: nc.vector.tensor_tensor(OHS[:], rt_.OH1[:], rt_.OH2[:], ALU.add), [rt_.OH1, rt_.OH2], [OHS])
        dv(lambda: nc.vector.memset(CUM[:, 0, :], 0.0), [], [CUM])
        for i in range(16):
            dv(lambda: nc.vector.tensor_tensor(CUM[:, i + 1, :], CUM[:, i, :], OHS[:, i, :], ALU.add), [CUM, OHS], [CUM])
        for i in range(16):
            bk = psr.next()
            p.op("pe", lambda: nc.tensor.matmul(bk[:, 0:NE], SUTR, OHS[:, i, :], start=True, stop=False), reads=[cst, OHS], writes=[bk], mark=False)
            p.op("pe", lambda: nc.tensor.matmul(bk[:, 0:NE], ONES, CUM[:, i, :], start=False, stop=True), reads=[cst, CUM], writes=[bk])
            p.op("act", lambda: nc.scalar.copy(RK[:, i, :], bk[:, 0:NE]), reads=[bk], writes=[RK])
        bk = psr.next()
        p.op("pe", lambda: nc.tensor.matmul(bk[:, 0:NE], ONES, CUM[:, 16, :], start=True, stop=True), reads=[cst, CUM], writes=[bk])
        cnt, r_, pf, pad, off = W[:, 0, :], W[:, 1, :], W[:, 2, :], W[:, 3, :], W[:, 6, :]
        p.op("act", lambda: nc.scalar.copy(cnt, bk[:, 0:NE]), reads=[bk], writes=[W])
        C2 = CMP[:, 0:NE, 0:16]
        dv(lambda: nc.vector.tensor_tensor(C2, cnt.unsqueeze(2).broadcast_to([128, NE, 16]), THR[:, 0:16].unsqueeze(1).broadcast_to([128, NE, 16]), ALU.is_gt),
           [W, cst], [CMP])
        dv(lambda: nc.vector.reduce_sum(pf, C2, axis=AX.X), [CMP], [W])
        dv(lambda: nc.vector.tensor_scalar(pad, pf, 128.0, None, ALU.mult), [W], [W])
        a, b = 4, 5
        dv(lambda: nc.vector.tensor_copy(W[:, a, :], pad), [W], [W])
        for sft in (1, 2, 4, 8, 16):
            dv(lambda: nc.vector.tensor_copy(W[:, b, 0:sft], W[:, a, 0:sft]), [W], [W])
            dv(lambda: nc.vector.tensor_tensor(W[:, b, sft:NE], W[:, a, sft:NE], W[:, a, 0:NE - sft], ALU.add), [W], [W])
            a, b = b, a
        END = W[:, a, :]
        dv(lambda: nc.vector.tensor_tensor(off, END, pad, ALU.subtract), [W], [W])
        dv(lambda: nc.vector.tensor_tensor(CMP[:], END.unsqueeze(1).broadcast_to([128, NSLOT, NE]), THR.unsqueeze(2).broadcast_to([128, NSLOT, NE]), ALU.is_le),
           [W, cst], [CMP])
        dv(lambda: nc.vector.reduce_sum(ET[:, 0, :], CMP[:], axis=AX.X), [CMP], [ET])
        dv(lambda: nc.vector.tensor_scalar(ET[:, 1, :], ET[:, 0, :], float(NE - 1), 128.0, ALU.min, ALU.mult), [ET], [ET])
        dv(lambda: nc.vector.tensor_scalar(IDXF[:], ET[:, 1, :], C8[:, 0:1], None, ALU.add), [ET, cst], [IDXF])
        dv(lambda: nc.vector.tensor_copy(rt_.IDXI[:], IDXF[:]), [IDXF], [rt_.IDXI])
        dv(lambda: nc.vector.tensor_tensor(TT[:], RK[:], off.unsqueeze(1).broadcast_to([128, 16, NE]), ALU.add), [RK, W], [TT])
        for k, OH in enumerate((rt_.OH1, rt_.OH2)):
            dv(lambda: nc.vector.tensor_tensor(MM[:], TT[:], OH[:], ALU.mult), [TT, OH], [MM])
            dv(lambda: nc.vector.reduce_sum(POSF[:, k, :], MM[:], axis=AX.X), [MM], [POSF])
        dv(lambda: nc.vector.tensor_copy(rt_.POSI[:], POSF[:]), [POSF], [rt_.POSI])
        xsb = Buf("XS_s")
        for tt in range(16):
            hb = hbs.next()
            p.dma("sp", hb[:], c.HNTM_s[tt * 128:(tt + 1) * 128, :], writes=[hb])
            for k in range(2):
                p.idma(c.XS_s, bass.IndirectOffsetOnAxis(rt_.POSI[:, k, tt:tt + 1], 0), hb[:], None, reads=[hb, rt_.POSI], writes=[xsb])
        p.barrier()


def phase_es(p, c, ps, rt_):
    nc = p.nc
    with ExitStack() as es:
        T = lambda name, shape, dt=F32: p.tile(es, name, shape, dt)
        idb = T("idb_s", [128, 128], BF16)
        xss = Rot([T(f"xs_s{i}", [128, D], BF16) for i in range(2)])
        xTs = Rot([T(f"xT_s{i}", [128, NDC, 128], BF16) for i in range(2)])
        units = Rot([T(f"wU{i}", [128, 3, 4096], BF16) for i in range(5)])
        sils = Rot([T(f"silS{i}", [128, 256]) for i in range(2)])
        hms = Rot([T(f"hmS{i}", [128, 8, 128], BF16) for i in range(2)])
        yts = Rot([T(f"ytS{i}", [128, D]) for i in range(2)])
        ybanks = ps[0:4]
        psr = Rot(ps[4:8])
        p.dma("pool", idb[:], c.ident, writes=[idb])
        precast_step(p, c, 1000)
        for tk in c.precast_toks:
            p._wait("pool", tk)
        pending = []

        def down(i, fc, w, hm, yt):
            for dtile in range(4):
                dsl = slice(dtile * 512, (dtile + 1) * 512)
                for j in range(2):
                    p.op("pe", lambda: nc.tensor.matmul(ybanks[dtile][:], hm[:, fc * 2 + j, :], w[:, 2, :].rearrange("p (a b) -> p a b", a=2)[:, j, dsl],
                                                        start=(fc == 0 and j == 0), stop=(fc == 3 and j == 1)),
                         reads=[hm, w], writes=[ybanks[dtile]], mark=(dtile == 3 and j == 1))
            if fc == 3:
                for dtile in range(4):
                    dsl = slice(dtile * 512, (dtile + 1) * 512)
                    if dtile % 2 == 0:
                        p.op("act", lambda: nc.scalar.copy(yt[:, dsl], ybanks[dtile][:]), reads=[ybanks[dtile]], writes=[yt])
                    else:
                        p.op("dve", lambda: nc.vector.tensor_copy(yt[:, dsl], ybanks[dtile][:]), reads=[ybanks[dtile]], writes=[yt])
                p.dma("sp", c.YP_s[i * 128:(i + 1) * 128, :], yt[:], reads=[yt])

        for i in range(NSLOT):
            xs_ = xss.next(); xT = xTs.next(); hm = hms.next(); yt = yts.next()
            p.dma("sp", xs_[:], c.XS_s[i * 128:(i + 1) * 128, :], writes=[xs_])
            for half in range(2):
                bank = psr.next()
                pb = bank[:].bitcast(BF16)
                for k in range(8):
                    dc = half * 8 + k
                    p.op("pe", lambda: nc.tensor.transpose(pb[:, k * 128:(k + 1) * 128], xs_[:, dc * 128:(dc + 1) * 128], idb[:]),
                         reads=[xs_, idb], writes=[bank], mark=(k == 7))
                if half == 0:
                    p.op("act", lambda: nc.scalar.copy(xT[:, 0:8, :], pb.rearrange("p (a b) -> p a b", a=8)), reads=[bank], writes=[xT])
                else:
                    p.op("dve", lambda: nc.vector.tensor_copy(xT[:, 8:16, :], pb.rearrange("p (a b) -> p a b", a=8)), reads=[bank], writes=[xT])
            for fc in range(4):
                w = units.next(); sil = sils.next()
                off = bass.IndirectOffsetOnAxis(rt_.IDXI[:, i:i + 1], 0)
                p.idma(w[:].rearrange("p a b -> p (a b)"), None, c.WB_s[fc], off, reads=[rt_.IDXI], writes=[w])
                bg = psr.next(); bu = psr.next()
                for (bank, m_) in ((bg, 0), (bu, 1)):
                    for j in range(2):
                        for kc in range(NDC):
                            lhs = w[:, m_, :].rearrange("p (h a b) -> p h a b", h=2, a=8)[:, kc // 8, kc % 8, j * 128:(j + 1) * 128]
                            p.op("pe", lambda: nc.tensor.matmul(bank[:, j * 128:(j + 1) * 128], lhs, xT[:, kc, :], start=(kc == 0), stop=(kc == NDC - 1)),
                                 reads=[w, xT], writes=[bank], mark=(kc == NDC - 1 and j == 1))
                p.op("act", lambda: nc.scalar.activation(out=sil[:], in_=bg[:, 0:256], func=AF.Silu), reads=[bg], writes=[sil])
                p.op("dve", lambda: nc.vector.tensor_tensor(hm[:, fc * 2:fc * 2 + 2, :], sil[:].rearrange("p (a b) -> p a b", a=2),
                                                            bu[:, 0:256].rearrange("p (a b) -> p a b", a=2), ALU.mult), reads=[sil, bu], writes=[hm])
                for fn in pending:
                    fn()
                pending = [lambda i=i, fc=fc, w=w, hm=hm, yt=yt: down(i, fc, w, hm, yt)]
        for fn in pending:
            fn()
        p.barrier()


def phase_ec(p, c, ps, rt_):
    nc = p.nc
    with ExitStack() as es:
        T = lambda name, shape, dt=F32: p.tile(es, name, shape, dt)
        fnw = T("fnw", [128, D])
        y1s = Rot([T(f"y1_{i}", [128, D]) for i in range(2)])
        y2s = Rot([T(f"y2_{i}", [128, D]) for i in range(2)])
        x2s = Rot([T(f"x2c_{i}", [128, D]) for i in range(2)])
        junk = T("junk_c", [128, D], BF16)
        st4 = Rot([T(f"st4c_{i}", [128, 4]) for i in range(2)])
        p.dma("sp", fnw[:], c.final_norm_w.partition_broadcast(128), writes=[fnw])
        for tt in range(16):
            y1 = y1s.next(); y2 = y2s.next(); x2 = x2s.next(); s4 = st4.next()
            p.dma("sp", x2[:], c.X2_s[tt * 128:(tt + 1) * 128, :], writes=[x2])
            p.idma(y1[:], None, c.YP_s, bass.IndirectOffsetOnAxis(rt_.POSI[:, 0, tt:tt + 1], 0), reads=[rt_.POSI], writes=[y1])
            p.idma(y2[:], None, c.YP_s, bass.IndirectOffsetOnAxis(rt_.POSI[:, 1, tt:tt + 1], 0), reads=[rt_.POSI], writes=[y2])
            p.op("dve", lambda: nc.vector.scalar_tensor_tensor(x2[:], y1[:], rt_.PG[:, tt, 0:1], x2[:], ALU.mult, ALU.add), reads=[y1, rt_.PG, x2], writes=[x2])
            p.op("dve", lambda: nc.vector.scalar_tensor_tensor(x2[:], y2[:], rt_.PG[:, tt, 1:2], x2[:], ALU.mult, ALU.add), reads=[y2, rt_.PG, x2], writes=[x2])
            p.op("act", lambda: nc.scalar.activation(out=junk[:], in_=x2[:], func=AF.Square, accum_out=s4[:, 0:1]), reads=[x2], writes=[junk, s4])
            p.op("dve", lambda: nc.vector.tensor_scalar(s4[:, 1:2], s4[:, 0:1], 1.0 / D, EPS, ALU.mult, ALU.add), reads=[s4], writes=[s4])
            p.op("act", lambda: nc.scalar.activation(out=s4[:, 2:3], in_=s4[:, 1:2], func=AF.Ln), reads=[s4], writes=[s4])
            p.op("act", lambda: nc.scalar.activation(out=s4[:, 3:4], in_=s4[:, 2:3], func=AF.Exp, scale=-0.5), reads=[s4], writes=[s4])
            p.op("dve", lambda: nc.vector.scalar_tensor_tensor(y1[:], x2[:], s4[:, 3:4], fnw[:], ALU.mult, ALU.mult), reads=[x2, s4, fnw], writes=[y1])
            p.dma("sp", c.out[tt * 128:(tt + 1) * 128, :], y1[:], reads=[y1])
        p.barrier()


def host_common_s(inp, m):
    for k in ("wg_t", "wu_t", "wd_t"):
        m.pop(k, None)
    wg = np.asarray(inp["w_exp_gate"])[0]
    wu = np.asarray(inp["w_exp_up"])[0]
    wd = np.asarray(inp["w_exp_down"])[0]
    tg = lambda w: np.ascontiguousarray(w.reshape(NE, 2, 8, 128, 4, 256).transpose(4, 0, 3, 1, 2, 5)).reshape(4, NE * 128, 4096)
    m["wg_r"] = tg(wg)
    m["wu_r"] = tg(wu)
    m["wd_r"] = np.ascontiguousarray(wd.reshape(NE, 4, 2, 128, D).transpose(1, 0, 3, 2, 4)).reshape(4, NE * 128, 4096)
    l = np.arange(128)
    sutr = (l[:, None] < l[None, :]).astype(np.float32)
    ones = np.ones((128, 128), np.float32)
    thr = np.tile((128.0 * np.arange(NSLOT, dtype=np.float32))[None, :], (128, 1))
    c8 = (np.arange(8, dtype=np.float32)[None, :] * 128.0 + l[:, None].astype(np.float32))
    m["cstE"] = np.ascontiguousarray(np.concatenate([sutr, ones, thr, c8], axis=1).astype(np.float32))
    return m
```

```python
import math
from contextlib import ExitStack

import numpy as np
import concourse.bass as bass
import concourse.mybir as mybir
from concourse.bass_utils import run_bass_kernel_spmd

F32 = mybir.dt.float32
BF16 = mybir.dt.bfloat16
AF = mybir.ActivationFunctionType
ALU = mybir.AluOpType
AX = mybir.AxisListType

D = 2048
NTOK = 2048
NPREV = 2048
NDC = D // 128
DI = 2048
NH = 32
HP = 64
NG = 8
DS = 128
CONV_DIM = DI + 2 * NG * DS
AH = 12
AE = 128
IN_COLS = 14880
C_Z, C_XBC, C_DT, C_Q, C_K, C_V, C_G = 0, 2048, 6144, 6176, 7712, 9248, 10784
DIL = (1, 4, 16)
NE = 32
DFF = 1024
EPS = 1e-6
NEG = -30000.0
NSLOT = 64
I32 = mybir.dt.int32


class Tok:
    __slots__ = ("sem", "val")

    def __init__(self, sem, val):
        self.sem = sem
        self.val = val


class Buf:
    def __init__(self, name=""):
        self.name = name
        self.w = []
        self.r = {}


class Tile(Buf):
    def __init__(self, name, t):
        super().__init__(name)
        self.t = t

    def __getitem__(self, k):
        return self.t[k]


def _flat(bs):
    out = []
    for b in bs:
        if b is None:
            continue
        if isinstance(b, (list, tuple)):
            out.extend(_flat(b))
        else:
            out.append(b)
    return out


class Prog:
    ENG = ("pe", "act", "dve", "pool", "sp")

    def __init__(self, nc, es):
        self.nc = nc
        self.es = es
        self.eng = {"pe": nc.tensor, "act": nc.scalar, "dve": nc.vector, "pool": nc.gpsimd, "sp": nc.sync}
        self.sem = {e: es.enter_context(nc.semaphore("s_" + e)) for e in self.ENG}
        self.cnt = {e: 0 for e in self.ENG}
        self.waited = {e: {} for e in self.ENG}
        nslots = {"sp": 28, "pool": 24, "act": 20, "bg": 14}
        self.slots = {q: [[es.enter_context(nc.semaphore(f"d_{q}{i}")), 0] for i in range(n)]
                      for q, n in nslots.items()}
        self.rr = {q: 0 for q in nslots}
        self.n_inst = 0

    def _wait(self, e, tok):
        if tok is None:
            return
        key = tok.sem.name
        if e == "pe" and tok.sem is self.sem["pe"]:
            return
        if self.waited[e].get(key, 0) >= tok.val:
            return
        self.eng[e].wait_ge(tok.sem, tok.val)
        self.waited[e][key] = tok.val
        self.n_inst += 1

    def _deps(self, e, reads, writes):
        for b in reads:
            for t in b.w:
                self._wait(e, t)
        for b in writes:
            for t in b.w:
                self._wait(e, t)
            for t in b.r.values():
                self._wait(e, t)

    def _commit(self, e, tok, reads, writes):
        for b in writes:
            b.w = [tok]
            b.r = {}
        for b in reads:
            if b not in writes:
                b.r[e] = tok

    def op(self, e, fn, reads=(), writes=(), mark=True):
        reads = _flat(reads)
        writes = _flat(writes)
        ex = [b for b in reads if getattr(b, "excl", False)]
        if ex:
            reads = [b for b in reads if b not in ex]
            writes = writes + [b for b in ex if b not in writes]
        self._deps(e, reads, writes)
        ins = fn()
        self.n_inst += 1
        if mark:
            self.cnt[e] += 1
            ins.then_inc(self.sem[e], 1)
            tok = Tok(self.sem[e], self.cnt[e])
        else:
            assert e == "pe"
            tok = Tok(self.sem[e], self.cnt[e] + 1)
        self._commit(e, tok, reads, writes)
        return tok

    def dma(self, q, out, in_, reads=(), writes=(), bg=False):
        reads = _flat(reads)
        writes = _flat(writes)
        if q == "sp" and reads and not writes:
            q = "act"
        sk = "bg" if bg else q
        slot = self.slots[sk][self.rr[sk]]
        self.rr[sk] = (self.rr[sk] + 1) % len(self.slots[sk])
        if slot[1] > 0:
            self._wait(q, Tok(slot[0], slot[1]))
        self._deps(q, reads, writes)
        ins = self.eng[q].dma_start(out=out, in_=in_)
        self.n_inst += 1
        slot[1] += 16
        ins.then_inc(slot[0], 16)
        tok = Tok(slot[0], slot[1])
        self._commit(q, tok, reads, writes)
        return tok

    def idma(self, out, out_off, in_, in_off, reads=(), writes=()):
        q = "pool"
        reads = _flat(reads)
        writes = _flat(writes)
        slot = self.slots[q][self.rr[q]]
        self.rr[q] = (self.rr[q] + 1) % len(self.slots[q])
        if slot[1] > 0:
            self._wait(q, Tok(slot[0], slot[1]))
        self._deps(q, reads, writes)
        ins = self.nc.gpsimd.indirect_dma_start(out, out_off, in_, in_off)
        self.n_inst += 1
        slot[1] += 16
        ins.then_inc(slot[0], 16)
        tok = Tok(slot[0], slot[1])
        self._commit(q, tok, reads, writes)
        return tok

    def barrier(self):
        toks = [Tok(self.sem[e], self.cnt[e]) for e in self.ENG if self.cnt[e] > 0]
        for q, sl in self.slots.items():
            if q == "bg":
                continue
            for s, c in sl:
                if c > 0:
                    toks.append(Tok(s, c))
        for e in self.ENG:
            for t in toks:
                if t.sem is self.sem[e]:
                    continue
                self._wait(e, t)

    def tile(self, es, name, shape, dtype):
        self.n_tiles = getattr(self, "n_tiles", 0) + 1
        t = es.enter_context(self.nc.sbuf_tensor(f"sb{self.n_tiles}_{name}", list(shape), dtype))
        return Tile(name, t)


class Rot:
    def __init__(self, tiles):
        self.tiles = tiles
        self.i = 0

    def next(self):
        t = self.tiles[self.i]
        self.i = (self.i + 1) % len(self.tiles)
        return t


def tile_w(w2d, cw):
    K, C = w2d.shape
    assert C % cw == 0 and K % 128 == 0
    kc = K // 128
    a = w2d.reshape(kc, 128, C // cw, cw).transpose(2, 1, 0, 3)
    return np.ascontiguousarray(a).reshape(C // cw, 128, kc * cw)


class Ctx:
    pass


def declare_io(nc, debug=False, phases="abcdeE"):
    c = Ctx()
    di = lambda name, shape, dt=F32: nc.dram_tensor(name, list(shape), dt, kind="ExternalInput").ap()
    c.x_own = di("x_own", [NTOK, D])
    c.x_prev = di("x_prev", [NPREV, D])
    c.w_fm = di("w_fm", [88, 128, NDC * 128])
    c.w_tm = di("w_tm", [7, 128, NDC * 512])
    c.w_dt = di("w_dt", [128, NDC * 32])
    c.attn_norm_w = di("attn_norm_w", [1, D])
    c.b_gate = di("b_gate", [128, 32])
    c.ident = di("ident", [128, 128])
    c.cstB = di("cstB", [128, 1024])
    c.conv_wb = di("conv_wb", [128, 32 * 5])
    c.vec32 = di("vec32", [3, NH])
    c.ssd_norm_w = di("ssd_norm_w", [1, DI])
    c.flag = di("flag", [128, 2])
    c.attn_bias = di("attn_bias", [3, 128, 1024])
    c.w_ssd_fm = di("w_ssd_fm", [16, 128, NDC * 128])
    c.w_attn_fm = di("w_attn_fm", [16, 128, 4 * 128])
    c.w_out_tm = di("w_out_tm", [4, 128, NDC * 512])
    c.ffn_norm_w = di("ffn_norm_w", [1, D])
    c.w_router = di("w_router", [128, NDC * 36])
    c.b_router = di("b_router", [1, 36])
    if "E" in phases:
        c.wg_r = di("wg_r", [4, NE * 128, 4096])
        c.wu_r = di("wu_r", [4, NE * 128, 4096])
        c.wd_r = di("wd_r", [4, NE * 128, 4096])
        c.WB_s = [nc.dram_tensor(f"WB_s{fc}", [NE * 128, 3 * 4096], BF16).ap() for fc in range(4)]
        c.precast = [(fc, k, src, r0) for fc in range(4) for r0 in range(0, NE * 128, 512) for (k, src) in ((0, c.wg_r), (1, c.wu_r), (2, c.wd_r))]
        c.precast_toks = []
    c.cstE = di("cstE", [128, 128 + 128 + 64 + 8])
    c.final_norm_w = di("final_norm_w", [1, D])
    c.out = nc.dram_tensor("out", [NTOK, D], F32, kind="ExternalOutput").ap()
    kind = "ExternalOutput" if debug else "Internal"
    ds = lambda name, shape, dt=F32: nc.dram_tensor(name, list(shape), dt, kind=kind).ap()
    c.XBC_s = ds("XBC_s", [CONV_DIM, 3 + NPREV + NTOK])
    c.Z_s = ds("Z_s", [NTOK, DI])
    c.DT_s = ds("DT_s", [NPREV + NTOK, NH])
    c.QT_s = ds("QT_s", [AH, 128, NTOK], BF16)
    c.KT_s = ds("KT_s", [AH, 128, NPREV + NTOK], BF16)
    c.V_s = ds("V_s", [3, 32, 128, 512], BF16)
    c.G_s = ds("G_s", [2 * D, NTOK])
    c.YN_s = ds("YN_s", [NDC, 128, NTOK], BF16)
    c.O_s = ds("O_s", [3, NTOK, 4 * 130])
    c.OT_s = ds("OT_s", [4, 128, NTOK], BF16)
    c.M_s = ds("M_s", [NDC, 128, NTOK], BF16)
    c.X2_s = ds("X2_s", [NTOK, D])
    c.HN_s = ds("HN_s", [NDC, 128, NTOK], BF16)
    c.GATE_s = ds("GATE_s", [NTOK, NE])
    c.HNTM_s = ds("HNTM_s", [NTOK, D], BF16)
    c.XS_s = ds("XS_s", [NSLOT * 128, D], BF16)
    c.YP_s = ds("YP_s", [NSLOT * 128, D])
    return c


def precast_step(p, c, n=1):
    for _ in range(n):
        if not getattr(c, "precast", None):
            return
        fc, k, src, r0 = c.precast.pop(0)
        dst = c.WB_s[fc][r0:r0 + 512, k * 4096:(k + 1) * 4096].rearrange("r (a b) -> r a b", a=2)
        c.precast_toks.append(p.dma("pool", dst, src[fc, r0:r0 + 512, :].rearrange("r (a b) -> r a b", a=2), bg=True))


def cls_view(ap2d, d):
    if d == 1:
        return ap2d.unsqueeze(1)
    return ap2d.rearrange("p (j r) -> p r j", r=d)


def phase_a(p, c, ps):
    nc = p.nc
    with ExitStack() as es:
        hT = p.tile(es, "hT", [128, NDC, 2048], BF16)
        nw = p.tile(es, "nw_bc", [128, D], F32)
        bg = p.tile(es, "bg", [128, 32], F32)
        idb = p.tile(es, "idb", [128, 128], BF16)
        zero = p.tile(es, "zero", [128, 4], F32)
        xts = Rot([p.tile(es, f"xt{i}", [128, D], F32) for i in range(2)])
        hbs = Rot([p.tile(es, f"hb{i}", [128, D], BF16) for i in range(2)])
        junk = p.tile(es, "junk", [128, D], BF16)
        sts = Rot([p.tile(es, f"st{i}", [128, 4], F32) for i in range(2)])
        wfm = Rot([p.tile(es, f"wfm{i}", [128, NDC, 128], BF16) for i in range(3)])
        wtm = Rot([p.tile(es, f"wtm{i}", [128, NDC, 512], BF16) for i in range(2)])
        wdt = p.tile(es, "wdt", [128, NDC, 32], BF16)
        so32 = Rot([p.tile(es, f"so32_{i}", [128, 512], F32) for i in range(4)])
        so16 = Rot([p.tile(es, f"so16_{i}", [128, 512], BF16) for i in range(4)])
        psr = Rot(ps)
        ev = [0]

        p.dma("sp", nw[:], c.attn_norm_w.partition_broadcast(128), writes=[nw])
        p.dma("sp", bg[:], c.b_gate, writes=[bg])
        p.dma("pool", idb[:], c.ident, writes=[idb])
        p.dma("pool", wdt[:].rearrange("p a b -> p (a b)"), c.w_dt, writes=[wdt])
        p.op("dve", lambda: nc.vector.memset(zero[:], 0.0), writes=[zero])
        for r0 in range(0, CONV_DIM, 128):
            p.dma("sp", c.XBC_s[r0:r0 + 128, 0:3], zero[:, 0:3], reads=[zero])

        def evac_copy(dst_ap, src_ap, reads, writes, scale=None):
            e = "act" if ev[0] % 2 == 0 else "dve"
            ev[0] += 1
            if e == "act":
                if scale is None:
                    return p.op("act", lambda: nc.scalar.copy(dst_ap, src_ap), reads=reads, writes=writes)
                return p.op("act", lambda: nc.scalar.activation(out=dst_ap, in_=src_ap, func=AF.Copy, scale=scale),
                            reads=reads, writes=writes)
            if scale is None:
                return p.op("dve", lambda: nc.vector.tensor_copy(dst_ap, src_ap), reads=reads, writes=writes)
            return p.op("dve", lambda: nc.vector.tensor_scalar(dst_ap, src_ap, scale, None, ALU.mult),
                        reads=reads, writes=writes)

        def build_hT(xsrc):
            for tt in range(16):
                xt = xts.next(); hb = hbs.next(); st = sts.next()
                p.dma("sp", xt[:], xsrc[tt * 128:(tt + 1) * 128, :], writes=[xt])
                p.op("act", lambda: nc.scalar.activation(out=junk[:], in_=xt[:], func=AF.Square, accum_out=st[:, 0:1]),
                     reads=[xt], writes=[junk, st])
                p.op("dve", lambda: nc.vector.tensor_scalar(st[:, 1:2], st[:, 0:1], 1.0 / D, EPS, ALU.mult, ALU.add),
                     reads=[st], writes=[st])
                p.op("act", lambda: nc.scalar.activation(out=st[:, 2:3], in_=st[:, 1:2], func=AF.Ln), reads=[st], writes=[st])
                p.op("act", lambda: nc.scalar.activation(out=st[:, 3:4], in_=st[:, 2:3], func=AF.Exp, scale=-0.5),
                     reads=[st], writes=[st])
                p.op("dve", lambda: nc.vector.scalar_tensor_tensor(hb[:], xt[:], st[:, 3:4], nw[:], ALU.mult, ALU.mult),
                     reads=[xt, st, nw], writes=[hb])
                for half in range(2):
                    bank = psr.next()
                    pb = bank[:].bitcast(BF16)
                    for k in range(8):
                        dc = half * 8 + k
                        p.op("pe", lambda: nc.tensor.transpose(pb[:, k * 128:(k + 1) * 128], hb[:, dc * 128:(dc + 1) * 128], idb[:]),
                             reads=[hb, idb], writes=[bank], mark=(k == 7))
                    evac_copy(hT[:, half * 8:half * 8 + 8, tt * 128:(tt + 1) * 128],
                              pb.rearrange("p (a b) -> p a b", a=8), [bank], [hT])

        def mm_group(bank, out_ap, lhs_fn, rhs_fn, reads):
            for dc in range(NDC):
                p.op("pe", lambda: nc.tensor.matmul(out_ap, lhs_fn(dc), rhs_fn(dc), start=(dc == 0), stop=(dc == NDC - 1)),
                     reads=reads, writes=[bank], mark=(dc == NDC - 1))

        def fm_chunk(cc, jobs):
            wb = wfm.next()
            p.dma("pool", wb[:].rearrange("p a b -> p (a b)"), c.w_fm[cc], writes=[wb])
            for rhs_fn, n, epi in jobs:
                bank = psr.next()
                mm_group(bank, bank[:, 0:n], lambda dc: wb[:, dc, :], rhs_fn, [wb, hT])
                epi(bank, n)

        def nat_rhs(st):
            return lambda dc: hT[:, dc, st * 512:(st + 1) * 512]

        def epi_store32(dst_ap):
            def f(bank, n):
                so = so32.next()
                evac_copy(so[:, 0:n], bank[:, 0:n], [bank], [so])
                p.dma("sp", dst_ap, so[:, 0:n], reads=[so])
            return f

        def epi_store16(dst_ap, scale=None, n3=None):
            def f(bank, n):
                so = so16.next()
                evac_copy(so[:, 0:n], bank[:, 0:n], [bank], [so], scale=scale)
                src = so[:, 0:n]
                if n3 is not None:
                    src = src.rearrange("p (a b) -> p a b", a=n3)
                p.dma("sp", dst_ap, src, reads=[so])
            return f

        def epi_gate(cc, dst_ap):
            def f(bank, n):
                so = so32.next()
                p.op("act", lambda: nc.scalar.activation(out=so[:, 0:n], in_=bank[:, 0:n], func=AF.Sigmoid, bias=bg[:, cc:cc + 1]),
                     reads=[bank, bg], writes=[so])
                p.dma("sp", dst_ap, so[:, 0:n], reads=[so])
            return f

        def kt_view(h, d):
            return c.KT_s[h].rearrange("e (r n j) -> e r n j", r=d, j=128)

        def tm_tile(wt_ap_fn, ncols, lhs_list, dst_list, dtype32=True):
            for lhs_fn, dst in zip(lhs_list, dst_list):
                bank = psr.next()
                mm_group(bank, bank[:, 0:ncols], lhs_fn, wt_ap_fn, [hT] + tm_reads[0])
                if dtype32:
                    so = so32.next()
                else:
                    so = so16.next()
                evac_copy(so[:, 0:ncols], bank[:, 0:ncols], [bank], [so])
                p.dma("sp", dst, so[:, 0:ncols], reads=[so])

        tm_reads = [[]]

        def v_blocks(g, prev):
            d = DIL[g]
            span = 128 * d
            nprevb = NPREV // span
            ncls = 4096 // span
            lhs, dst = [], []
            if prev:
                blocks = [(r, nprevb - 1) for r in range(d)]
            else:
                blocks = [(r, n) for r in range(d) for n in range(NTOK // span)]
            for r, n in blocks:
                def lf(dc, r=r, n=n):
                    return cls_view(hT[:, dc, :], d)[:, r, n * 128:(n + 1) * 128]
                lhs.append(lf)
                ng = n if prev else nprevb + n
                dst.append(c.V_s[g, r * ncls + ng])
            return lhs, dst

        build_hT(c.x_prev)
        for cc in range(24):
            fm_chunk(cc, [(nat_rhs(st), 512, epi_store32(c.XBC_s[cc * 128:(cc + 1) * 128, 3 + st * 512: 3 + (st + 1) * 512]))
                          for st in range(4)])
        for cc in range(24, 32):
            fm_chunk(cc, [(nat_rhs(3), 512, epi_store32(c.XBC_s[cc * 128:(cc + 1) * 128, 3 + 3 * 512: 3 + 4 * 512]))])
        for h in range(AH):
            g = h // 4
            d = DIL[g]
            cc = 44 + h
            if g == 0:
                jobs = [(lambda dc: hT[:, dc, 1920:2048], 128, epi_store16(c.KT_s[h][:, 15 * 128:16 * 128]))]
            elif g == 1:
                jobs = [(lambda dc: cls_view(hT[:, dc, :], 4)[:, :, 384:512], 512,
                         epi_store16(kt_view(h, 4)[:, :, 3, :], n3=4))]
            else:
                jobs = [((lambda dc, st=st: cls_view(hT[:, dc, :], 16)[:, 4 * st:4 * st + 4, :]), 512,
                         epi_store16(kt_view(h, 16)[:, 4 * st:4 * st + 4, 0, :], n3=4)) for st in range(4)]
            fm_chunk(cc, jobs)
        tm_reads[0] = [wdt]
        tm_tile(lambda dc: wdt[:, dc, :], 32,
                [(lambda dc, tt=tt: hT[:, dc, tt * 128:(tt + 1) * 128]) for tt in range(16)],
                [c.DT_s[tt * 128:(tt + 1) * 128, :] for tt in range(16)])
        for g in range(3):
            wt = wtm.next()
            p.dma("pool", wt[:].rearrange("p a b -> p (a b)"), c.w_tm[4 + g], writes=[wt])
            tm_reads[0] = [wt]
            lhs, dst = v_blocks(g, True)
            tm_tile(lambda dc: wt[:, dc, :], 512, lhs, dst, dtype32=False)

        build_hT(c.x_own)
        for cc in range(32):
            precast_step(p, c, 2)
            fm_chunk(cc, [(nat_rhs(st), 512,
                           epi_store32(c.XBC_s[cc * 128:(cc + 1) * 128, 3 + NPREV + st * 512: 3 + NPREV + (st + 1) * 512]))
                          for st in range(4)])
        inv = 1.0 / math.sqrt(AE)
        for h in range(AH):
            g = h // 4
            d = DIL[g]
            if g == 0:
                jobs = [(nat_rhs(st), 512, epi_store16(c.QT_s[h][:, st * 512:(st + 1) * 512], scale=inv)) for st in range(4)]
            elif g == 1:
                jobs = [((lambda dc, r=r: cls_view(hT[:, dc, :], 4)[:, r, :]), 512,
                         epi_store16(c.QT_s[h][:, r * 512:(r + 1) * 512], scale=inv)) for r in range(4)]
            else:
                jobs = [((lambda dc, st=st: cls_view(hT[:, dc, :], 16)[:, 4 * st:4 * st + 4, :]), 512,
                         epi_store16(c.QT_s[h][:, st * 512:(st + 1) * 512], scale=inv)) for st in range(4)]
            fm_chunk(32 + h, jobs)
            if g == 0:
                jobs = [(nat_rhs(st), 512, epi_store16(c.KT_s[h][:, (16 + 4 * st) * 128:(20 + 4 * st) * 128])) for st in range(4)]
            elif g == 1:
                jobs = [((lambda dc, r=r: cls_view(hT[:, dc, :], 4)[:, r, :]), 512,
                         epi_store16(c.KT_s[h][:, r * 1024 + 512:(r + 1) * 1024])) for r in range(4)]
            else:
                jobs = [((lambda dc, st=st: cls_view(hT[:, dc, :], 16)[:, 4 * st:4 * st + 4, :]), 512,
                         epi_store16(kt_view(h, 16)[:, 4 * st:4 * st + 4, 1, :], n3=4)) for st in range(4)]
            fm_chunk(44 + h, jobs)
        for cc in range(32):
            fm_chunk(56 + cc, [(nat_rhs(st), 512, epi_gate(cc, c.G_s[cc * 128:(cc + 1) * 128, st * 512:(st + 1) * 512]))
                               for st in range(4)])
        for ct in range(4):
            wt = wtm.next()
            p.dma("pool", wt[:].rearrange("p a b -> p (a b)"), c.w_tm[ct], writes=[wt])
            tm_reads[0] = [wt]
            tm_tile(lambda dc: wt[:, dc, :], 512,
                    [(lambda dc, tt=tt: hT[:, dc, tt * 128:(tt + 1) * 128]) for tt in range(16)],
                    [c.Z_s[tt * 128:(tt + 1) * 128, ct * 512:(ct + 1) * 512] for tt in range(16)])
        tm_reads[0] = [wdt]
        tm_tile(lambda dc: wdt[:, dc, :], 32,
                [(lambda dc, tt=tt: hT[:, dc, tt * 128:(tt + 1) * 128]) for tt in range(16)],
                [c.DT_s[NPREV + tt * 128:NPREV + (tt + 1) * 128, :] for tt in range(16)])
        for g in range(3):
            wt = wtm.next()
            p.dma("pool", wt[:].rearrange("p a b -> p (a b)"), c.w_tm[4 + g], writes=[wt])
            tm_reads[0] = [wt]
            lhs, dst = v_blocks(g, False)
            tm_tile(lambda dc: wt[:, dc, :], 512, lhs, dst, dtype32=False)
        p.barrier()


def host_common(inp):
    w_in = np.asarray(inp["w_in"])[0]
    fm_cols = np.concatenate([w_in[:, C_XBC:C_DT], w_in[:, C_Q:C_K], w_in[:, C_K:C_V], w_in[:, C_G:IN_COLS]], axis=1)
    tm_cols = np.concatenate([w_in[:, C_Z:C_XBC], w_in[:, C_V:C_G]], axis=1)
    m = {}
    m["w_fm"] = tile_w(fm_cols, 128)
    m["w_tm"] = tile_w(tm_cols, 512)
    m["w_dt"] = tile_w(w_in[:, C_DT:C_Q], 32)[0]
    m["attn_norm_w"] = np.ascontiguousarray(np.asarray(inp["attn_norm_w"]).reshape(1, D))
    m["b_gate"] = np.ascontiguousarray(np.asarray(inp["b_gate"]).reshape(32, 128).T)
    m["ident"] = np.eye(128, dtype=np.float32)
    return m


def phase_b(p, c, ps):
    nc = p.nc
    with ExitStack() as es:
        T = lambda name, shape, dt=F32: p.tile(es, name, shape, dt)
        cst = T("cstB", [128, 1024])
        TRI, SU, ONES, IDF, NEGM = (cst[:, 0:128], cst[:, 128:256], cst[:, 256:384], cst[:, 384:512], cst[:, 512:1024])
        idb = T("idbB", [128, 128], BF16)
        cwb = T("cwb", [128, 32, 5])
        v32 = T("v32", [128, 3, NH])
        A_bc = T("A_bc", [128, NH])
        nws = T("nws", [128, DI])
        flag = T("flagB", [128, 2])
        us = Rot([T(f"u{i}", [128, 515]) for i in range(3)])
        accs = Rot([T(f"acc{i}", [128, 512]) for i in range(2)])
        xcs = Rot([T(f"xc{i}", [128, 512]) for i in range(2)])
        xs_tm = T("xs_tm", [128, 4, DI])
        B_tm = T("B_tm", [128, 4, NG * DS], BF16)
        BT = T("BT", [128, NG, 512], BF16)
        CT = T("CT", [128, NG, 512], BF16)
        dtr = Rot([T(f"dtr{i}", [128, NH]) for i in range(2)])
        sm = Rot([T(f"sm{i}", [128, 8, NH]) for i in range(2)])
        Ex = Rot([T(f"Ex{i}", [128, 3, NH]) for i in range(2)])
        L_all = T("L_all", [128, NH, 128])
        x_dt = T("x_dt", [128, DI], BF16)
        xw = T("xw", [128, DI], BF16)
        decs = Rot([T(f"dec{i}", [128, 512]) for i in range(2)])
        MTs = Rot([T(f"MT{i}", [128, 512], BF16) for i in range(2)])
        y = T("y", [128, DI])
        tmp = T("tmpB", [128, DI])
        zts = Rot([T(f"zt{i}", [128, DI]) for i in range(2)])
        yn = T("yn", [128, DI], BF16)
        junk = T("junkB", [128, DI], BF16)
        st4 = Rot([T(f"st4_{i}", [128, 4]) for i in range(2)])
        S = T("S", [128, DI])
        S_bf = T("S_bf", [128, DI], BF16)
        ynT = Rot([T(f"ynT{i}", [128, NDC, 128], BF16) for i in range(2)])
        psr = Rot(ps)

        p.dma("sp", cst[:], c.cstB, writes=[cst])
        p.dma("pool", idb[:], c.ident, writes=[idb])
        p.dma("sp", cwb[:].rearrange("p a b -> p (a b)"), c.conv_wb, writes=[cwb])
        p.dma("sp", v32[:].rearrange("p a b -> p (a b)"), c.vec32.rearrange("a b -> (a b)").partition_broadcast(128), writes=[v32])
        p.dma("sp", nws[:], c.ssd_norm_w.partition_broadcast(128), writes=[nws])
        p.dma("sp", flag[:], c.flag, writes=[flag])
        p.op("act", lambda: nc.scalar.activation(out=A_bc[:], in_=v32[:, 1, :], func=AF.Exp), reads=[v32], writes=[A_bc])
        p.op("dve", lambda: nc.vector.tensor_scalar(A_bc[:], A_bc[:], -1.0, None, ALU.mult), reads=[A_bc], writes=[A_bc])
        p.op("dve", lambda: nc.vector.memset(S[:], 0.0), writes=[S])
        p.op("dve", lambda: nc.vector.memset(S_bf[:], 0.0), writes=[S_bf])
        DTB = v32[:, 0, :]
        D_bc = v32[:, 2, :]
        bc = lambda ap, n: ap.unsqueeze(2).broadcast_to([128, ap.shape[1], n])

        def conv_supertile(t0, own):
            ncc = 32 if own else 24
            for cc in range(ncc):
                u = us.next(); acc = accs.next(); xc = xcs.next()
                p.dma("sp", u[:], c.XBC_s[cc * 128:(cc + 1) * 128, t0:t0 + 515], writes=[u])
                p.op("dve", lambda: nc.vector.tensor_scalar(acc[:], u[:, 0:512], cwb[:, cc, 0:1], cwb[:, cc, 4:5], ALU.mult, ALU.add),
                     reads=[u, cwb], writes=[acc])
                for w in range(1, 4):
                    p.op("dve", lambda: nc.vector.scalar_tensor_tensor(acc[:], u[:, w:w + 512], cwb[:, cc, w:w + 1], acc[:], ALU.mult, ALU.add),
                         reads=[u, cwb, acc], writes=[acc])
                if cc < 16:
                    p.op("act", lambda: nc.scalar.activation(out=xc[:], in_=acc[:], func=AF.Silu), reads=[acc], writes=[xc])
                    bank = psr.next()
                    for k in range(4):
                        p.op("pe", lambda: nc.tensor.transpose(bank[:, k * 128:(k + 1) * 128], xc[:, k * 128:(k + 1) * 128], IDF),
                             reads=[xc, cst], writes=[bank], mark=(k == 3))
                    p.op("dve", lambda: nc.vector.tensor_copy(xs_tm[:, :, cc * 128:(cc + 1) * 128], bank[:].rearrange("p (a b) -> p a b", a=4)),
                         reads=[bank], writes=[xs_tm])
                elif cc < 24:
                    g = cc - 16
                    p.op("act", lambda: nc.scalar.activation(out=BT[:, g, :], in_=acc[:], func=AF.Silu), reads=[acc], writes=[BT])
                    bank = psr.next()
                    pb = bank[:].bitcast(BF16)
                    for k in range(4):
                        p.op("pe", lambda: nc.tensor.transpose(pb[:, k * 128:(k + 1) * 128], BT[:, g, k * 128:(k + 1) * 128], idb[:]),
                             reads=[BT, idb], writes=[bank], mark=(k == 3))
                    p.op("dve", lambda: nc.vector.tensor_copy(B_tm[:, :, g * 128:(g + 1) * 128], pb[:, 0:512].rearrange("p (a b) -> p a b", a=4)),
                         reads=[bank], writes=[B_tm])
                else:
                    g = cc - 24
                    p.op("act", lambda: nc.scalar.activation(out=CT[:, g, :], in_=acc[:], func=AF.Silu), reads=[acc], writes=[CT])

        def chunk(ci, k, own):
            precast_step(p, c)
            dr = dtr.next(); s = sm.next(); E = Ex.next()
            p.dma("sp", dr[:], c.DT_s[ci * 128:(ci + 1) * 128, :], writes=[dr])
            p.op("dve", lambda: nc.vector.tensor_tensor(s[:, 0, :], dr[:], DTB, ALU.add), reads=[dr, v32], writes=[s])
            p.op("act", lambda: nc.scalar.activation(out=s[:, 1, :], in_=s[:, 0, :], func=AF.Exp), reads=[s], writes=[s])
            p.op("act", lambda: nc.scalar.activation(out=s[:, 2, :], in_=s[:, 1, :], func=AF.Ln, bias=1.0), reads=[s], writes=[s])
            p.op("dve", lambda: nc.vector.tensor_tensor(s[:, 3, :], s[:, 2, :], A_bc[:], ALU.mult), reads=[s, A_bc], writes=[s])
            bk = psr.next()
            p.op("pe", lambda: nc.tensor.matmul(bk[:, 0:NH], TRI, s[:, 3, :], start=True, stop=True), reads=[cst, s], writes=[bk], mark=False)
            p.op("pe", lambda: nc.tensor.matmul(bk[:, NH:2 * NH], ONES, s[:, 3, :], start=True, stop=True), reads=[cst, s], writes=[bk])
            p.op("act", lambda: nc.scalar.copy(s[:, 4:6, :].rearrange("p a b -> p (a b)"), bk[:, 0:2 * NH]), reads=[bk], writes=[s])
            p.op("dve", lambda: nc.vector.tensor_tensor(s[:, 6, :], s[:, 5, :], s[:, 4, :], ALU.subtract), reads=[s], writes=[s])
            p.op("act", lambda: nc.scalar.activation(out=E[:].rearrange("p a b -> p (a b)"), in_=s[:, 4:7, :].rearrange("p a b -> p (a b)"), func=AF.Exp),
                 reads=[s], writes=[E])
            p.op("dve", lambda: nc.vector.tensor_tensor(s[:, 7, :], s[:, 2, :], E[:, 2, :], ALU.mult), reads=[s, E], writes=[s])
            xs3 = xs_tm[:, k, :].rearrange("p (h q) -> p h q", h=NH)
            p.op("dve", lambda: nc.vector.tensor_tensor(xw[:].rearrange("p (h q) -> p h q", h=NH), xs3, bc(s[:, 7, :], HP), ALU.mult),
                 reads=[xs_tm, s], writes=[xw])
            if own:
                p.op("dve", lambda: nc.vector.tensor_tensor(x_dt[:].rearrange("p (h q) -> p h q", h=NH), xs3, bc(s[:, 2, :], HP), ALU.mult),
                     reads=[xs_tm, s], writes=[x_dt])
                p.op("dve", lambda: nc.vector.tensor_tensor(L_all[:], SU.unsqueeze(1).broadcast_to([128, NH, 128]), bc(s[:, 3, :], 128), ALU.mult),
                     reads=[cst, s], writes=[L_all])
            for g in range(NG):
                hs = slice(g * 4, g * 4 + 4)
                cs = slice(g * 256, (g + 1) * 256)
                if own:
                    bseg = psr.next()
                    for hh in range(4):
                        p.op("pe", lambda: nc.tensor.matmul(bseg[:, hh * 128:(hh + 1) * 128], L_all[:, g * 4 + hh, :], TRI, start=True, stop=False),
                             reads=[L_all, cst], writes=[bseg], mark=False)
                        p.op("pe", lambda: nc.tensor.matmul(bseg[:, hh * 128:(hh + 1) * 128], IDF, NEGM[:, 0:128], start=False, stop=True),
                             reads=[cst], writes=[bseg], mark=(hh == 3))
                    dec = decs.next(); MT = MTs.next()
                    p.op("act", lambda: nc.scalar.activation(out=dec[:], in_=bseg[:], func=AF.Exp), reads=[bseg], writes=[dec])
                    bcb = psr.next()
                    p.op("pe", lambda: nc.tensor.matmul(bcb[:, 0:128], BT[:, g, k * 128:(k + 1) * 128], CT[:, g, k * 128:(k + 1) * 128], start=True, stop=True),
                         reads=[BT, CT], writes=[bcb])
                    p.op("dve", lambda: nc.vector.tensor_tensor(MT[:].rearrange("p (h l) -> p h l", h=4), dec[:].rearrange("p (h l) -> p h l", h=4),
                                                                bcb[:, 0:128].unsqueeze(1).broadcast_to([128, 4, 128]), ALU.mult),
                         reads=[dec, bcb], writes=[MT])
                    by = psr.next()
                    for hh in range(4):
                        h = g * 4 + hh
                        p.op("pe", lambda: nc.tensor.matmul(by[:, hh * 64:(hh + 1) * 64], MT[:, hh * 128:(hh + 1) * 128], x_dt[:, h * 64:(h + 1) * 64], start=True, stop=True),
                             reads=[MT, x_dt], writes=[by], mark=False)
                    p.op("pe", lambda: nc.tensor.matmul(by[:, 256:512], CT[:, g, k * 128:(k + 1) * 128], S_bf[:, cs], start=True, stop=True),
                         reads=[CT, S_bf], writes=[by])
                    p.op("dve", lambda: nc.vector.tensor_tensor(tmp[:, cs].rearrange("p (h q) -> p h q", h=4), by[:, 256:512].rearrange("p (h q) -> p h q", h=4),
                                                                bc(E[:, 0, hs], HP), ALU.mult), reads=[by, E], writes=[tmp])
                    p.op("dve", lambda: nc.vector.tensor_tensor(y[:, cs], tmp[:, cs], by[:, 0:256], ALU.add), reads=[tmp, by], writes=[y])
                bs_ = psr.next()
                p.op("pe", lambda: nc.tensor.matmul(bs_[:, 0:256], B_tm[:, k, g * 128:(g + 1) * 128], xw[:, cs], start=True, stop=True),
                     reads=[B_tm, xw], writes=[bs_])
                p.op("dve", lambda: nc.vector.tensor_tensor(S[:, cs].rearrange("p (h q) -> p h q", h=4), S[:, cs].rearrange("p (h q) -> p h q", h=4),
                                                            bc(E[:, 1, hs], HP), ALU.mult), reads=[S, E], writes=[S])
                p.op("dve", lambda: nc.vector.tensor_tensor(S[:, cs], S[:, cs], bs_[:, 0:256], ALU.add), reads=[S, bs_], writes=[S])
                p.op("act", lambda: nc.scalar.copy(S_bf[:, cs], S[:, cs]), reads=[S], writes=[S_bf])
            if not own:
                return
            tt = ci - 16
            p.op("dve", lambda: nc.vector.tensor_tensor(tmp[:].rearrange("p (h q) -> p h q", h=NH), xs3, bc(D_bc, HP), ALU.mult),
                 reads=[xs_tm, v32], writes=[tmp])
            p.op("dve", lambda: nc.vector.tensor_tensor(y[:], y[:], tmp[:], ALU.add), reads=[y, tmp], writes=[y])
            zt = zts.next(); s4 = st4.next()
            p.dma("sp", zt[:], c.Z_s[tt * 128:(tt + 1) * 128, :], writes=[zt])
            p.op("act", lambda: nc.scalar.activation(out=zt[:], in_=zt[:], func=AF.Silu), reads=[zt], writes=[zt])
            p.op("dve", lambda: nc.vector.tensor_tensor(y[:], y[:], zt[:], ALU.mult), reads=[y, zt], writes=[y])
            p.op("act", lambda: nc.scalar.activation(out=junk[:], in_=y[:], func=AF.Square, accum_out=s4[:, 0:1]), reads=[y], writes=[junk, s4])
            p.op("dve", lambda: nc.vector.tensor_scalar(s4[:, 1:2], s4[:, 0:1], 1.0 / DI, EPS, ALU.mult, ALU.add), reads=[s4], writes=[s4])
            p.op("act", lambda: nc.scalar.activation(out=s4[:, 2:3], in_=s4[:, 1:2], func=AF.Ln), reads=[s4], writes=[s4])
            p.op("act", lambda: nc.scalar.activation(out=s4[:, 3:4], in_=s4[:, 2:3], func=AF.Exp, scale=-0.5), reads=[s4], writes=[s4])
            p.op("dve", lambda: nc.vector.scalar_tensor_tensor(yn[:], y[:], s4[:, 3:4], nws[:], ALU.mult, ALU.mult), reads=[y, s4, nws], writes=[yn])
            yT = ynT.next()
            for half in range(2):
                bank = psr.next()
                pb = bank[:].bitcast(BF16)
                for kk in range(8):
                    dc = half * 8 + kk
                    p.op("pe", lambda: nc.tensor.transpose(pb[:, kk * 128:(kk + 1) * 128], yn[:, dc * 128:(dc + 1) * 128], idb[:]),
                         reads=[yn, idb], writes=[bank], mark=(kk == 7))
                if half == 0:
                    p.op("act", lambda: nc.scalar.copy(yT[:, 0:8, :], pb.rearrange("p (a b) -> p a b", a=8)), reads=[bank], writes=[yT])
                else:
                    p.op("dve", lambda: nc.vector.tensor_copy(yT[:, 8:16, :], pb.rearrange("p (a b) -> p a b", a=8)), reads=[bank], writes=[yT])
            p.dma("sp", c.YN_s[:, :, tt * 128:(tt + 1) * 128].rearrange("a p t -> p a t"), yT[:], reads=[yT])

        for sti in range(8):
            own = sti >= 4
            conv_supertile(sti * 512, own)
            for k in range(4):
                chunk(sti * 4 + k, k, own)
            if sti == 3:
                p.op("dve", lambda: nc.vector.tensor_scalar(S[:], S[:], flag[:, 0:1], None, ALU.mult), reads=[S, flag], writes=[S])
                p.op("act", lambda: nc.scalar.copy(S_bf[:], S[:]), reads=[S], writes=[S_bf])
        p.barrier()


def host_common_b(inp, m):
    l = np.arange(128)
    tri = (l[:, None] <= l[None, :]).astype(np.float32)
    su = (l[:, None] > l[None, :]).astype(np.float32)
    ones = np.ones((128, 128), np.float32)
    idf = np.eye(128, dtype=np.float32)
    negm = np.where(l[None, :] < l[:, None], NEG, 0.0).astype(np.float32)
    m["cstB"] = np.ascontiguousarray(np.concatenate([tri, su, ones, idf, np.tile(negm, (1, 4))], axis=1))
    cw = np.asarray(inp["conv_w"])[0]
    cb = np.asarray(inp["conv_b"])[0]
    wb = np.concatenate([cw, cb[None, :]], axis=0)
    m["conv_wb"] = np.ascontiguousarray(wb.reshape(5, 32, 128).transpose(2, 1, 0)).reshape(128, 160)
    m["vec32"] = np.ascontiguousarray(np.stack([np.asarray(inp["dt_bias"])[0], np.asarray(inp["a_log"])[0], np.asarray(inp["d_skip"])[0]]))
    m["ssd_norm_w"] = np.ascontiguousarray(np.asarray(inp["ssd_norm_w"]).reshape(1, DI))
    return m


def phase_c(p, c, ps):
    nc = p.nc
    with ExitStack() as es:
        T = lambda name, shape, dt=F32: p.tile(es, name, shape, dt)
        idb = T("idbC", [128, 128], BF16)
        flag = T("flagC", [128, 2])
        bmid = T("bmid", [128, 4, 256])
        bfirst = T("bfirst", [128, 4, 256])
        qts = Rot([T(f"qt{i}", [128, 4, 128], BF16) for i in range(3)])
        kts = Rot([T(f"kt{i}", [128, 4, 256], BF16) for i in range(3)])
        vts = Rot([T(f"vt{i}", [128, 2, 512], BF16) for i in range(3)])
        scs = Rot([T(f"sc{i}", [128, 4, 256]) for i in range(2)])
        Ps = Rot([T(f"P{i}", [128, 4, 256], BF16) for i in range(2)])
        PTs = Rot([T(f"PT{i}", [128, 8, 128], BF16) for i in range(2)])
        mss = Rot([T(f"ms{i}", [128, 2, 4]) for i in range(2)])
        stg = Rot([T(f"stg{i}", [128, 4, 130]) for i in range(2)])
        psr = Rot(ps)
        p.dma("pool", idb[:], c.ident, writes=[idb])
        p.dma("sp", flag[:], c.flag, writes=[flag])

        for g in range(3):
            d = DIL[g]
            span = 128 * d
            nprevb = NPREV // span
            ncls = 4096 // span
            nown = NTOK // span
            p.dma("sp", bmid[:].rearrange("p a b -> p (a b)"), c.attn_bias[g], writes=[bmid])
            p.op("dve", lambda: nc.vector.tensor_copy(bfirst[:], bmid[:]), reads=[bmid], writes=[bfirst])
            p.op("dve", lambda: nc.vector.tensor_scalar(bfirst[:, :, 0:128], bfirst[:, :, 0:128], flag[:, 1:2], None, ALU.add),
                 reads=[bfirst, flag], writes=[bfirst])
            og = c.O_s[g].rearrange("(j r) c -> r j c", r=d)
            for r in range(d):
                for n in range(nown):
                    qt = qts.next(); kt = kts.next(); vt = vts.next(); sc = scs.next(); P = Ps.next(); PT = PTs.next()
                    ms = mss.next(); sg = stg.next()
                    pos0 = r * (NTOK // d) + n * 128
                    kpos = r * (4096 // d) + (nprevb + n - 1) * 128
                    p.dma("sp", qt[:], c.QT_s[4 * g:4 * g + 4, :, pos0:pos0 + 128].rearrange("h e q -> e h q"), writes=[qt])
                    p.dma("sp", kt[:], c.KT_s[4 * g:4 * g + 4, :, kpos:kpos + 256].rearrange("h e k -> e h k"), writes=[kt])
                    vb = r * ncls + nprevb + n - 1
                    p.dma("sp", vt[:], c.V_s[g, vb:vb + 2].rearrange("b k e -> k b e"), writes=[vt])
                    bias = bfirst if n == 0 else bmid
                    banks = [psr.next(), psr.next()]
                    for hh in range(4):
                        bk = banks[hh // 2]
                        p.op("pe", lambda: nc.tensor.matmul(bk[:, (hh % 2) * 256:(hh % 2 + 1) * 256], qt[:, hh, :], kt[:, hh, :], start=True, stop=True),
                             reads=[qt, kt], writes=[bk], mark=(hh % 2 == 1))
                    for i in range(2):
                        p.op("dve", lambda: nc.vector.tensor_tensor(sc[:, 2 * i:2 * i + 2, :], banks[i][:].rearrange("p (a b) -> p a b", a=2),
                                                                    bias[:, 2 * i:2 * i + 2, :], ALU.add), reads=[banks[i], bias], writes=[sc])
                    p.op("dve", lambda: nc.vector.reduce_max(ms[:, 0, :], sc[:], axis=AX.X), reads=[sc], writes=[ms])
                    p.op("dve", lambda: nc.vector.tensor_tensor(sc[:], sc[:], ms[:, 0, :].unsqueeze(2).broadcast_to([128, 4, 256]), ALU.subtract),
                         reads=[sc, ms], writes=[sc])
                    p.op("act", lambda: nc.scalar.activation(out=P[:], in_=sc[:], func=AF.Exp), reads=[sc], writes=[P])
                    p.op("dve", lambda: nc.vector.reduce_sum(ms[:, 1, :], P[:], axis=AX.X), reads=[P], writes=[ms])
                    bt = psr.next()
                    pb = bt[:].bitcast(BF16)
                    for hh in range(4):
                        for kb in range(2):
                            i = hh * 2 + kb
                            p.op("pe", lambda: nc.tensor.transpose(pb[:, i * 128:(i + 1) * 128], P[:, hh, kb * 128:(kb + 1) * 128], idb[:]),
                                 reads=[P, idb], writes=[bt], mark=(i == 7))
                    p.op("act", lambda: nc.scalar.copy(PT[:], pb.rearrange("p (a b) -> p a b", a=8)), reads=[bt], writes=[PT])
                    bu = psr.next()
                    for hh in range(4):
                        for kb in range(2):
                            p.op("pe", lambda: nc.tensor.matmul(bu[:, hh * 128:(hh + 1) * 128], PT[:, hh * 2 + kb, :], vt[:, kb, hh * 128:(hh + 1) * 128],
                                                                start=(kb == 0), stop=(kb == 1)), reads=[PT, vt], writes=[bu], mark=(hh == 3 and kb == 1))
                    p.op("act", lambda: nc.scalar.copy(sg[:, :, 0:128], bu[:].rearrange("p (a b) -> p a b", a=4)), reads=[bu], writes=[sg])
                    p.op("dve", lambda: nc.vector.tensor_copy(sg[:, :, 128:130], ms[:].rearrange("p a b -> p b a")), reads=[ms], writes=[sg])
                    p.dma("sp", og[r, n * 128:(n + 1) * 128, :], sg[:].rearrange("p a b -> p (a b)"), reads=[sg])
        p.barrier()
        lds = Rot([T(f"ld{i}", [128, 3, 4, 130]) for i in range(2)])
        wk = Rot([T(f"wk{i}", [128, 8, 12]) for i in range(2)])
        ob = Rot([T(f"ob{i}", [128, 4, 128]) for i in range(2)])
        o16 = Rot([T(f"o16_{i}", [128, 4, 128], BF16) for i in range(2)])
        oT = Rot([T(f"oT{i}", [128, 4, 128], BF16) for i in range(2)])
        for tt in range(16):
            ld = lds.next(); w = wk.next(); o = ob.next(); ob16 = o16.next(); ot = oT.next()
            p.dma("sp", ld[:].rearrange("p g h c -> p g (h c)"), c.O_s[:, tt * 128:(tt + 1) * 128, :].rearrange("g t c -> t g c"), writes=[ld])
            mg = ld[:, :, :, 128]
            sgm = ld[:, :, :, 129]
            M = w[:, 0, 0:4]
            p.op("dve", lambda: nc.vector.tensor_tensor(M, mg[:, 0, :], mg[:, 1, :], ALU.max), reads=[ld], writes=[w])
            p.op("dve", lambda: nc.vector.tensor_tensor(M, M, mg[:, 2, :], ALU.max), reads=[ld, w], writes=[w])
            wg3 = w[:, 1, :].rearrange("p (g h) -> p g h", g=3)
            p.op("dve", lambda: nc.vector.tensor_tensor(wg3, mg, M.unsqueeze(1).broadcast_to([128, 3, 4]), ALU.subtract), reads=[ld, w], writes=[w])
            p.op("act", lambda: nc.scalar.activation(out=w[:, 2, :], in_=w[:, 1, :], func=AF.Exp), reads=[w], writes=[w])
            e3 = w[:, 2, :].rearrange("p (g h) -> p g h", g=3)
            ws3 = w[:, 3, :].rearrange("p (g h) -> p g h", g=3)
            p.op("dve", lambda: nc.vector.tensor_tensor(ws3, e3, sgm, ALU.mult), reads=[ld, w], writes=[w])
            den = w[:, 4, 0:4]
            p.op("dve", lambda: nc.vector.tensor_tensor(den, ws3[:, 0, :], ws3[:, 1, :], ALU.add), reads=[w], writes=[w])
            p.op("dve", lambda: nc.vector.tensor_tensor(den, den, ws3[:, 2, :], ALU.add), reads=[w], writes=[w])
            p.op("dve", lambda: nc.vector.reciprocal(w[:, 5, 0:4], den), reads=[w], writes=[w])
            wn3 = w[:, 6, :].rearrange("p (g h) -> p g h", g=3)
            p.op("dve", lambda: nc.vector.tensor_tensor(wn3, e3, w[:, 5, 0:4].unsqueeze(1).broadcast_to([128, 3, 4]), ALU.mult), reads=[w], writes=[w])
            bcw = lambda gi: wn3[:, gi, :].unsqueeze(2).broadcast_to([128, 4, 128])
            p.op("dve", lambda: nc.vector.tensor_tensor(o[:], ld[:, 0, :, 0:128], bcw(0), ALU.mult), reads=[ld, w], writes=[o])
            for gi in (1, 2):
                p.op("dve", lambda: nc.vector.tensor_tensor(ld[:, gi, :, 0:128], ld[:, gi, :, 0:128], bcw(gi), ALU.mult), reads=[ld, w], writes=[ld])
                dst = o[:] if gi == 1 else ob16[:]
                p.op("dve", lambda: nc.vector.tensor_tensor(dst, o[:], ld[:, gi, :, 0:128], ALU.add), reads=[ld, o], writes=[o, ob16])
            bt = psr.next()
            pb = bt[:].bitcast(BF16)
            for hh in range(4):
                p.op("pe", lambda: nc.tensor.transpose(pb[:, hh * 128:(hh + 1) * 128], ob16[:, hh, :], idb[:]), reads=[ob16, idb], writes=[bt], mark=(hh == 3))
            p.op("act", lambda: nc.scalar.copy(ot[:], pb[:, 0:512].rearrange("p (a b) -> p a b", a=4)), reads=[bt], writes=[ot])
            p.dma("sp", c.OT_s[:, :, tt * 128:(tt + 1) * 128].rearrange("h e t -> e h t"), ot[:], reads=[ot])
        p.barrier()


def host_common_c(inp, m):
    slopes = (2.0 ** (-8.0 * np.arange(1, AH + 1) / AH)).astype(np.float32)
    q = np.arange(128)[:, None]
    k = np.arange(256)[None, :]
    rel = (q + 128) - k
    valid = (rel >= 0) & (rel <= 128)
    ab = np.zeros((3, 128, 4, 256), np.float32)
    for g in range(3):
        for hh in range(4):
            bias = -slopes[4 * g + hh] * (rel * DIL[g]).astype(np.float32)
            ab[g, :, hh, :] = np.where(valid, bias, NEG)
    m["attn_bias"] = np.ascontiguousarray(ab.reshape(3, 128, 1024))
    return m


def phase_d(p, c, ps):
    nc = p.nc
    with ExitStack() as es:
        T = lambda name, shape, dt=F32: p.tile(es, name, shape, dt)
        ynT = T("ynT_d", [128, NDC, NTOK], BF16)
        oT = T("oT_d", [128, 4, NTOK], BF16)
        wss = Rot([T(f"wss{i}", [128, NDC, 128], BF16) for i in range(2)])
        was = Rot([T(f"was{i}", [128, 4, 128], BF16) for i in range(2)])
        g0s = Rot([T(f"g0_{i}", [128, 512]) for i in range(2)])
        g1s = Rot([T(f"g1_{i}", [128, 512]) for i in range(2)])
        t1s = Rot([T(f"t1_{i}", [128, 512]) for i in range(2)])
        mgs = Rot([T(f"mg{i}", [128, 512], BF16) for i in range(2)])
        psr = Rot(ps)
        for kc in range(NDC):
            p.dma("sp", ynT[:, kc, :], c.YN_s[kc], writes=[ynT])
        for kc in range(4):
            p.dma("sp", oT[:, kc, :], c.OT_s[kc], writes=[oT])
        for dmc in range(16):
            ws = wss.next(); wa = was.next()
            p.dma("pool", ws[:].rearrange("p a b -> p (a b)"), c.w_ssd_fm[dmc], writes=[ws])
            p.dma("pool", wa[:].rearrange("p a b -> p (a b)"), c.w_attn_fm[dmc], writes=[wa])
            for st in range(4):
                ts = slice(st * 512, (st + 1) * 512)
                g0 = g0s.next(); g1 = g1s.next(); t1 = t1s.next(); mg = mgs.next()
                p.dma("sp", g0[:], c.G_s[dmc * 128:(dmc + 1) * 128, ts], writes=[g0])
                p.dma("sp", g1[:], c.G_s[D + dmc * 128:D + (dmc + 1) * 128, ts], writes=[g1])
                b1 = psr.next()
                for kc in range(NDC):
                    p.op("pe", lambda: nc.tensor.matmul(b1[:], ws[:, kc, :], ynT[:, kc, ts], start=(kc == 0), stop=(kc == NDC - 1)),
                         reads=[ws, ynT], writes=[b1], mark=(kc == NDC - 1))
                b2 = psr.next()
                for kc in range(4):
                    p.op("pe", lambda: nc.tensor.matmul(b2[:], wa[:, kc, :], oT[:, kc, ts], start=(kc == 0), stop=(kc == 3)),
                         reads=[wa, oT], writes=[b2], mark=(kc == 3))
                p.op("dve", lambda: nc.vector.tensor_tensor(t1[:], g0[:], b1[:], ALU.mult), reads=[g0, b1], writes=[t1])
                p.op("dve", lambda: nc.vector.tensor_tensor(g1[:], g1[:], b2[:], ALU.mult), reads=[g1, b2], writes=[g1])
                p.op("dve", lambda: nc.vector.tensor_tensor(mg[:], t1[:], g1[:], ALU.add), reads=[t1, g1], writes=[mg])
                p.dma("sp", c.M_s[dmc][:, ts], mg[:], reads=[mg])
        p.barrier()
    with ExitStack() as es:
        T = lambda name, shape, dt=F32: p.tile(es, name, shape, dt)
        mT = T("mT_d", [128, NDC, NTOK], BF16)
        wos = Rot([T(f"wo{i}", [128, NDC, 512], BF16) for i in range(2)])
        xss = Rot([T(f"xs_d{i}", [128, 512]) for i in range(3)])
        psr = Rot(ps)
        for kc in range(NDC):
            p.dma("sp", mT[:, kc, :], c.M_s[kc], writes=[mT])
        for ct in range(4):
            wo = wos.next()
            p.dma("pool", wo[:].rearrange("p a b -> p (a b)"), c.w_out_tm[ct], writes=[wo])
            for tt in range(16):
                xs_ = xss.next()
                p.dma("sp", xs_[:], c.x_own[tt * 128:(tt + 1) * 128, ct * 512:(ct + 1) * 512], writes=[xs_])
                bk = psr.next()
                for kc in range(NDC):
                    p.op("pe", lambda: nc.tensor.matmul(bk[:], mT[:, kc, tt * 128:(tt + 1) * 128], wo[:, kc, :], start=(kc == 0), stop=(kc == NDC - 1)),
                         reads=[mT, wo], writes=[bk], mark=(kc == NDC - 1))
                p.op("dve", lambda: nc.vector.tensor_tensor(xs_[:], xs_[:], bk[:], ALU.add), reads=[xs_, bk], writes=[xs_])
                p.dma("sp", c.X2_s[tt * 128:(tt + 1) * 128, ct * 512:(ct + 1) * 512], xs_[:], reads=[xs_])
        p.barrier()


def phase_e0(p, c, ps):
    nc = p.nc
    with ExitStack() as es:
        T = lambda name, shape, dt=F32: p.tile(es, name, shape, dt)
        idf = T("idf_e", [128, 128])
        fw = T("fw_e", [128, D])
        wr = T("wr_e", [128, NDC, 36])
        br = T("br_e", [128, 36])
        x2s = Rot([T(f"x2_{i}", [128, D]) for i in range(2)])
        hns = Rot([T(f"hn_{i}", [128, D]) for i in range(2)])
        junk = T("junk_e", [128, D], BF16)
        st4 = Rot([T(f"st4e_{i}", [128, 4]) for i in range(2)])
        h32 = Rot([T(f"h32_{i}", [128, NDC, 128]) for i in range(2)])
        h16 = Rot([T(f"h16_{i}", [128, NDC, 128], BF16) for i in range(2)])
        rt = Rot([T(f"rt{i}", [128, 8, 36]) for i in range(2)])
        sc = Rot([T(f"rs{i}", [128, 16]) for i in range(2)])
        gt = Rot([T(f"gt{i}", [128, NE]) for i in range(2)])
        psr = Rot(ps)
        p.dma("sp", idf[:], c.ident, writes=[idf])
        p.dma("sp", fw[:], c.ffn_norm_w.partition_broadcast(128), writes=[fw])
        p.dma("sp", wr[:].rearrange("p a b -> p (a b)"), c.w_router, writes=[wr])
        p.dma("sp", br[:], c.b_router.partition_broadcast(128), writes=[br])
        for tt in range(16):
            x2 = x2s.next(); hn = hns.next(); s4 = st4.next(); a32 = h32.next(); a16 = h16.next()
            R = rt.next(); S = sc.next(); G = gt.next()
            p.dma("sp", x2[:], c.X2_s[tt * 128:(tt + 1) * 128, :], writes=[x2])
            p.op("act", lambda: nc.scalar.activation(out=junk[:], in_=x2[:], func=AF.Square, accum_out=s4[:, 0:1]), reads=[x2], writes=[junk, s4])
            p.op("dve", lambda: nc.vector.tensor_scalar(s4[:, 1:2], s4[:, 0:1], 1.0 / D, EPS, ALU.mult, ALU.add), reads=[s4], writes=[s4])
            p.op("act", lambda: nc.scalar.activation(out=s4[:, 2:3], in_=s4[:, 1:2], func=AF.Ln), reads=[s4], writes=[s4])
            p.op("act", lambda: nc.scalar.activation(out=s4[:, 3:4], in_=s4[:, 2:3], func=AF.Exp, scale=-0.5), reads=[s4], writes=[s4])
            p.op("dve", lambda: nc.vector.scalar_tensor_tensor(hn[:], x2[:], s4[:, 3:4], fw[:], ALU.mult, ALU.mult), reads=[x2, s4, fw], writes=[hn])
            for q4 in range(4):
                bk = psr.next()
                for k in range(4):
                    dc = q4 * 4 + k
                    p.op("pe", lambda: nc.tensor.transpose(bk[:, k * 128:(k + 1) * 128], hn[:, dc * 128:(dc + 1) * 128], idf[:]),
                         reads=[hn, idf], writes=[bk], mark=(k == 3))
                p.op("act", lambda: nc.scalar.copy(a32[:, q4 * 4:q4 * 4 + 4, :], bk[:].rearrange("p (a b) -> p a b", a=4)), reads=[bk], writes=[a32])
                p.op("dve", lambda: nc.vector.tensor_copy(a16[:, q4 * 4:q4 * 4 + 4, :], bk[:].rearrange("p (a b) -> p a b", a=4)), reads=[bk], writes=[a16])
            p.dma("sp", c.HN_s[:, :, tt * 128:(tt + 1) * 128].rearrange("a p t -> p a t"), a16[:], reads=[a16])
            bk = psr.next()
            for kc in range(NDC):
                p.op("pe", lambda: nc.tensor.matmul(bk[:, 0:36], a32[:, kc, :], wr[:, kc, :], start=(kc == 0), stop=(kc == NDC - 1)),
                     reads=[a32, wr], writes=[bk], mark=(kc == NDC - 1))
            L = R[:, 0, :]
            dv = lambda fn, reads, writes: p.op("dve", fn, reads=reads, writes=writes)
            dv(lambda: nc.vector.tensor_tensor(L, bk[:, 0:36], br[:], ALU.add), [bk, br], [R])
            gl = R[:, 0, 0:4]
            el = R[:, 0, 4:36]
            dv(lambda: nc.vector.reduce_max(S[:, 0:1], gl, axis=AX.X), [R], [S])
            dv(lambda: nc.vector.tensor_scalar(R[:, 1, 0:4], gl, S[:, 0:1], None, ALU.subtract), [R, S], [R])
            p.op("act", lambda: nc.scalar.activation(out=R[:, 1, 4:8], in_=R[:, 1, 0:4], func=AF.Exp, accum_out=S[:, 1:2]), reads=[R], writes=[R, S])
            dv(lambda: nc.vector.reciprocal(S[:, 2:3], S[:, 1:2]), [S], [S])
            dv(lambda: nc.vector.tensor_scalar(R[:, 1, 8:12], gl, S[:, 0:1], None, ALU.is_equal), [R, S], [R])
            dv(lambda: nc.vector.tensor_scalar(R[:, 1, 12:16], R[:, 1, 8:12], -NEG, NEG, ALU.mult, ALU.add), [R], [R])
            elm = R[:, 2, 0:32]
            dv(lambda: nc.vector.tensor_tensor(elm.rearrange("p (g e) -> p g e", g=4), el.rearrange("p (g e) -> p g e", g=4),
                                               R[:, 1, 12:16].unsqueeze(2).broadcast_to([128, 4, 8]), ALU.add), [R], [R])
            dv(lambda: nc.vector.reduce_max(S[:, 3:4], elm, axis=AX.X), [R], [S])
            oh1 = R[:, 3, 0:32]
            dv(lambda: nc.vector.tensor_scalar(oh1, elm, S[:, 3:4], None, ALU.is_equal), [R, S], [R])
            elm2 = R[:, 4, 0:32]
            dv(lambda: nc.vector.scalar_tensor_tensor(elm2, oh1, NEG, elm, ALU.mult, ALU.add), [R], [R])
            dv(lambda: nc.vector.reduce_max(S[:, 4:5], elm2, axis=AX.X), [R], [S])
            oh2 = R[:, 5, 0:32]
            dv(lambda: nc.vector.tensor_scalar(oh2, elm2, S[:, 4:5], None, ALU.is_equal), [R, S], [R])
            dv(lambda: nc.vector.tensor_tensor(S[:, 5:6], S[:, 4:5], S[:, 3:4], ALU.subtract), [S], [S])
            p.op("act", lambda: nc.scalar.activation(out=S[:, 6:7], in_=S[:, 5:6], func=AF.Exp), reads=[S], writes=[S])
            dv(lambda: nc.vector.tensor_scalar(S[:, 7:8], S[:, 6:7], 1.0, None, ALU.add), [S], [S])
            dv(lambda: nc.vector.reciprocal(S[:, 8:9], S[:, 7:8]), [S], [S])
            dv(lambda: nc.vector.tensor_tensor(S[:, 9:10], S[:, 8:9], S[:, 2:3], ALU.mult), [S], [S])
            dv(lambda: nc.vector.tensor_tensor(S[:, 10:11], S[:, 9:10], S[:, 6:7], ALU.mult), [S], [S])
            dv(lambda: nc.vector.tensor_scalar(G[:], oh1, S[:, 9:10], None, ALU.mult), [R, S], [G])
            dv(lambda: nc.vector.scalar_tensor_tensor(G[:], oh2, S[:, 10:11], G[:], ALU.mult, ALU.add), [R, S, G], [G])
            p.dma("sp", c.GATE_s[tt * 128:(tt + 1) * 128, :], G[:], reads=[G])
        p.barrier()


def phase_e(p, c, ps):
    nc = p.nc
    NP = 1024
    with ExitStack() as es:
        T = lambda name, shape, dt=F32: p.tile(es, name, shape, dt)
        fnw = T("fnw", [128, D])
        gates = T("gates", [128, 16, NE])
        hnT = T("hnT_e", [128, NDC, NP], BF16)
        yacc = T("yacc", [128, NP // 128, D])
        wgs = Rot([T(f"wg{i}", [128, NDC, 256], BF16) for i in range(2)])
        wus = Rot([T(f"wu{i}", [128, NDC, 256], BF16) for i in range(2)])
        wds = Rot([T(f"wd{i}", [128, 2, D], BF16) for i in range(2)])
        sils = Rot([T(f"sil{i}", [128, 512]) for i in range(2)])
        hms = Rot([T(f"hm{i}", [128, 2, NP], BF16) for i in range(2)])
        junk = T("junk_m", [128, D], BF16)
        st4 = Rot([T(f"st4m_{i}", [128, 4]) for i in range(2)])
        psr = Rot(ps)
        p.dma("sp", fnw[:], c.final_norm_w.partition_broadcast(128), writes=[fnw])
        p.dma("sp", gates[:], c.GATE_s.rearrange("(t p) e -> p t e", p=128), writes=[gates])
        for ps_i in range(NTOK // NP):
            t0 = ps_i * NP
            for kc in range(NDC):
                p.dma("sp", hnT[:, kc, :], c.HN_s[kc][:, t0:t0 + NP], writes=[hnT])
            for t in range(NP // 128):
                p.dma("sp", yacc[:, t, :], c.X2_s[t0 + t * 128:t0 + (t + 1) * 128, :], writes=[yacc])
            for e in range(NE):
                for fc in range(4):
                    wg = wgs.next(); wu = wus.next(); wd = wds.next(); hm = hms.next()
                    p.dma("pool", wg[:].rearrange("p a b -> p (a b)"), c.wg_t[e, fc], writes=[wg])
                    p.dma("pool", wu[:].rearrange("p a b -> p (a b)"), c.wu_t[e, fc], writes=[wu])
                    p.dma("pool", wd[:].rearrange("p a b -> p (a b)"), c.wd_t[e, fc], writes=[wd])
                    for j in range(2):
                        for ts in range(NP // 512):
                            tsl = slice(ts * 512, (ts + 1) * 512)
                            bg = psr.next(); bu = psr.next(); sil = sils.next()
                            for kc in range(NDC):
                                p.op("pe", lambda: nc.tensor.matmul(bg[:], wg[:, kc, j * 128:(j + 1) * 128], hnT[:, kc, tsl], start=(kc == 0), stop=(kc == NDC - 1)),
                                     reads=[wg, hnT], writes=[bg], mark=(kc == NDC - 1))
                            for kc in range(NDC):
                                p.op("pe", lambda: nc.tensor.matmul(bu[:], wu[:, kc, j * 128:(j + 1) * 128], hnT[:, kc, tsl], start=(kc == 0), stop=(kc == NDC - 1)),
                                     reads=[wu, hnT], writes=[bu], mark=(kc == NDC - 1))
                            p.op("act", lambda: nc.scalar.activation(out=sil[:], in_=bg[:], func=AF.Silu), reads=[bg], writes=[sil])
                            p.op("dve", lambda: nc.vector.tensor_tensor(hm[:, j, tsl], sil[:], bu[:], ALU.mult), reads=[sil, bu], writes=[hm])
                    for t in range(NP // 128):
                        gcol = gates[:, ps_i * (NP // 128) + t, e:e + 1]
                        for dtile in range(4):
                            dsl = slice(dtile * 512, (dtile + 1) * 512)
                            bk = psr.next()
                            for j in range(2):
                                p.op("pe", lambda: nc.tensor.matmul(bk[:], hm[:, j, t * 128:(t + 1) * 128], wd[:, j, dsl], start=(j == 0), stop=(j == 1)),
                                     reads=[hm, wd], writes=[bk], mark=(j == 1))
                            p.op("dve", lambda: nc.vector.scalar_tensor_tensor(yacc[:, t, dsl], bk[:], gcol, yacc[:, t, dsl], ALU.mult, ALU.add),
                                 reads=[bk, gates, yacc], writes=[yacc])
            for t in range(NP // 128):
                s4 = st4.next()
                p.op("act", lambda: nc.scalar.activation(out=junk[:], in_=yacc[:, t, :], func=AF.Square, accum_out=s4[:, 0:1]), reads=[yacc], writes=[junk, s4])
                p.op("dve", lambda: nc.vector.tensor_scalar(s4[:, 1:2], s4[:, 0:1], 1.0 / D, EPS, ALU.mult, ALU.add), reads=[s4], writes=[s4])
                p.op("act", lambda: nc.scalar.activation(out=s4[:, 2:3], in_=s4[:, 1:2], func=AF.Ln), reads=[s4], writes=[s4])
                p.op("act", lambda: nc.scalar.activation(out=s4[:, 3:4], in_=s4[:, 2:3], func=AF.Exp, scale=-0.5), reads=[s4], writes=[s4])
                p.op("dve", lambda: nc.vector.scalar_tensor_tensor(yacc[:, t, :], yacc[:, t, :], s4[:, 3:4], fnw[:], ALU.mult, ALU.mult),
                     reads=[yacc, s4, fnw], writes=[yacc])
                p.dma("sp", c.out[t0 + t * 128:t0 + (t + 1) * 128, :], yacc[:, t, :], reads=[yacc])
        p.barrier()


def host_common_de(inp, m):
    m["w_ssd_fm"] = tile_w(np.asarray(inp["w_ssd_out"])[0], 128)
    m["w_attn_fm"] = tile_w(np.asarray(inp["w_attn_out"])[0], 128)
    m["w_out_tm"] = tile_w(np.asarray(inp["w_out"])[0], 512)
    m["ffn_norm_w"] = np.ascontiguousarray(np.asarray(inp["ffn_norm_w"]).reshape(1, D))
    wr = np.concatenate([np.asarray(inp["w_group_router"])[0], np.asarray(inp["w_expert_router"])[0]], axis=1)
    m["w_router"] = tile_w(wr, 36)[0]
    m["b_router"] = np.ascontiguousarray(np.concatenate([np.asarray(inp["b_group_router"])[0], np.asarray(inp["b_expert_router"])[0]]).reshape(1, 36))
    m["final_norm_w"] = np.ascontiguousarray(np.asarray(inp["final_norm_w"]).reshape(1, D))
    return m


def build_program(debug=False, phases="abcdeE"):
    nc = bass.Bass("TRN2", target_bir_lowering=False)
    c = declare_io(nc, debug=debug, phases=phases)
    with ExitStack() as es:
        p = Prog(nc, es)
        ps = [Tile(f"ps{i}", es.enter_context(nc.psum_tensor(f"psum{i}", [128, 512], F32))) for i in range(8)]
        for b in ps:
            b.excl = True
        if "a" in phases:
            phase_a(p, c, ps)
        if "b" in phases:
            phase_b(p, c, ps)
        if "c" in phases:
            phase_c(p, c, ps)
        if "d" in phases:
            phase_d(p, c, ps)
        rt_ = route_tiles(p, es)
        if "e" in phases:
            phase_e0s(p, c, ps, rt_)
            phase_e1(p, c, ps, rt_)
        if "E" in phases:
            phase_es(p, c, ps, rt_)
            phase_ec(p, c, ps, rt_)
        p.barrier()
    return nc, p


def host_maps(inp):
    m = host_common(inp)
    host_common_b(inp, m)
    host_common_c(inp, m)
    host_common_de(inp, m)
    host_common_s(inp, m)
    x = np.asarray(inp["x"])
    maps = []
    zeros = np.zeros((NPREV, D), np.float32)
    for core in range(8):
        b, half = core // 2, core % 2
        mm = dict(m)
        mm["x_own"] = np.ascontiguousarray(x[b, half * NTOK:(half + 1) * NTOK])
        mm["x_prev"] = np.ascontiguousarray(x[b, 0:NPREV]) if half == 1 else zeros
        fl = np.array([[1.0, 0.0]], np.float32) if half == 1 else np.array([[0.0, NEG]], np.float32)
        mm["flag"] = np.ascontiguousarray(np.tile(fl, (128, 1)))
        maps.append(mm)
    return maps


_PROG = {}


def kernel(**inputs):
    if "nc" not in _PROG:
        _PROG["nc"], _ = build_program()
    nc = _PROG["nc"]
    maps = host_maps(inputs)
    res = run_bass_kernel_spmd(nc, maps, core_ids=list(range(8)))
    x = np.asarray(inputs["x"])
    out = np.empty(x.shape, np.float32)
    for core in range(8):
        b, half = core // 2, core % 2
        out[b, half * NTOK:(half + 1) * NTOK] = np.asarray(res.results[core]["out"], np.float32)
    return out


def route_tiles(p, es):
    r = Ctx()
    T = lambda name, shape, dt=F32: p.tile(es, name, shape, dt)
    r.OH1 = T("OH1", [128, 16, NE])
    r.OH2 = T("OH2", [128, 16, NE])
    r.PG = T("PG", [128, 16, 2])
    r.POSI = T("POSI", [128, 2, 16], I32)
    r.IDXI = T("IDXI", [128, NSLOT], I32)
    return r


def phase_e0s(p, c, ps, rt_):
    nc = p.nc
    with ExitStack() as es:
        T = lambda name, shape, dt=F32: p.tile(es, name, shape, dt)
        idf = T("idf_e", [128, 128])
        fw = T("fw_e", [128, D])
        wr = T("wr_e", [128, NDC, 36])
        br = T("br_e", [128, 36])
        zt = T("zt_e", [128, D], BF16)
        x2s = Rot([T(f"x2_{i}", [128, D]) for i in range(2)])
        hns = Rot([T(f"hn_{i}", [128, D]) for i in range(2)])
        hbs = Rot([T(f"hb_{i}", [128, D], BF16) for i in range(2)])
        junk = T("junk_e", [128, D], BF16)
        st4 = Rot([T(f"st4e_{i}", [128, 4]) for i in range(2)])
        h32 = Rot([T(f"h32_{i}", [128, NDC, 128]) for i in range(2)])
        rt = Rot([T(f"rt{i}", [128, 8, 36]) for i in range(2)])
        sc = Rot([T(f"rs{i}", [128, 16]) for i in range(2)])
        psr = Rot(ps)
        p.dma("sp", idf[:], c.ident, writes=[idf])
        p.dma("sp", fw[:], c.ffn_norm_w.partition_broadcast(128), writes=[fw])
        p.dma("sp", wr[:].rearrange("p a b -> p (a b)"), c.w_router, writes=[wr])
        p.dma("sp", br[:], c.b_router.partition_broadcast(128), writes=[br])
        p.op("dve", lambda: nc.vector.memset(zt[:], 0.0), writes=[zt])
        for i in range(NSLOT):
            p.dma("sp", c.XS_s[i * 128:(i + 1) * 128, :], zt[:], reads=[zt])
        for tt in range(16):
            x2 = x2s.next(); hn = hns.next(); hb = hbs.next(); s4 = st4.next(); a32 = h32.next()
            R = rt.next(); S = sc.next()
            p.dma("sp", x2[:], c.X2_s[tt * 128:(tt + 1) * 128, :], writes=[x2])
            p.op("act", lambda: nc.scalar.activation(out=junk[:], in_=x2[:], func=AF.Square, accum_out=s4[:, 0:1]), reads=[x2], writes=[junk, s4])
            p.op("dve", lambda: nc.vector.tensor_scalar(s4[:, 1:2], s4[:, 0:1], 1.0 / D, EPS, ALU.mult, ALU.add), reads=[s4], writes=[s4])
            p.op("act", lambda: nc.scalar.activation(out=s4[:, 2:3], in_=s4[:, 1:2], func=AF.Ln), reads=[s4], writes=[s4])
            p.op("act", lambda: nc.scalar.activation(out=s4[:, 3:4], in_=s4[:, 2:3], func=AF.Exp, scale=-0.5), reads=[s4], writes=[s4])
            p.op("dve", lambda: nc.vector.scalar_tensor_tensor(hn[:], x2[:], s4[:, 3:4], fw[:], ALU.mult, ALU.mult), reads=[x2, s4, fw], writes=[hn])
            p.op("act", lambda: nc.scalar.copy(hb[:], hn[:]), reads=[hn], writes=[hb])
            p.dma("sp", c.HNTM_s[tt * 128:(tt + 1) * 128, :], hb[:], reads=[hb])
            for q4 in range(4):
                bk = psr.next()
                for k in range(4):
                    dc = q4 * 4 + k
                    p.op("pe", lambda: nc.tensor.transpose(bk[:, k * 128:(k + 1) * 128], hn[:, dc * 128:(dc + 1) * 128], idf[:]),
                         reads=[hn, idf], writes=[bk], mark=(k == 3))
                if q4 % 2 == 0:
                    p.op("act", lambda: nc.scalar.copy(a32[:, q4 * 4:q4 * 4 + 4, :], bk[:].rearrange("p (a b) -> p a b", a=4)), reads=[bk], writes=[a32])
                else:
                    p.op("dve", lambda: nc.vector.tensor_copy(a32[:, q4 * 4:q4 * 4 + 4, :], bk[:].rearrange("p (a b) -> p a b", a=4)), reads=[bk], writes=[a32])
            bk = psr.next()
            for kc in range(NDC):
                p.op("pe", lambda: nc.tensor.matmul(bk[:, 0:36], a32[:, kc, :], wr[:, kc, :], start=(kc == 0), stop=(kc == NDC - 1)),
                     reads=[a32, wr], writes=[bk], mark=(kc == NDC - 1))
            L = R[:, 0, :]
            dv = lambda fn, reads, writes: p.op("dve", fn, reads=reads, writes=writes)
            dv(lambda: nc.vector.tensor_tensor(L, bk[:, 0:36], br[:], ALU.add), [bk, br], [R])
            gl = R[:, 0, 0:4]
            el = R[:, 0, 4:36]
            dv(lambda: nc.vector.reduce_max(S[:, 0:1], gl, axis=AX.X), [R], [S])
            dv(lambda: nc.vector.tensor_scalar(R[:, 1, 0:4], gl, S[:, 0:1], None, ALU.subtract), [R, S], [R])
            p.op("act", lambda: nc.scalar.activation(out=R[:, 1, 4:8], in_=R[:, 1, 0:4], func=AF.Exp, accum_out=S[:, 1:2]), reads=[R], writes=[R, S])
            dv(lambda: nc.vector.reciprocal(S[:, 2:3], S[:, 1:2]), [S], [S])
            dv(lambda: nc.vector.tensor_scalar(R[:, 1, 8:12], gl, S[:, 0:1], None, ALU.is_equal), [R, S], [R])
            dv(lambda: nc.vector.tensor_scalar(R[:, 1, 12:16], R[:, 1, 8:12], -NEG, NEG, ALU.mult, ALU.add), [R], [R])
            elm = R[:, 2, 0:32]
            dv(lambda: nc.vector.tensor_tensor(elm.rearrange("p (g e) -> p g e", g=4), el.rearrange("p (g e) -> p g e", g=4),
                                               R[:, 1, 12:16].unsqueeze(2).broadcast_to([128, 4, 8]), ALU.add), [R], [R])
            dv(lambda: nc.vector.reduce_max(S[:, 3:4], elm, axis=AX.X), [R], [S])
            oh1 = rt_.OH1[:, tt, :]
            dv(lambda: nc.vector.tensor_scalar(oh1, elm, S[:, 3:4], None, ALU.is_equal), [R, S], [rt_.OH1])
            elm2 = R[:, 4, 0:32]
            dv(lambda: nc.vector.scalar_tensor_tensor(elm2, oh1, NEG, elm, ALU.mult, ALU.add), [R, rt_.OH1], [R])
            dv(lambda: nc.vector.reduce_max(S[:, 4:5], elm2, axis=AX.X), [R], [S])
            oh2 = rt_.OH2[:, tt, :]
            dv(lambda: nc.vector.tensor_scalar(oh2, elm2, S[:, 4:5], None, ALU.is_equal), [R, S], [rt_.OH2])
            dv(lambda: nc.vector.tensor_tensor(S[:, 5:6], S[:, 4:5], S[:, 3:4], ALU.subtract), [S], [S])
            p.op("act", lambda: nc.scalar.activation(out=S[:, 6:7], in_=S[:, 5:6], func=AF.Exp), reads=[S], writes=[S])
            dv(lambda: nc.vector.tensor_scalar(S[:, 7:8], S[:, 6:7], 1.0, None, ALU.add), [S], [S])
            dv(lambda: nc.vector.reciprocal(S[:, 8:9], S[:, 7:8]), [S], [S])
            dv(lambda: nc.vector.tensor_tensor(rt_.PG[:, tt, 0:1], S[:, 8:9], S[:, 2:3], ALU.mult), [S], [rt_.PG])
            dv(lambda: nc.vector.tensor_tensor(rt_.PG[:, tt, 1:2], rt_.PG[:, tt, 0:1], S[:, 6:7], ALU.mult), [S, rt_.PG], [rt_.PG])
        p.barrier()


def phase_e1(p, c, ps, rt_):
    nc = p.nc
    with ExitStack() as es:
        T = lambda name, shape, dt=F32: p.tile(es, name, shape, dt)
        cst = T("cstE", [128, 328])
        SUTR, ONES, THR, C8 = cst[:, 0:128], cst[:, 128:256], cst[:, 256:320], cst[:, 320:328]
        OHS = T("OHS", [128, 16, NE])
        CUM = T("CUM", [128, 17, NE])
        RK = T("RK", [128, 16, NE])
        W = T("W_e1", [128, 12, NE])
        CMP = T("CMP", [128, NSLOT, NE])
        ET = T("ET", [128, 2, NSLOT])
        IDXF = T("IDXF", [128, NSLOT])
        TT = T("TT_e1", [128, 16, NE])
        MM = T("MM_e1", [128, 16, NE])
        POSF = T("POSF", [128, 2, 16])
        hbs = Rot([T(f"hb1_{i}", [128, D], BF16) for i in range(3)])
        psr = Rot(ps)
        dv = lambda fn, reads, writes: p.op("dve", fn, reads=reads, writes=writes)
        p.dma("sp", cst[:], c.cstE, writes=[cst])
        dv(lambda: nc.vector.tensor_tensor(OHS[:], rt_.OH1[:], rt_.OH2[:], ALU.add), [rt_.OH1, rt_.OH2], [OHS])
        dv(lambda: nc.vector.memset(CUM[:, 0, :], 0.0), [], [CUM])
        for i in range(16):
            dv(lambda: nc.vector.tensor_tensor(CUM[:, i + 1, :], CUM[:, i, :], OHS[:, i, :], ALU.add), [CUM, OHS], [CUM])
        for i in range(16):
            bk = psr.next()
            p.op("pe", lambda: nc.tensor.matmul(bk[:, 0:NE], SUTR, OHS[:, i, :], start=True, stop=False), reads=[cst, OHS], writes=[bk], mark=False)
            p.op("pe", lambda: nc.tensor.matmul(bk[:, 0:NE], ONES, CUM[:, i, :], start=False, stop=True), reads=[cst, CUM], writes=[bk])
            p.op("act", lambda: nc.scalar.copy(RK[:, i, :], bk[:, 0:NE]), reads=[bk], writes=[RK])
        bk = psr.next()
        p.op("pe", lambda: nc.tensor.matmul(bk[:, 0:NE], ONES, CUM[:, 16, :], start=True, stop=True), reads=[cst, CUM], writes=[bk])
        cnt, r_, pf, pad, off = W[:, 0, :], W[:, 1, :], W[:, 2, :], W[:, 3, :], W[:, 6, :]
        p.op("act", lambda: nc.scalar.copy(cnt, bk[:, 0:NE]), reads=[bk], writes=[W])
        C2 = CMP[:, 0:NE, 0:16]
        dv(lambda: nc.vector.tensor_tensor(C2, cnt.unsqueeze(2).broadcast_to([128, NE, 16]), THR[:, 0:16].unsqueeze(1).broadcast_to([128, NE, 16]), ALU.is_gt),
           [W, cst], [CMP])
        dv(lambda: nc.vector.reduce_sum(pf, C2, axis=AX.X), [CMP], [W])
        dv(lambda: nc.vector.tensor_scalar(pad, pf, 128.0, None, ALU.mult), [W], [W])
        a, b = 4, 5
        dv(lambda: nc.vector.tensor_copy(W[:, a, :], pad), [W], [W])
        for sft in (1, 2, 4, 8, 16):
            dv(lambda: nc.vector.tensor_copy(W[:, b, 0:sft], W[:, a, 0:sft]), [W], [W])
            dv(lambda: nc.vector.tensor_tensor(W[:, b, sft:NE], W[:, a, sft:NE], W[:, a, 0:NE - sft], ALU.add), [W], [W])
            a, b = b, a
        END = W[:, a, :]
        dv(lambda: nc.vector.tensor_tensor(off, END, pad, ALU.subtract), [W], [W])
        dv(lambda: nc.vector.tensor_tensor(CMP[:], END.unsqueeze(1).broadcast_to([128, NSLOT, NE]), THR.unsqueeze(2).broadcast_to([128, NSLOT, NE]), ALU.is_le),
           [W, cst], [CMP])
        dv(lambda: nc.vector.reduce_sum(ET[:, 0, :], CMP[:], axis=AX.X), [CMP], [ET])
        dv(lambda: nc.vector.tensor_scalar(ET[:, 1, :], ET[:, 0, :], float(NE - 1), 128.0, ALU.min, ALU.mult), [ET], [ET])
        dv(lambda: nc.vector.tensor_scalar(IDXF[:], ET[:, 1, :], C8[:, 0:1], None, ALU.add), [ET, cst], [IDXF])
        dv(lambda: nc.vector.tensor_copy(rt_.IDXI[:], IDXF[:]), [IDXF], [rt_.IDXI])
        dv(lambda: nc.vector.tensor_tensor(TT[:], RK[:], off.unsqueeze(1).broadcast_to([128, 16, NE]), ALU.add), [RK, W], [TT])
        for k, OH in enumerate((rt_.OH1, rt_.OH2)):
            dv(lambda: nc.vector.tensor_tensor(MM[:], TT[:], OH[:], ALU.mult), [TT, OH], [MM])
            dv(lambda: nc.vector.reduce_sum(POSF[:, k, :], MM[:], axis=AX.X), [MM], [POSF])
        dv(lambda: nc.vector.tensor_copy(rt_.POSI[:], POSF[:]), [POSF], [rt_.POSI])
        xsb = Buf("XS_s")
        for tt in range(16):
            hb = hbs.next()
            p.dma("sp", hb[:], c.HNTM_s[tt * 128:(tt + 1) * 128, :], writes=[hb])
            for k in range(2):
                p.idma(c.XS_s, bass.IndirectOffsetOnAxis(rt_.POSI[:, k, tt:tt + 1], 0), hb[:], None, reads=[hb, rt_.POSI], writes=[xsb])
        p.barrier()


def phase_es(p, c, ps, rt_):
    nc = p.nc
    with ExitStack() as es:
        T = lambda name, shape, dt=F32: p.tile(es, name, shape, dt)
        idb = T("idb_s", [128, 128], BF16)
        xss = Rot([T(f"xs_s{i}", [128, D], BF16) for i in range(2)])
        xTs = Rot([T(f"xT_s{i}", [128, NDC, 128], BF16) for i in range(2)])
        units = Rot([T(f"wU{i}", [128, 3, 4096], BF16) for i in range(5)])
        sils = Rot([T(f"silS{i}", [128, 256]) for i in range(2)])
        hms = Rot([T(f"hmS{i}", [128, 8, 128], BF16) for i in range(2)])
        yts = Rot([T(f"ytS{i}", [128, D]) for i in range(2)])
        ybanks = ps[0:4]
        psr = Rot(ps[4:8])
        p.dma("pool", idb[:], c.ident, writes=[idb])
        precast_step(p, c, 1000)
        for tk in c.precast_toks:
            p._wait("pool", tk)
        pending = []

        def down(i, fc, w, hm, yt):
            for dtile in range(4):
                dsl = slice(dtile * 512, (dtile + 1) * 512)
                for j in range(2):
                    p.op("pe", lambda: nc.tensor.matmul(ybanks[dtile][:], hm[:, fc * 2 + j, :], w[:, 2, :].rearrange("p (a b) -> p a b", a=2)[:, j, dsl],
                                                        start=(fc == 0 and j == 0), stop=(fc == 3 and j == 1)),
                         reads=[hm, w], writes=[ybanks[dtile]], mark=(dtile == 3 and j == 1))
            if fc == 3:
                for dtile in range(4):
                    dsl = slice(dtile * 512, (dtile + 1) * 512)
                    if dtile % 2 == 0:
                        p.op("act", lambda: nc.scalar.copy(yt[:, dsl], ybanks[dtile][:]), reads=[ybanks[dtile]], writes=[yt])
                    else:
                        p.op("dve", lambda: nc.vector.tensor_copy(yt[:, dsl], ybanks[dtile][:]), reads=[ybanks[dtile]], writes=[yt])
                p.dma("sp", c.YP_s[i * 128:(i + 1) * 128, :], yt[:], reads=[yt])

        for i in range(NSLOT):
            xs_ = xss.next(); xT = xTs.next(); hm = hms.next(); yt = yts.next()
            p.dma("sp", xs_[:], c.XS_s[i * 128:(i + 1) * 128, :], writes=[xs_])
            for half in range(2):
                bank = psr.next()
                pb = bank[:].bitcast(BF16)
                for k in range(8):
                    dc = half * 8 + k
                    p.op("pe", lambda: nc.tensor.transpose(pb[:, k * 128:(k + 1) * 128], xs_[:, dc * 128:(dc + 1) * 128], idb[:]),
                         reads=[xs_, idb], writes=[bank], mark=(k == 7))
                if half == 0:
                    p.op("act", lambda: nc.scalar.copy(xT[:, 0:8, :], pb.rearrange("p (a b) -> p a b", a=8)), reads=[bank], writes=[xT])
                else:
                    p.op("dve", lambda: nc.vector.tensor_copy(xT[:, 8:16, :], pb.rearrange("p (a b) -> p a b", a=8)), reads=[bank], writes=[xT])
            for fc in range(4):
                w = units.next(); sil = sils.next()
                off = bass.IndirectOffsetOnAxis(rt_.IDXI[:, i:i + 1], 0)
                p.idma(w[:].rearrange("p a b -> p (a b)"), None, c.WB_s[fc], off, reads=[rt_.IDXI], writes=[w])
                bg = psr.next(); bu = psr.next()
                for (bank, m_) in ((bg, 0), (bu, 1)):
                    for j in range(2):
                        for kc in range(NDC):
                            lhs = w[:, m_, :].rearrange("p (h a b) -> p h a b", h=2, a=8)[:, kc // 8, kc % 8, j * 128:(j + 1) * 128]
                            p.op("pe", lambda: nc.tensor.matmul(bank[:, j * 128:(j + 1) * 128], lhs, xT[:, kc, :], start=(kc == 0), stop=(kc == NDC - 1)),
                                 reads=[w, xT], writes=[bank], mark=(kc == NDC - 1 and j == 1))
                p.op("act", lambda: nc.scalar.activation(out=sil[:], in_=bg[:, 0:256], func=AF.Silu), reads=[bg], writes=[sil])
                p.op("dve", lambda: nc.vector.tensor_tensor(hm[:, fc * 2:fc * 2 + 2, :], sil[:].rearrange("p (a b) -> p a b", a=2),
                                                            bu[:, 0:256].rearrange("p (a b) -> p a b", a=2), ALU.mult), reads=[sil, bu], writes=[hm])
                for fn in pending:
                    fn()
                pending = [lambda i=i, fc=fc, w=w, hm=hm, yt=yt: down(i, fc, w, hm, yt)]
        for fn in pending:
            fn()
        p.barrier()


def phase_ec(p, c, ps, rt_):
    nc = p.nc
    with ExitStack() as es:
        T = lambda name, shape, dt=F32: p.tile(es, name, shape, dt)
        fnw = T("fnw", [128, D])
        y1s = Rot([T(f"y1_{i}", [128, D]) for i in range(2)])
        y2s = Rot([T(f"y2_{i}", [128, D]) for i in range(2)])
        x2s = Rot([T(f"x2c_{i}", [128, D]) for i in range(2)])
        junk = T("junk_c", [128, D], BF16)
        st4 = Rot([T(f"st4c_{i}", [128, 4]) for i in range(2)])
        p.dma("sp", fnw[:], c.final_norm_w.partition_broadcast(128), writes=[fnw])
        for tt in range(16):
            y1 = y1s.next(); y2 = y2s.next(); x2 = x2s.next(); s4 = st4.next()
            p.dma("sp", x2[:], c.X2_s[tt * 128:(tt + 1) * 128, :], writes=[x2])
            p.idma(y1[:], None, c.YP_s, bass.IndirectOffsetOnAxis(rt_.POSI[:, 0, tt:tt + 1], 0), reads=[rt_.POSI], writes=[y1])
            p.idma(y2[:], None, c.YP_s, bass.IndirectOffsetOnAxis(rt_.POSI[:, 1, tt:tt + 1], 0), reads=[rt_.POSI], writes=[y2])
            p.op("dve", lambda: nc.vector.scalar_tensor_tensor(x2[:], y1[:], rt_.PG[:, tt, 0:1], x2[:], ALU.mult, ALU.add), reads=[y1, rt_.PG, x2], writes=[x2])
            p.op("dve", lambda: nc.vector.scalar_tensor_tensor(x2[:], y2[:], rt_.PG[:, tt, 1:2], x2[:], ALU.mult, ALU.add), reads=[y2, rt_.PG, x2], writes=[x2])
            p.op("act", lambda: nc.scalar.activation(out=junk[:], in_=x2[:], func=AF.Square, accum_out=s4[:, 0:1]), reads=[x2], writes=[junk, s4])
            p.op("dve", lambda: nc.vector.tensor_scalar(s4[:, 1:2], s4[:, 0:1], 1.0 / D, EPS, ALU.mult, ALU.add), reads=[s4], writes=[s4])
            p.op("act", lambda: nc.scalar.activation(out=s4[:, 2:3], in_=s4[:, 1:2], func=AF.Ln), reads=[s4], writes=[s4])
            p.op("act", lambda: nc.scalar.activation(out=s4[:, 3:4], in_=s4[:, 2:3], func=AF.Exp, scale=-0.5), reads=[s4], writes=[s4])
            p.op("dve", lambda: nc.vector.scalar_tensor_tensor(y1[:], x2[:], s4[:, 3:4], fnw[:], ALU.mult, ALU.mult), reads=[x2, s4, fnw], writes=[y1])
            p.dma("sp", c.out[tt * 128:(tt + 1) * 128, :], y1[:], reads=[y1])
        p.barrier()


def host_common_s(inp, m):
    for k in ("wg_t", "wu_t", "wd_t"):
        m.pop(k, None)
    wg = np.asarray(inp["w_exp_gate"])[0]
    wu = np.asarray(inp["w_exp_up"])[0]
    wd = np.asarray(inp["w_exp_down"])[0]
    tg = lambda w: np.ascontiguousarray(w.reshape(NE, 2, 8, 128, 4, 256).transpose(4, 0, 3, 1, 2, 5)).reshape(4, NE * 128, 4096)
    m["wg_r"] = tg(wg)
    m["wu_r"] = tg(wu)
    m["wd_r"] = np.ascontiguousarray(wd.reshape(NE, 4, 2, 128, D).transpose(1, 0, 3, 2, 4)).reshape(4, NE * 128, 4096)
    l = np.arange(128)
    sutr = (l[:, None] < l[None, :]).astype(np.float32)
    ones = np.ones((128, 128), np.float32)
    thr = np.tile((128.0 * np.arange(NSLOT, dtype=np.float32))[None, :], (128, 1))
    c8 = (np.arange(8, dtype=np.float32)[None, :] * 128.0 + l[:, None].astype(np.float32))
    m["cstE"] = np.ascontiguousarray(np.concatenate([sutr, ones, thr, c8], axis=1).astype(np.float32))
    return m
```

```python
import math
from contextlib import ExitStack

import numpy as np
import concourse.bass as bass
import concourse.mybir as mybir
from concourse.bass_utils import run_bass_kernel_spmd

F32 = mybir.dt.float32
BF16 = mybir.dt.bfloat16
AF = mybir.ActivationFunctionType
ALU = mybir.AluOpType
AX = mybir.AxisListType

D = 2048
NTOK = 2048
NPREV = 2048
NDC = D // 128
DI = 2048
NH = 32
HP = 64
NG = 8
DS = 128
CONV_DIM = DI + 2 * NG * DS
AH = 12
AE = 128
IN_COLS = 14880
C_Z, C_XBC, C_DT, C_Q, C_K, C_V, C_G = 0, 2048, 6144, 6176, 7712, 9248, 10784
DIL = (1, 4, 16)
NE = 32
DFF = 1024
EPS = 1e-6
NEG = -30000.0
NSLOT = 64
I32 = mybir.dt.int32


class Tok:
    __slots__ = ("sem", "val")

    def __init__(self, sem, val):
        self.sem = sem
        self.val = val


class Buf:
    def __init__(self, name=""):
        self.name = name
        self.w = []
        self.r = {}


class Tile(Buf):
    def __init__(self, name, t):
        super().__init__(name)
        self.t = t

    def __getitem__(self, k):
        return self.t[k]


def _flat(bs):
    out = []
    for b in bs:
        if b is None:
            continue
        if isinstance(b, (list, tuple)):
            out.extend(_flat(b))
        else:
            out.append(b)
    return out


class Prog:
    ENG = ("pe", "act", "dve", "pool", "sp")

    def __init__(self, nc, es):
        self.nc = nc
        self.es = es
        self.eng = {"pe": nc.tensor, "act": nc.scalar, "dve": nc.vector, "pool": nc.gpsimd, "sp": nc.sync}
        self.sem = {e: es.enter_context(nc.semaphore("s_" + e)) for e in self.ENG}
        self.cnt = {e: 0 for e in self.ENG}
        self.waited = {e: {} for e in self.ENG}
        nslots = {"sp": 28, "pool": 24, "act": 20, "bg": 14}
        self.slots = {q: [[es.enter_context(nc.semaphore(f"d_{q}{i}")), 0] for i in range(n)]
                      for q, n in nslots.items()}
        self.rr = {q: 0 for q in nslots}
        self.n_inst = 0

    def _wait(self, e, tok):
        if tok is None:
            return
        key = tok.sem.name
        if e == "pe" and tok.sem is self.sem["pe"]:
            return
        if self.waited[e].get(key, 0) >= tok.val:
            return
        self.eng[e].wait_ge(tok.sem, tok.val)
        self.waited[e][key] = tok.val
        self.n_inst += 1

    def _deps(self, e, reads, writes):
        for b in reads:
            for t in b.w:
                self._wait(e, t)
        for b in writes:
            for t in b.w:
                self._wait(e, t)
            for t in b.r.values():
                self._wait(e, t)

    def _commit(self, e, tok, reads, writes):
        for b in writes:
            b.w = [tok]
            b.r = {}
        for b in reads:
            if b not in writes:
                b.r[e] = tok

    def op(self, e, fn, reads=(), writes=(), mark=True):
        reads = _flat(reads)
        writes = _flat(writes)
        ex = [b for b in reads if getattr(b, "excl", False)]
        if ex:
            reads = [b for b in reads if b not in ex]
            writes = writes + [b for b in ex if b not in writes]
        self._deps(e, reads, writes)
        ins = fn()
        self.n_inst += 1
        if mark:
            self.cnt[e] += 1
            ins.then_inc(self.sem[e], 1)
            tok = Tok(self.sem[e], self.cnt[e])
        else:
            assert e == "pe"
            tok = Tok(self.sem[e], self.cnt[e] + 1)
        self._commit(e, tok, reads, writes)
        return tok

    def dma(self, q, out, in_, reads=(), writes=(), bg=False):
        reads = _flat(reads)
        writes = _flat(writes)
        if q == "sp" and reads and not writes:
            q = "act"
        sk = "bg" if bg else q
        slot = self.slots[sk][self.rr[sk]]
        self.rr[sk] = (self.rr[sk] + 1) % len(self.slots[sk])
        if slot[1] > 0:
            self._wait(q, Tok(slot[0], slot[1]))
        self._deps(q, reads, writes)
        ins = self.eng[q].dma_start(out=out, in_=in_)
        self.n_inst += 1
        slot[1] += 16
        ins.then_inc(slot[0], 16)
        tok = Tok(slot[0], slot[1])
        self._commit(q, tok, reads, writes)
        return tok

    def idma(self, out, out_off, in_, in_off, reads=(), writes=()):
        q = "pool"
        reads = _flat(reads)
        writes = _flat(writes)
        slot = self.slots[q][self.rr[q]]
        self.rr[q] = (self.rr[q] + 1) % len(self.slots[q])
        if slot[1] > 0:
            self._wait(q, Tok(slot[0], slot[1]))
        self._deps(q, reads, writes)
        ins = self.nc.gpsimd.indirect_dma_start(out, out_off, in_, in_off)
        self.n_inst += 1
        slot[1] += 16
        ins.then_inc(slot[0], 16)
        tok = Tok(slot[0], slot[1])
        self._commit(q, tok, reads, writes)
        return tok

    def barrier(self):
        toks = [Tok(self.sem[e], self.cnt[e]) for e in self.ENG if self.cnt[e] > 0]
        for q, sl in self.slots.items():
            if q == "bg":
                continue
            for s, c in sl:
                if c > 0:
                    toks.append(Tok(s, c))
        for e in self.ENG:
            for t in toks:
                if t.sem is self.sem[e]:
                    continue
                self._wait(e, t)

    def tile(self, es, name, shape, dtype):
        self.n_tiles = getattr(self, "n_tiles", 0) + 1
        t = es.enter_context(self.nc.sbuf_tensor(f"sb{self.n_tiles}_{name}", list(shape), dtype))
        return Tile(name, t)


class Rot:
    def __init__(self, tiles):
        self.tiles = tiles
        self.i = 0

    def next(self):
        t = self.tiles[self.i]
        self.i = (self.i + 1) % len(self.tiles)
        return t


def tile_w(w2d, cw):
    K, C = w2d.shape
    assert C % cw == 0 and K % 128 == 0
    kc = K // 128
    a = w2d.reshape(kc, 128, C // cw, cw).transpose(2, 1, 0, 3)
    return np.ascontiguousarray(a).reshape(C // cw, 128, kc * cw)


class Ctx:
    pass


def declare_io(nc, debug=False, phases="abcdeE"):
    c = Ctx()
    di = lambda name, shape, dt=F32: nc.dram_tensor(name, list(shape), dt, kind="ExternalInput").ap()
    c.x_own = di("x_own", [NTOK, D])
    c.x_prev = di("x_prev", [NPREV, D])
    c.w_fm = di("w_fm", [88, 128, NDC * 128])
    c.w_tm = di("w_tm", [7, 128, NDC * 512])
    c.w_dt = di("w_dt", [128, NDC * 32])
    c.attn_norm_w = di("attn_norm_w", [1, D])
    c.b_gate = di("b_gate", [128, 32])
    c.ident = di("ident", [128, 128])
    c.cstB = di("cstB", [128, 1024])
    c.conv_wb = di("conv_wb", [128, 32 * 5])
    c.vec32 = di("vec32", [3, NH])
    c.ssd_norm_w = di("ssd_norm_w", [1, DI])
    c.flag = di("flag", [128, 2])
    c.attn_bias = di("attn_bias", [3, 128, 1024])
    c.w_ssd_fm = di("w_ssd_fm", [16, 128, NDC * 128])
    c.w_attn_fm = di("w_attn_fm", [16, 128, 4 * 128])
    c.w_out_tm = di("w_out_tm", [4, 128, NDC * 512])
    c.ffn_norm_w = di("ffn_norm_w", [1, D])
    c.w_router = di("w_router", [128, NDC * 36])
    c.b_router = di("b_router", [1, 36])
    if "E" in phases:
        c.wg_r = di("wg_r", [4, NE * 128, 4096])
        c.wu_r = di("wu_r", [4, NE * 128, 4096])
        c.wd_r = di("wd_r", [4, NE * 128, 4096])
        c.WB_s = [nc.dram_tensor(f"WB_s{fc}", [NE * 128, 3 * 4096], BF16).ap() for fc in range(4)]
        c.precast = [(fc, k, src, r0) for fc in range(4) for r0 in range(0, NE * 128, 512) for (k, src) in ((0, c.wg_r), (1, c.wu_r), (2, c.wd_r))]
        c.precast_toks = []
    c.cstE = di("cstE", [128, 128 + 128 + 64 + 8])
    c.final_norm_w = di("final_norm_w", [1, D])
    c.out = nc.dram_tensor("out", [NTOK, D], F32, kind="ExternalOutput").ap()
    kind = "ExternalOutput" if debug else "Internal"
    ds = lambda name, shape, dt=F32: nc.dram_tensor(name, list(shape), dt, kind=kind).ap()
    c.XBC_s = ds("XBC_s", [CONV_DIM, 3 + NPREV + NTOK])
    c.Z_s = ds("Z_s", [NTOK, DI])
    c.DT_s = ds("DT_s", [NPREV + NTOK, NH])
    c.QT_s = ds("QT_s", [AH, 128, NTOK], BF16)
    c.KT_s = ds("KT_s", [AH, 128, NPREV + NTOK], BF16)
    c.V_s = ds("V_s", [3, 32, 128, 512], BF16)
    c.G_s = ds("G_s", [2 * D, NTOK])
    c.YN_s = ds("YN_s", [NDC, 128, NTOK], BF16)
    c.O_s = ds("O_s", [3, NTOK, 4 * 130])
    c.OT_s = ds("OT_s", [4, 128, NTOK], BF16)
    c.M_s = ds("M_s", [NDC, 128, NTOK], BF16)
    c.X2_s = ds("X2_s", [NTOK, D])
    c.HN_s = ds("HN_s", [NDC, 128, NTOK], BF16)
    c.GATE_s = ds("GATE_s", [NTOK, NE])
    c.HNTM_s = ds("HNTM_s", [NTOK, D], BF16)
    c.XS_s = ds("XS_s", [NSLOT * 128, D], BF16)
    c.YP_s = ds("YP_s", [NSLOT * 128, D])
    return c


def precast_step(p, c, n=1):
    for _ in range(n):
        if not getattr(c, "precast", None):
            return
        fc, k, src, r0 = c.precast.pop(0)
        dst = c.WB_s[fc][r0:r0 + 512, k * 4096:(k + 1) * 4096].rearrange("r (a b) -> r a b", a=2)
        c.precast_toks.append(p.dma("pool", dst, src[fc, r0:r0 + 512, :].rearrange("r (a b) -> r a b", a=2), bg=True))


def cls_view(ap2d, d):
    if d == 1:
        return ap2d.unsqueeze(1)
    return ap2d.rearrange("p (j r) -> p r j", r=d)


def phase_a(p, c, ps):
    nc = p.nc
    with ExitStack() as es:
        hT = p.tile(es, "hT", [128, NDC, 2048], BF16)
        nw = p.tile(es, "nw_bc", [128, D], F32)
        bg = p.tile(es, "bg", [128, 32], F32)
        idb = p.tile(es, "idb", [128, 128], BF16)
        zero = p.tile(es, "zero", [128, 4], F32)
        xts = Rot([p.tile(es, f"xt{i}", [128, D], F32) for i in range(2)])
        hbs = Rot([p.tile(es, f"hb{i}", [128, D], BF16) for i in range(2)])
        junk = p.tile(es, "junk", [128, D], BF16)
        sts = Rot([p.tile(es, f"st{i}", [128, 4], F32) for i in range(2)])
        wfm = Rot([p.tile(es, f"wfm{i}", [128, NDC, 128], BF16) for i in range(6)])
        wtm = Rot([p.tile(es, f"wtm{i}", [128, NDC, 512], BF16) for i in range(2)])
        wdt = p.tile(es, "wdt", [128, NDC, 32], BF16)
        so32 = Rot([p.tile(es, f"so32_{i}", [128, 512], F32) for i in range(4)])
        so16 = Rot([p.tile(es, f"so16_{i}", [128, 512], BF16) for i in range(4)])
        psr = Rot(ps)
        ev = [0]

        p.dma("sp", nw[:], c.attn_norm_w.partition_broadcast(128), writes=[nw])
        p.dma("sp", bg[:], c.b_gate, writes=[bg])
        p.dma("pool", idb[:], c.ident, writes=[idb])
        p.dma("pool", wdt[:].rearrange("p a b -> p (a b)"), c.w_dt, writes=[wdt])
        p.op("dve", lambda: nc.vector.memset(zero[:], 0.0), writes=[zero])
        for r0 in range(0, CONV_DIM, 128):
            p.dma("sp", c.XBC_s[r0:r0 + 128, 0:3], zero[:, 0:3], reads=[zero])

        def evac_copy(dst_ap, src_ap, reads, writes, scale=None):
            e = "act" if ev[0] % 2 == 0 else "dve"
            ev[0] += 1
            if e == "act":
                if scale is None:
                    return p.op("act", lambda: nc.scalar.copy(dst_ap, src_ap), reads=reads, writes=writes)
                return p.op("act", lambda: nc.scalar.activation(out=dst_ap, in_=src_ap, func=AF.Copy, scale=scale),
                            reads=reads, writes=writes)
            if scale is None:
                return p.op("dve", lambda: nc.vector.tensor_copy(dst_ap, src_ap), reads=reads, writes=writes)
            return p.op("dve", lambda: nc.vector.tensor_scalar(dst_ap, src_ap, scale, None, ALU.mult),
                        reads=reads, writes=writes)

        def build_hT(xsrc):
            for tt in range(16):
                xt = xts.next(); hb = hbs.next(); st = sts.next()
                p.dma("sp", xt[:], xsrc[tt * 128:(tt + 1) * 128, :], writes=[xt])
                p.op("act", lambda: nc.scalar.activation(out=junk[:], in_=xt[:], func=AF.Square, accum_out=st[:, 0:1]),
                     reads=[xt], writes=[junk, st])
                p.op("dve", lambda: nc.vector.tensor_scalar(st[:, 1:2], st[:, 0:1], 1.0 / D, EPS, ALU.mult, ALU.add),
                     reads=[st], writes=[st])
                p.op("act", lambda: nc.scalar.activation(out=st[:, 2:3], in_=st[:, 1:2], func=AF.Ln), reads=[st], writes=[st])
                p.op("act", lambda: nc.scalar.activation(out=st[:, 3:4], in_=st[:, 2:3], func=AF.Exp, scale=-0.5),
                     reads=[st], writes=[st])
                p.op("dve", lambda: nc.vector.scalar_tensor_tensor(hb[:], xt[:], st[:, 3:4], nw[:], ALU.mult, ALU.mult),
                     reads=[xt, st, nw], writes=[hb])
                for half in range(2):
                    bank = psr.next()
                    pb = bank[:].bitcast(BF16)
                    for k in range(8):
                        dc = half * 8 + k
                        p.op("pe", lambda: nc.tensor.transpose(pb[:, k * 128:(k + 1) * 128], hb[:, dc * 128:(dc + 1) * 128], idb[:]),
                             reads=[hb, idb], writes=[bank], mark=(k == 7))
                    evac_copy(hT[:, half * 8:half * 8 + 8, tt * 128:(tt + 1) * 128],
                              pb.rearrange("p (a b) -> p a b", a=8), [bank], [hT])

        def mm_group(bank, out_ap, lhs_fn, rhs_fn, reads):
            for dc in range(NDC):
                p.op("pe", lambda: nc.tensor.matmul(out_ap, lhs_fn(dc), rhs_fn(dc), start=(dc == 0), stop=(dc == NDC - 1)),
                     reads=reads, writes=[bank], mark=(dc == NDC - 1))

        nfm = [0]

        def fm_chunk(cc, jobs):
            nfm[0] += 1
            if nfm[0] % 4 == 0:
                precast_step(p, c)
            wb = wfm.next()
            p.dma("pool", wb[:].rearrange("p a b -> p (a b)"), c.w_fm[cc], writes=[wb])
            for rhs_fn, n, epi in jobs:
                bank = psr.next()
                mm_group(bank, bank[:, 0:n], lambda dc: wb[:, dc, :], rhs_fn, [wb, hT])
                epi(bank, n)

        def nat_rhs(st):
            return lambda dc: hT[:, dc, st * 512:(st + 1) * 512]

        def epi_store32(dst_ap):
            def f(bank, n):
                so = so32.next()
                evac_copy(so[:, 0:n], bank[:, 0:n], [bank], [so])
                p.dma("sp", dst_ap, so[:, 0:n], reads=[so])
            return f

        def epi_store16(dst_ap, scale=None, n3=None):
            def f(bank, n):
                so = so16.next()
                evac_copy(so[:, 0:n], bank[:, 0:n], [bank], [so], scale=scale)
                src = so[:, 0:n]
                if n3 is not None:
                    src = src.rearrange("p (a b) -> p a b", a=n3)
                p.dma("sp", dst_ap, src, reads=[so])
            return f

        def epi_gate(cc, dst_ap):
            def f(bank, n):
                so = so32.next()
                p.op("act", lambda: nc.scalar.activation(out=so[:, 0:n], in_=bank[:, 0:n], func=AF.Sigmoid, bias=bg[:, cc:cc + 1]),
                     reads=[bank, bg], writes=[so])
                p.dma("sp", dst_ap, so[:, 0:n], reads=[so])
            return f

        def kt_view(h, d):
            return c.KT_s[h].rearrange("e (r n j) -> e r n j", r=d, j=128)

        def tm_tile(wt_ap_fn, ncols, lhs_list, dst_list, dtype32=True):
            for lhs_fn, dst in zip(lhs_list, dst_list):
                bank = psr.next()
                mm_group(bank, bank[:, 0:ncols], lhs_fn, wt_ap_fn, [hT] + tm_reads[0])
                if dtype32:
                    so = so32.next()
                else:
                    so = so16.next()
                evac_copy(so[:, 0:ncols], bank[:, 0:ncols], [bank], [so])
                p.dma("sp", dst, so[:, 0:ncols], reads=[so])

        tm_reads = [[]]

        def v_blocks(g, prev):
            d = DIL[g]
            span = 128 * d
            nprevb = NPREV // span
            ncls = 4096 // span
            lhs, dst = [], []
            if prev:
                blocks = [(r, nprevb - 1) for r in range(d)]
            else:
                blocks = [(r, n) for r in range(d) for n in range(NTOK // span)]
            for r, n in blocks:
                def lf(dc, r=r, n=n):
                    return cls_view(hT[:, dc, :], d)[:, r, n * 128:(n + 1) * 128]
                lhs.append(lf)
                ng = n if prev else nprevb + n
                dst.append(c.V_s[g, r * ncls + ng])
            return lhs, dst

        build_hT(c.x_prev)
        for cc in range(24):
            fm_chunk(cc, [(nat_rhs(st), 512, epi_store32(c.XBC_s[cc * 128:(cc + 1) * 128, 3 + st * 512: 3 + (st + 1) * 512]))
                          for st in range(4)])
        for cc in range(24, 32):
            fm_chunk(cc, [(nat_rhs(3), 512, epi_store32(c.XBC_s[cc * 128:(cc + 1) * 128, 3 + 3 * 512: 3 + 4 * 512]))])
        for h in range(AH):
            g = h // 4
            d = DIL[g]
            cc = 44 + h
            if g == 0:
                jobs = [(lambda dc: hT[:, dc, 1920:2048], 128, epi_store16(c.KT_s[h][:, 15 * 128:16 * 128]))]
            elif g == 1:
                jobs = [(lambda dc: cls_view(hT[:, dc, :], 4)[:, :, 384:512], 512,
                         epi_store16(kt_view(h, 4)[:, :, 3, :], n3=4))]
            else:
                jobs = [((lambda dc, st=st: cls_view(hT[:, dc, :], 16)[:, 4 * st:4 * st + 4, :]), 512,
                         epi_store16(kt_view(h, 16)[:, 4 * st:4 * st + 4, 0, :], n3=4)) for st in range(4)]
            fm_chunk(cc, jobs)
        tm_reads[0] = [wdt]
        tm_tile(lambda dc: wdt[:, dc, :], 32,
                [(lambda dc, tt=tt: hT[:, dc, tt * 128:(tt + 1) * 128]) for tt in range(16)],
                [c.DT_s[tt * 128:(tt + 1) * 128, :] for tt in range(16)])
        for g in range(3):
            wt = wtm.next()
            p.dma("pool", wt[:].rearrange("p a b -> p (a b)"), c.w_tm[4 + g], writes=[wt])
            tm_reads[0] = [wt]
            lhs, dst = v_blocks(g, True)
            tm_tile(lambda dc: wt[:, dc, :], 512, lhs, dst, dtype32=False)

        build_hT(c.x_own)
        for cc in range(32):
            fm_chunk(cc, [(nat_rhs(st), 512,
                           epi_store32(c.XBC_s[cc * 128:(cc + 1) * 128, 3 + NPREV + st * 512: 3 + NPREV + (st + 1) * 512]))
                          for st in range(4)])
        inv = 1.0 / math.sqrt(AE)
        for h in range(AH):
            g = h // 4
            d = DIL[g]
            if g == 0:
                jobs = [(nat_rhs(st), 512, epi_store16(c.QT_s[h][:, st * 512:(st + 1) * 512], scale=inv)) for st in range(4)]
            elif g == 1:
                jobs = [((lambda dc, r=r: cls_view(hT[:, dc, :], 4)[:, r, :]), 512,
                         epi_store16(c.QT_s[h][:, r * 512:(r + 1) * 512], scale=inv)) for r in range(4)]
            else:
                jobs = [((lambda dc, st=st: cls_view(hT[:, dc, :], 16)[:, 4 * st:4 * st + 4, :]), 512,
                         epi_store16(c.QT_s[h][:, st * 512:(st + 1) * 512], scale=inv)) for st in range(4)]
            fm_chunk(32 + h, jobs)
            if g == 0:
                jobs = [(nat_rhs(st), 512, epi_store16(c.KT_s[h][:, (16 + 4 * st) * 128:(20 + 4 * st) * 128])) for st in range(4)]
            elif g == 1:
                jobs = [((lambda dc, r=r: cls_view(hT[:, dc, :], 4)[:, r, :]), 512,
                         epi_store16(c.KT_s[h][:, r * 1024 + 512:(r + 1) * 1024])) for r in range(4)]
            else:
                jobs = [((lambda dc, st=st: cls_view(hT[:, dc, :], 16)[:, 4 * st:4 * st + 4, :]), 512,
                         epi_store16(kt_view(h, 16)[:, 4 * st:4 * st + 4, 1, :], n3=4)) for st in range(4)]
            fm_chunk(44 + h, jobs)
        for cc in range(32):
            fm_chunk(56 + cc, [(nat_rhs(st), 512, epi_gate(cc, c.G_s[cc * 128:(cc + 1) * 128, st * 512:(st + 1) * 512]))
                               for st in range(4)])
        for ct in range(4):
            wt = wtm.next()
            p.dma("pool", wt[:].rearrange("p a b -> p (a b)"), c.w_tm[ct], writes=[wt])
            tm_reads[0] = [wt]
            tm_tile(lambda dc: wt[:, dc, :], 512,
                    [(lambda dc, tt=tt: hT[:, dc, tt * 128:(tt + 1) * 128]) for tt in range(16)],
                    [c.Z_s[tt * 128:(tt + 1) * 128, ct * 512:(ct + 1) * 512] for tt in range(16)])
        tm_reads[0] = [wdt]
        tm_tile(lambda dc: wdt[:, dc, :], 32,
                [(lambda dc, tt=tt: hT[:, dc, tt * 128:(tt + 1) * 128]) for tt in range(16)],
                [c.DT_s[NPREV + tt * 128:NPREV + (tt + 1) * 128, :] for tt in range(16)])
        for g in range(3):
            wt = wtm.next()
            p.dma("pool", wt[:].rearrange("p a b -> p (a b)"), c.w_tm[4 + g], writes=[wt])
            tm_reads[0] = [wt]
            lhs, dst = v_blocks(g, False)
            tm_tile(lambda dc: wt[:, dc, :], 512, lhs, dst, dtype32=False)
        p.barrier()


def host_common(inp):
    w_in = np.asarray(inp["w_in"])[0]
    fm_cols = np.concatenate([w_in[:, C_XBC:C_DT], w_in[:, C_Q:C_K], w_in[:, C_K:C_V], w_in[:, C_G:IN_COLS]], axis=1)
    tm_cols = np.concatenate([w_in[:, C_Z:C_XBC], w_in[:, C_V:C_G]], axis=1)
    m = {}
    m["w_fm"] = tile_w(fm_cols, 128)
    m["w_tm"] = tile_w(tm_cols, 512)
    m["w_dt"] = tile_w(w_in[:, C_DT:C_Q], 32)[0]
    m["attn_norm_w"] = np.ascontiguousarray(np.asarray(inp["attn_norm_w"]).reshape(1, D))
    m["b_gate"] = np.ascontiguousarray(np.asarray(inp["b_gate"]).reshape(32, 128).T)
    m["ident"] = np.eye(128, dtype=np.float32)
    return m


def phase_b(p, c, ps):
    nc = p.nc
    with ExitStack() as es:
        T = lambda name, shape, dt=F32: p.tile(es, name, shape, dt)
        cst = T("cstB", [128, 1024])
        TRI, SU, ONES, IDF, NEGM = (cst[:, 0:128], cst[:, 128:256], cst[:, 256:384], cst[:, 384:512], cst[:, 512:1024])
        idb = T("idbB", [128, 128], BF16)
        cwb = T("cwb", [128, 32, 5])
        v32 = T("v32", [128, 3, NH])
        A_bc = T("A_bc", [128, NH])
        nws = T("nws", [128, DI])
        flag = T("flagB", [128, 2])
        us = Rot([T(f"u{i}", [128, 515]) for i in range(3)])
        accs = Rot([T(f"acc{i}", [128, 512]) for i in range(2)])
        xcs = Rot([T(f"xc{i}", [128, 512]) for i in range(2)])
        xs_tm = T("xs_tm", [128, 4, DI])
        B_tm = T("B_tm", [128, 4, NG * DS], BF16)
        BT = T("BT", [128, NG, 512], BF16)
        CT = T("CT", [128, NG, 512], BF16)
        dtr = Rot([T(f"dtr{i}", [128, NH]) for i in range(2)])
        sm = Rot([T(f"sm{i}", [128, 8, NH]) for i in range(2)])
        Ex = Rot([T(f"Ex{i}", [128, 3, NH]) for i in range(2)])
        L_all = T("L_all", [128, NH, 128])
        x_dt = T("x_dt", [128, DI], BF16)
        xw = T("xw", [128, DI], BF16)
        decs = Rot([T(f"dec{i}", [128, 512]) for i in range(2)])
        MTs = Rot([T(f"MT{i}", [128, 512], BF16) for i in range(2)])
        y = T("y", [128, DI])
        tmp = T("tmpB", [128, DI])
        zts = Rot([T(f"zt{i}", [128, DI]) for i in range(2)])
        yn = T("yn", [128, DI], BF16)
        junk = T("junkB", [128, DI], BF16)
        st4 = Rot([T(f"st4_{i}", [128, 4]) for i in range(2)])
        S = T("S", [128, DI])
        S_bf = T("S_bf", [128, DI], BF16)
        ynT = Rot([T(f"ynT{i}", [128, NDC, 128], BF16) for i in range(2)])
        psr = Rot(ps)

        p.dma("sp", cst[:], c.cstB, writes=[cst])
        p.dma("pool", idb[:], c.ident, writes=[idb])
        p.dma("sp", cwb[:].rearrange("p a b -> p (a b)"), c.conv_wb, writes=[cwb])
        p.dma("sp", v32[:].rearrange("p a b -> p (a b)"), c.vec32.rearrange("a b -> (a b)").partition_broadcast(128), writes=[v32])
        p.dma("sp", nws[:], c.ssd_norm_w.partition_broadcast(128), writes=[nws])
        p.dma("sp", flag[:], c.flag, writes=[flag])
        p.op("act", lambda: nc.scalar.activation(out=A_bc[:], in_=v32[:, 1, :], func=AF.Exp), reads=[v32], writes=[A_bc])
        p.op("dve", lambda: nc.vector.tensor_scalar(A_bc[:], A_bc[:], -1.0, None, ALU.mult), reads=[A_bc], writes=[A_bc])
        p.op("dve", lambda: nc.vector.memset(S[:], 0.0), writes=[S])
        p.op("dve", lambda: nc.vector.memset(S_bf[:], 0.0), writes=[S_bf])
        DTB = v32[:, 0, :]
        D_bc = v32[:, 2, :]
        bc = lambda ap, n: ap.unsqueeze(2).broadcast_to([128, ap.shape[1], n])

        def conv_supertile(t0, own):
            ncc = 32 if own else 24
            for cc in range(ncc):
                u = us.next(); acc = accs.next(); xc = xcs.next()
                p.dma("sp", u[:], c.XBC_s[cc * 128:(cc + 1) * 128, t0:t0 + 515], writes=[u])
                p.op("dve", lambda: nc.vector.tensor_scalar(acc[:], u[:, 0:512], cwb[:, cc, 0:1], cwb[:, cc, 4:5], ALU.mult, ALU.add),
                     reads=[u, cwb], writes=[acc])
                for w in range(1, 4):
                    p.op("dve", lambda: nc.vector.scalar_tensor_tensor(acc[:], u[:, w:w + 512], cwb[:, cc, w:w + 1], acc[:], ALU.mult, ALU.add),
                         reads=[u, cwb, acc], writes=[acc])
                if cc < 16:
                    p.op("act", lambda: nc.scalar.activation(out=xc[:], in_=acc[:], func=AF.Silu), reads=[acc], writes=[xc])
                    bank = psr.next()
                    for k in range(4):
                        p.op("pe", lambda: nc.tensor.transpose(bank[:, k * 128:(k + 1) * 128], xc[:, k * 128:(k + 1) * 128], IDF),
                             reads=[xc, cst], writes=[bank], mark=(k == 3))
                    p.op("dve", lambda: nc.vector.tensor_copy(xs_tm[:, :, cc * 128:(cc + 1) * 128], bank[:].rearrange("p (a b) -> p a b", a=4)),
                         reads=[bank], writes=[xs_tm])
                elif cc < 24:
                    g = cc - 16
                    p.op("act", lambda: nc.scalar.activation(out=BT[:, g, :], in_=acc[:], func=AF.Silu), reads=[acc], writes=[BT])
                    bank = psr.next()
                    pb = bank[:].bitcast(BF16)
                    for k in range(4):
                        p.op("pe", lambda: nc.tensor.transpose(pb[:, k * 128:(k + 1) * 128], BT[:, g, k * 128:(k + 1) * 128], idb[:]),
                             reads=[BT, idb], writes=[bank], mark=(k == 3))
                    p.op("dve", lambda: nc.vector.tensor_copy(B_tm[:, :, g * 128:(g + 1) * 128], pb[:, 0:512].rearrange("p (a b) -> p a b", a=4)),
                         reads=[bank], writes=[B_tm])
                else:
                    g = cc - 24
                    p.op("act", lambda: nc.scalar.activation(out=CT[:, g, :], in_=acc[:], func=AF.Silu), reads=[acc], writes=[CT])

        def chunk(ci, k, own):
            precast_step(p, c, 2)
            dr = dtr.next(); s = sm.next(); E = Ex.next()
            p.dma("sp", dr[:], c.DT_s[ci * 128:(ci + 1) * 128, :], writes=[dr])
            p.op("dve", lambda: nc.vector.tensor_tensor(s[:, 0, :], dr[:], DTB, ALU.add), reads=[dr, v32], writes=[s])
            p.op("act", lambda: nc.scalar.activation(out=s[:, 1, :], in_=s[:, 0, :], func=AF.Exp), reads=[s], writes=[s])
            p.op("act", lambda: nc.scalar.activation(out=s[:, 2, :], in_=s[:, 1, :], func=AF.Ln, bias=1.0), reads=[s], writes=[s])
            p.op("dve", lambda: nc.vector.tensor_tensor(s[:, 3, :], s[:, 2, :], A_bc[:], ALU.mult), reads=[s, A_bc], writes=[s])
            bk = psr.next()
            p.op("pe", lambda: nc.tensor.matmul(bk[:, 0:NH], TRI, s[:, 3, :], start=True, stop=True), reads=[cst, s], writes=[bk], mark=False)
            p.op("pe", lambda: nc.tensor.matmul(bk[:, NH:2 * NH], ONES, s[:, 3, :], start=True, stop=True), reads=[cst, s], writes=[bk])
            p.op("act", lambda: nc.scalar.copy(s[:, 4:6, :].rearrange("p a b -> p (a b)"), bk[:, 0:2 * NH]), reads=[bk], writes=[s])
            p.op("dve", lambda: nc.vector.tensor_tensor(s[:, 6, :], s[:, 5, :], s[:, 4, :], ALU.subtract), reads=[s], writes=[s])
            p.op("act", lambda: nc.scalar.activation(out=E[:].rearrange("p a b -> p (a b)"), in_=s[:, 4:7, :].rearrange("p a b -> p (a b)"), func=AF.Exp),
                 reads=[s], writes=[E])
            p.op("dve", lambda: nc.vector.tensor_tensor(s[:, 7, :], s[:, 2, :], E[:, 2, :], ALU.mult), reads=[s, E], writes=[s])
            xs3 = xs_tm[:, k, :].rearrange("p (h q) -> p h q", h=NH)
            p.op("dve", lambda: nc.vector.tensor_tensor(xw[:].rearrange("p (h q) -> p h q", h=NH), xs3, bc(s[:, 7, :], HP), ALU.mult),
                 reads=[xs_tm, s], writes=[xw])
            if own:
                p.op("dve", lambda: nc.vector.tensor_tensor(x_dt[:].rearrange("p (h q) -> p h q", h=NH), xs3, bc(s[:, 2, :], HP), ALU.mult),
                     reads=[xs_tm, s], writes=[x_dt])
                p.op("dve", lambda: nc.vector.tensor_tensor(L_all[:], SU.unsqueeze(1).broadcast_to([128, NH, 128]), bc(s[:, 3, :], 128), ALU.mult),
                     reads=[cst, s], writes=[L_all])
            for g in range(NG):
                hs = slice(g * 4, g * 4 + 4)
                cs = slice(g * 256, (g + 1) * 256)
                if own:
                    bseg = psr.next()
                    for hh in range(4):
                        p.op("pe", lambda: nc.tensor.matmul(bseg[:, hh * 128:(hh + 1) * 128], L_all[:, g * 4 + hh, :], TRI, start=True, stop=False),
                             reads=[L_all, cst], writes=[bseg], mark=False)
                        p.op("pe", lambda: nc.tensor.matmul(bseg[:, hh * 128:(hh + 1) * 128], IDF, NEGM[:, 0:128], start=False, stop=True),
                             reads=[cst], writes=[bseg], mark=(hh == 3))
                    dec = decs.next(); MT = MTs.next()
                    p.op("act", lambda: nc.scalar.activation(out=dec[:], in_=bseg[:], func=AF.Exp), reads=[bseg], writes=[dec])
                    bcb = psr.next()
                    p.op("pe", lambda: nc.tensor.matmul(bcb[:, 0:128], BT[:, g, k * 128:(k + 1) * 128], CT[:, g, k * 128:(k + 1) * 128], start=True, stop=True),
                         reads=[BT, CT], writes=[bcb])
                    p.op("dve", lambda: nc.vector.tensor_tensor(MT[:].rearrange("p (h l) -> p h l", h=4), dec[:].rearrange("p (h l) -> p h l", h=4),
                                                                bcb[:, 0:128].unsqueeze(1).broadcast_to([128, 4, 128]), ALU.mult),
                         reads=[dec, bcb], writes=[MT])
                    by = psr.next()
                    for hh in range(4):
                        h = g * 4 + hh
                        p.op("pe", lambda: nc.tensor.matmul(by[:, hh * 64:(hh + 1) * 64], MT[:, hh * 128:(hh + 1) * 128], x_dt[:, h * 64:(h + 1) * 64], start=True, stop=True),
                             reads=[MT, x_dt], writes=[by], mark=False)
                    p.op("pe", lambda: nc.tensor.matmul(by[:, 256:512], CT[:, g, k * 128:(k + 1) * 128], S_bf[:, cs], start=True, stop=True),
                         reads=[CT, S_bf], writes=[by])
                    p.op("dve", lambda: nc.vector.tensor_tensor(tmp[:, cs].rearrange("p (h q) -> p h q", h=4), by[:, 256:512].rearrange("p (h q) -> p h q", h=4),
                                                                bc(E[:, 0, hs], HP), ALU.mult), reads=[by, E], writes=[tmp])
                    p.op("dve", lambda: nc.vector.tensor_tensor(y[:, cs], tmp[:, cs], by[:, 0:256], ALU.add), reads=[tmp, by], writes=[y])
                bs_ = psr.next()
                p.op("pe", lambda: nc.tensor.matmul(bs_[:, 0:256], B_tm[:, k, g * 128:(g + 1) * 128], xw[:, cs], start=True, stop=True),
                     reads=[B_tm, xw], writes=[bs_])
                p.op("dve", lambda: nc.vector.tensor_tensor(S[:, cs].rearrange("p (h q) -> p h q", h=4), S[:, cs].rearrange("p (h q) -> p h q", h=4),
                                                            bc(E[:, 1, hs], HP), ALU.mult), reads=[S, E], writes=[S])
                p.op("dve", lambda: nc.vector.tensor_tensor(S[:, cs], S[:, cs], bs_[:, 0:256], ALU.add), reads=[S, bs_], writes=[S])
                p.op("act", lambda: nc.scalar.copy(S_bf[:, cs], S[:, cs]), reads=[S], writes=[S_bf])
            if not own:
                return
            tt = ci - 16
            p.op("dve", lambda: nc.vector.tensor_tensor(tmp[:].rearrange("p (h q) -> p h q", h=NH), xs3, bc(D_bc, HP), ALU.mult),
                 reads=[xs_tm, v32], writes=[tmp])
            p.op("dve", lambda: nc.vector.tensor_tensor(y[:], y[:], tmp[:], ALU.add), reads=[y, tmp], writes=[y])
            zt = zts.next(); s4 = st4.next()
            p.dma("sp", zt[:], c.Z_s[tt * 128:(tt + 1) * 128, :], writes=[zt])
            p.op("act", lambda: nc.scalar.activation(out=zt[:], in_=zt[:], func=AF.Silu), reads=[zt], writes=[zt])
            p.op("dve", lambda: nc.vector.tensor_tensor(y[:], y[:], zt[:], ALU.mult), reads=[y, zt], writes=[y])
            p.op("act", lambda: nc.scalar.activation(out=junk[:], in_=y[:], func=AF.Square, accum_out=s4[:, 0:1]), reads=[y], writes=[junk, s4])
            p.op("dve", lambda: nc.vector.tensor_scalar(s4[:, 1:2], s4[:, 0:1], 1.0 / DI, EPS, ALU.mult, ALU.add), reads=[s4], writes=[s4])
            p.op("act", lambda: nc.scalar.activation(out=s4[:, 2:3], in_=s4[:, 1:2], func=AF.Ln), reads=[s4], writes=[s4])
            p.op("act", lambda: nc.scalar.activation(out=s4[:, 3:4], in_=s4[:, 2:3], func=AF.Exp, scale=-0.5), reads=[s4], writes=[s4])
            p.op("dve", lambda: nc.vector.scalar_tensor_tensor(yn[:], y[:], s4[:, 3:4], nws[:], ALU.mult, ALU.mult), reads=[y, s4, nws], writes=[yn])
            yT = ynT.next()
            for half in range(2):
                bank = psr.next()
                pb = bank[:].bitcast(BF16)
                for kk in range(8):
                    dc = half * 8 + kk
                    p.op("pe", lambda: nc.tensor.transpose(pb[:, kk * 128:(kk + 1) * 128], yn[:, dc * 128:(dc + 1) * 128], idb[:]),
                         reads=[yn, idb], writes=[bank], mark=(kk == 7))
                if half == 0:
                    p.op("act", lambda: nc.scalar.copy(yT[:, 0:8, :], pb.rearrange("p (a b) -> p a b", a=8)), reads=[bank], writes=[yT])
                else:
                    p.op("dve", lambda: nc.vector.tensor_copy(yT[:, 8:16, :], pb.rearrange("p (a b) -> p a b", a=8)), reads=[bank], writes=[yT])
            p.dma("sp", c.YN_s[:, :, tt * 128:(tt + 1) * 128].rearrange("a p t -> p a t"), yT[:], reads=[yT])

        for sti in range(8):
            own = sti >= 4
            conv_supertile(sti * 512, own)
            for k in range(4):
                chunk(sti * 4 + k, k, own)
            if sti == 3:
                p.op("dve", lambda: nc.vector.tensor_scalar(S[:], S[:], flag[:, 0:1], None, ALU.mult), reads=[S, flag], writes=[S])
                p.op("act", lambda: nc.scalar.copy(S_bf[:], S[:]), reads=[S], writes=[S_bf])
        p.barrier()


def host_common_b(inp, m):
    l = np.arange(128)
    tri = (l[:, None] <= l[None, :]).astype(np.float32)
    su = (l[:, None] > l[None, :]).astype(np.float32)
    ones = np.ones((128, 128), np.float32)
    idf = np.eye(128, dtype=np.float32)
    negm = np.where(l[None, :] < l[:, None], NEG, 0.0).astype(np.float32)
    m["cstB"] = np.ascontiguousarray(np.concatenate([tri, su, ones, idf, np.tile(negm, (1, 4))], axis=1))
    cw = np.asarray(inp["conv_w"])[0]
    cb = np.asarray(inp["conv_b"])[0]
    wb = np.concatenate([cw, cb[None, :]], axis=0)
    m["conv_wb"] = np.ascontiguousarray(wb.reshape(5, 32, 128).transpose(2, 1, 0)).reshape(128, 160)
    m["vec32"] = np.ascontiguousarray(np.stack([np.asarray(inp["dt_bias"])[0], np.asarray(inp["a_log"])[0], np.asarray(inp["d_skip"])[0]]))
    m["ssd_norm_w"] = np.ascontiguousarray(np.asarray(inp["ssd_norm_w"]).reshape(1, DI))
    return m


def phase_c(p, c, ps):
    nc = p.nc
    with ExitStack() as es:
        T = lambda name, shape, dt=F32: p.tile(es, name, shape, dt)
        idb = T("idbC", [128, 128], BF16)
        flag = T("flagC", [128, 2])
        bmid = T("bmid", [128, 4, 256])
        bfirst = T("bfirst", [128, 4, 256])
        qts = Rot([T(f"qt{i}", [128, 4, 128], BF16) for i in range(3)])
        kts = Rot([T(f"kt{i}", [128, 4, 256], BF16) for i in range(3)])
        vts = Rot([T(f"vt{i}", [128, 2, 512], BF16) for i in range(3)])
        scs = Rot([T(f"sc{i}", [128, 4, 256]) for i in range(2)])
        Ps = Rot([T(f"P{i}", [128, 4, 256], BF16) for i in range(2)])
        PTs = Rot([T(f"PT{i}", [128, 8, 128], BF16) for i in range(2)])
        mss = Rot([T(f"ms{i}", [128, 2, 4]) for i in range(2)])
        stg = Rot([T(f"stg{i}", [128, 4, 130]) for i in range(2)])
        psr = Rot(ps)
        p.dma("pool", idb[:], c.ident, writes=[idb])
        p.dma("sp", flag[:], c.flag, writes=[flag])

        for g in range(3):
            d = DIL[g]
            span = 128 * d
            nprevb = NPREV // span
            ncls = 4096 // span
            nown = NTOK // span
            p.dma("sp", bmid[:].rearrange("p a b -> p (a b)"), c.attn_bias[g], writes=[bmid])
            p.op("dve", lambda: nc.vector.tensor_copy(bfirst[:], bmid[:]), reads=[bmid], writes=[bfirst])
            p.op("dve", lambda: nc.vector.tensor_scalar(bfirst[:, :, 0:128], bfirst[:, :, 0:128], flag[:, 1:2], None, ALU.add),
                 reads=[bfirst, flag], writes=[bfirst])
            og = c.O_s[g].rearrange("(j r) c -> r j c", r=d)
            for r in range(d):
                for n in range(nown):
                    qt = qts.next(); kt = kts.next(); vt = vts.next(); sc = scs.next(); P = Ps.next(); PT = PTs.next()
                    ms = mss.next(); sg = stg.next()
                    pos0 = r * (NTOK // d) + n * 128
                    kpos = r * (4096 // d) + (nprevb + n - 1) * 128
                    p.dma("sp", qt[:], c.QT_s[4 * g:4 * g + 4, :, pos0:pos0 + 128].rearrange("h e q -> e h q"), writes=[qt])
                    p.dma("sp", kt[:], c.KT_s[4 * g:4 * g + 4, :, kpos:kpos + 256].rearrange("h e k -> e h k"), writes=[kt])
                    vb = r * ncls + nprevb + n - 1
                    p.dma("sp", vt[:], c.V_s[g, vb:vb + 2].rearrange("b k e -> k b e"), writes=[vt])
                    bias = bfirst if n == 0 else bmid
                    banks = [psr.next(), psr.next()]
                    for hh in range(4):
                        bk = banks[hh // 2]
                        p.op("pe", lambda: nc.tensor.matmul(bk[:, (hh % 2) * 256:(hh % 2 + 1) * 256], qt[:, hh, :], kt[:, hh, :], start=True, stop=True),
                             reads=[qt, kt], writes=[bk], mark=(hh % 2 == 1))
                    for i in range(2):
                        p.op("dve", lambda: nc.vector.tensor_tensor(sc[:, 2 * i:2 * i + 2, :], banks[i][:].rearrange("p (a b) -> p a b", a=2),
                                                                    bias[:, 2 * i:2 * i + 2, :], ALU.add), reads=[banks[i], bias], writes=[sc])
                    p.op("dve", lambda: nc.vector.reduce_max(ms[:, 0, :], sc[:], axis=AX.X), reads=[sc], writes=[ms])
                    p.op("dve", lambda: nc.vector.tensor_tensor(sc[:], sc[:], ms[:, 0, :].unsqueeze(2).broadcast_to([128, 4, 256]), ALU.subtract),
                         reads=[sc, ms], writes=[sc])
                    p.op("act", lambda: nc.scalar.activation(out=P[:], in_=sc[:], func=AF.Exp), reads=[sc], writes=[P])
                    p.op("dve", lambda: nc.vector.reduce_sum(ms[:, 1, :], P[:], axis=AX.X), reads=[P], writes=[ms])
                    bt = psr.next()
                    pb = bt[:].bitcast(BF16)
                    for hh in range(4):
                        for kb in range(2):
                            i = hh * 2 + kb
                            p.op("pe", lambda: nc.tensor.transpose(pb[:, i * 128:(i + 1) * 128], P[:, hh, kb * 128:(kb + 1) * 128], idb[:]),
                                 reads=[P, idb], writes=[bt], mark=(i == 7))
                    p.op("act", lambda: nc.scalar.copy(PT[:], pb.rearrange("p (a b) -> p a b", a=8)), reads=[bt], writes=[PT])
                    bu = psr.next()
                    for hh in range(4):
                        for kb in range(2):
                            p.op("pe", lambda: nc.tensor.matmul(bu[:, hh * 128:(hh + 1) * 128], PT[:, hh * 2 + kb, :], vt[:, kb, hh * 128:(hh + 1) * 128],
                                                                start=(kb == 0), stop=(kb == 1)), reads=[PT, vt], writes=[bu], mark=(hh == 3 and kb == 1))
                    p.op("act", lambda: nc.scalar.copy(sg[:, :, 0:128], bu[:].rearrange("p (a b) -> p a b", a=4)), reads=[bu], writes=[sg])
                    p.op("dve", lambda: nc.vector.tensor_copy(sg[:, :, 128:130], ms[:].rearrange("p a b -> p b a")), reads=[ms], writes=[sg])
                    p.dma("sp", og[r, n * 128:(n + 1) * 128, :], sg[:].rearrange("p a b -> p (a b)"), reads=[sg])
        p.barrier()
        lds = Rot([T(f"ld{i}", [128, 3, 4, 130]) for i in range(2)])
        wk = Rot([T(f"wk{i}", [128, 8, 12]) for i in range(2)])
        ob = Rot([T(f"ob{i}", [128, 4, 128]) for i in range(2)])
        o16 = Rot([T(f"o16_{i}", [128, 4, 128], BF16) for i in range(2)])
        oT = Rot([T(f"oT{i}", [128, 4, 128], BF16) for i in range(2)])
        for tt in range(16):
            ld = lds.next(); w = wk.next(); o = ob.next(); ob16 = o16.next(); ot = oT.next()
            p.dma("sp", ld[:].rearrange("p g h c -> p g (h c)"), c.O_s[:, tt * 128:(tt + 1) * 128, :].rearrange("g t c -> t g c"), writes=[ld])
            mg = ld[:, :, :, 128]
            sgm = ld[:, :, :, 129]
            M = w[:, 0, 0:4]
            p.op("dve", lambda: nc.vector.tensor_tensor(M, mg[:, 0, :], mg[:, 1, :], ALU.max), reads=[ld], writes=[w])
            p.op("dve", lambda: nc.vector.tensor_tensor(M, M, mg[:, 2, :], ALU.max), reads=[ld, w], writes=[w])
            wg3 = w[:, 1, :].rearrange("p (g h) -> p g h", g=3)
            p.op("dve", lambda: nc.vector.tensor_tensor(wg3, mg, M.unsqueeze(1).broadcast_to([128, 3, 4]), ALU.subtract), reads=[ld, w], writes=[w])
            p.op("act", lambda: nc.scalar.activation(out=w[:, 2, :], in_=w[:, 1, :], func=AF.Exp), reads=[w], writes=[w])
            e3 = w[:, 2, :].rearrange("p (g h) -> p g h", g=3)
            ws3 = w[:, 3, :].rearrange("p (g h) -> p g h", g=3)
            p.op("dve", lambda: nc.vector.tensor_tensor(ws3, e3, sgm, ALU.mult), reads=[ld, w], writes=[w])
            den = w[:, 4, 0:4]
            p.op("dve", lambda: nc.vector.tensor_tensor(den, ws3[:, 0, :], ws3[:, 1, :], ALU.add), reads=[w], writes=[w])
            p.op("dve", lambda: nc.vector.tensor_tensor(den, den, ws3[:, 2, :], ALU.add), reads=[w], writes=[w])
            p.op("dve", lambda: nc.vector.reciprocal(w[:, 5, 0:4], den), reads=[w], writes=[w])
            wn3 = w[:, 6, :].rearrange("p (g h) -> p g h", g=3)
            p.op("dve", lambda: nc.vector.tensor_tensor(wn3, e3, w[:, 5, 0:4].unsqueeze(1).broadcast_to([128, 3, 4]), ALU.mult), reads=[w], writes=[w])
            bcw = lambda gi: wn3[:, gi, :].unsqueeze(2).broadcast_to([128, 4, 128])
            p.op("dve", lambda: nc.vector.tensor_tensor(o[:], ld[:, 0, :, 0:128], bcw(0), ALU.mult), reads=[ld, w], writes=[o])
            for gi in (1, 2):
                p.op("dve", lambda: nc.vector.tensor_tensor(ld[:, gi, :, 0:128], ld[:, gi, :, 0:128], bcw(gi), ALU.mult), reads=[ld, w], writes=[ld])
                dst = o[:] if gi == 1 else ob16[:]
                p.op("dve", lambda: nc.vector.tensor_tensor(dst, o[:], ld[:, gi, :, 0:128], ALU.add), reads=[ld, o], writes=[o, ob16])
            bt = psr.next()
            pb = bt[:].bitcast(BF16)
            for hh in range(4):
                p.op("pe", lambda: nc.tensor.transpose(pb[:, hh * 128:(hh + 1) * 128], ob16[:, hh, :], idb[:]), reads=[ob16, idb], writes=[bt], mark=(hh == 3))
            p.op("act", lambda: nc.scalar.copy(ot[:], pb[:, 0:512].rearrange("p (a b) -> p a b", a=4)), reads=[bt], writes=[ot])
            p.dma("sp", c.OT_s[:, :, tt * 128:(tt + 1) * 128].rearrange("h e t -> e h t"), ot[:], reads=[ot])
        p.barrier()


def host_common_c(inp, m):
    slopes = (2.0 ** (-8.0 * np.arange(1, AH + 1) / AH)).astype(np.float32)
    q = np.arange(128)[:, None]
    k = np.arange(256)[None, :]
    rel = (q + 128) - k
    valid = (rel >= 0) & (rel <= 128)
    ab = np.zeros((3, 128, 4, 256), np.float32)
    for g in range(3):
        for hh in range(4):
            bias = -slopes[4 * g + hh] * (rel * DIL[g]).astype(np.float32)
            ab[g, :, hh, :] = np.where(valid, bias, NEG)
    m["attn_bias"] = np.ascontiguousarray(ab.reshape(3, 128, 1024))
    return m


def phase_d(p, c, ps):
    nc = p.nc
    with ExitStack() as es:
        T = lambda name, shape, dt=F32: p.tile(es, name, shape, dt)
        ynT = T("ynT_d", [128, NDC, NTOK], BF16)
        oT = T("oT_d", [128, 4, NTOK], BF16)
        wss = Rot([T(f"wss{i}", [128, NDC, 128], BF16) for i in range(2)])
        was = Rot([T(f"was{i}", [128, 4, 128], BF16) for i in range(2)])
        g0s = Rot([T(f"g0_{i}", [128, 512]) for i in range(2)])
        g1s = Rot([T(f"g1_{i}", [128, 512]) for i in range(2)])
        t1s = Rot([T(f"t1_{i}", [128, 512]) for i in range(2)])
        mgs = Rot([T(f"mg{i}", [128, 512], BF16) for i in range(2)])
        psr = Rot(ps)
        for kc in range(NDC):
            p.dma("sp", ynT[:, kc, :], c.YN_s[kc], writes=[ynT])
        for kc in range(4):
            p.dma("sp", oT[:, kc, :], c.OT_s[kc], writes=[oT])
        for dmc in range(16):
            ws = wss.next(); wa = was.next()
            p.dma("pool", ws[:].rearrange("p a b -> p (a b)"), c.w_ssd_fm[dmc], writes=[ws])
            p.dma("pool", wa[:].rearrange("p a b -> p (a b)"), c.w_attn_fm[dmc], writes=[wa])
            for st in range(4):
                ts = slice(st * 512, (st + 1) * 512)
                g0 = g0s.next(); g1 = g1s.next(); t1 = t1s.next(); mg = mgs.next()
                p.dma("sp", g0[:], c.G_s[dmc * 128:(dmc + 1) * 128, ts], writes=[g0])
                p.dma("sp", g1[:], c.G_s[D + dmc * 128:D + (dmc + 1) * 128, ts], writes=[g1])
                b1 = psr.next()
                for kc in range(NDC):
                    p.op("pe", lambda: nc.tensor.matmul(b1[:], ws[:, kc, :], ynT[:, kc, ts], start=(kc == 0), stop=(kc == NDC - 1)),
                         reads=[ws, ynT], writes=[b1], mark=(kc == NDC - 1))
                b2 = psr.next()
                for kc in range(4):
                    p.op("pe", lambda: nc.tensor.matmul(b2[:], wa[:, kc, :], oT[:, kc, ts], start=(kc == 0), stop=(kc == 3)),
                         reads=[wa, oT], writes=[b2], mark=(kc == 3))
                p.op("dve", lambda: nc.vector.tensor_tensor(t1[:], g0[:], b1[:], ALU.mult), reads=[g0, b1], writes=[t1])
                p.op("dve", lambda: nc.vector.tensor_tensor(g1[:], g1[:], b2[:], ALU.mult), reads=[g1, b2], writes=[g1])
                p.op("dve", lambda: nc.vector.tensor_tensor(mg[:], t1[:], g1[:], ALU.add), reads=[t1, g1], writes=[mg])
                p.dma("sp", c.M_s[dmc][:, ts], mg[:], reads=[mg])
        p.barrier()
    with ExitStack() as es:
        T = lambda name, shape, dt=F32: p.tile(es, name, shape, dt)
        mT = T("mT_d", [128, NDC, NTOK], BF16)
        wos = Rot([T(f"wo{i}", [128, NDC, 512], BF16) for i in range(2)])
        xss = Rot([T(f"xs_d{i}", [128, 512]) for i in range(3)])
        psr = Rot(ps)
        for kc in range(NDC):
            p.dma("sp", mT[:, kc, :], c.M_s[kc], writes=[mT])
        for ct in range(4):
            wo = wos.next()
            p.dma("pool", wo[:].rearrange("p a b -> p (a b)"), c.w_out_tm[ct], writes=[wo])
            for tt in range(16):
                xs_ = xss.next()
                p.dma("sp", xs_[:], c.x_own[tt * 128:(tt + 1) * 128, ct * 512:(ct + 1) * 512], writes=[xs_])
                bk = psr.next()
                for kc in range(NDC):
                    p.op("pe", lambda: nc.tensor.matmul(bk[:], mT[:, kc, tt * 128:(tt + 1) * 128], wo[:, kc, :], start=(kc == 0), stop=(kc == NDC - 1)),
                         reads=[mT, wo], writes=[bk], mark=(kc == NDC - 1))
                p.op("dve", lambda: nc.vector.tensor_tensor(xs_[:], xs_[:], bk[:], ALU.add), reads=[xs_, bk], writes=[xs_])
                p.dma("sp", c.X2_s[tt * 128:(tt + 1) * 128, ct * 512:(ct + 1) * 512], xs_[:], reads=[xs_])
        p.barrier()


def phase_e0(p, c, ps):
    nc = p.nc
    with ExitStack() as es:
        T = lambda name, shape, dt=F32: p.tile(es, name, shape, dt)
        idf = T("idf_e", [128, 128])
        fw = T("fw_e", [128, D])
        wr = T("wr_e", [128, NDC, 36])
        br = T("br_e", [128, 36])
        x2s = Rot([T(f"x2_{i}", [128, D]) for i in range(2)])
        hns = Rot([T(f"hn_{i}", [128, D]) for i in range(2)])
        junk = T("junk_e", [128, D], BF16)
        st4 = Rot([T(f"st4e_{i}", [128, 4]) for i in range(2)])
        h32 = Rot([T(f"h32_{i}", [128, NDC, 128]) for i in range(2)])
        h16 = Rot([T(f"h16_{i}", [128, NDC, 128], BF16) for i in range(2)])
        rt = Rot([T(f"rt{i}", [128, 8, 36]) for i in range(2)])
        sc = Rot([T(f"rs{i}", [128, 16]) for i in range(2)])
        gt = Rot([T(f"gt{i}", [128, NE]) for i in range(2)])
        psr = Rot(ps)
        p.dma("sp", idf[:], c.ident, writes=[idf])
        p.dma("sp", fw[:], c.ffn_norm_w.partition_broadcast(128), writes=[fw])
        p.dma("sp", wr[:].rearrange("p a b -> p (a b)"), c.w_router, writes=[wr])
        p.dma("sp", br[:], c.b_router.partition_broadcast(128), writes=[br])
        for tt in range(16):
            x2 = x2s.next(); hn = hns.next(); s4 = st4.next(); a32 = h32.next(); a16 = h16.next()
            R = rt.next(); S = sc.next(); G = gt.next()
            p.dma("sp", x2[:], c.X2_s[tt * 128:(tt + 1) * 128, :], writes=[x2])
            p.op("act", lambda: nc.scalar.activation(out=junk[:], in_=x2[:], func=AF.Square, accum_out=s4[:, 0:1]), reads=[x2], writes=[junk, s4])
            p.op("dve", lambda: nc.vector.tensor_scalar(s4[:, 1:2], s4[:, 0:1], 1.0 / D, EPS, ALU.mult, ALU.add), reads=[s4], writes=[s4])
            p.op("act", lambda: nc.scalar.activation(out=s4[:, 2:3], in_=s4[:, 1:2], func=AF.Ln), reads=[s4], writes=[s4])
            p.op("act", lambda: nc.scalar.activation(out=s4[:, 3:4], in_=s4[:, 2:3], func=AF.Exp, scale=-0.5), reads=[s4], writes=[s4])
            p.op("dve", lambda: nc.vector.scalar_tensor_tensor(hn[:], x2[:], s4[:, 3:4], fw[:], ALU.mult, ALU.mult), reads=[x2, s4, fw], writes=[hn])
            for q4 in range(4):
                bk = psr.next()
                for k in range(4):
                    dc = q4 * 4 + k
                    p.op("pe", lambda: nc.tensor.transpose(bk[:, k * 128:(k + 1) * 128], hn[:, dc * 128:(dc + 1) * 128], idf[:]),
                         reads=[hn, idf], writes=[bk], mark=(k == 3))
                p.op("act", lambda: nc.scalar.copy(a32[:, q4 * 4:q4 * 4 + 4, :], bk[:].rearrange("p (a b) -> p a b", a=4)), reads=[bk], writes=[a32])
                p.op("dve", lambda: nc.vector.tensor_copy(a16[:, q4 * 4:q4 * 4 + 4, :], bk[:].rearrange("p (a b) -> p a b", a=4)), reads=[bk], writes=[a16])
            p.dma("sp", c.HN_s[:, :, tt * 128:(tt + 1) * 128].rearrange("a p t -> p a t"), a16[:], reads=[a16])
            bk = psr.next()
            for kc in range(NDC):
                p.op("pe", lambda: nc.tensor.matmul(bk[:, 0:36], a32[:, kc, :], wr[:, kc, :], start=(kc == 0), stop=(kc == NDC - 1)),
                     reads=[a32, wr], writes=[bk], mark=(kc == NDC - 1))
            L = R[:, 0, :]
            dv = lambda fn, reads, writes: p.op("dve", fn, reads=reads, writes=writes)
            dv(lambda: nc.vector.tensor_tensor(L, bk[:, 0:36], br[:], ALU.add), [bk, br], [R])
            gl = R[:, 0, 0:4]
            el = R[:, 0, 4:36]
            dv(lambda: nc.vector.reduce_max(S[:, 0:1], gl, axis=AX.X), [R], [S])
            dv(lambda: nc.vector.tensor_scalar(R[:, 1, 0:4], gl, S[:, 0:1], None, ALU.subtract), [R, S], [R])
            p.op("act", lambda: nc.scalar.activation(out=R[:, 1, 4:8], in_=R[:, 1, 0:4], func=AF.Exp, accum_out=S[:, 1:2]), reads=[R], writes=[R, S])
            dv(lambda: nc.vector.reciprocal(S[:, 2:3], S[:, 1:2]), [S], [S])
            dv(lambda: nc.vector.tensor_scalar(R[:, 1, 8:12], gl, S[:, 0:1], None, ALU.is_equal), [R, S], [R])
            dv(lambda: nc.vector.tensor_scalar(R[:, 1, 12:16], R[:, 1, 8:12], -NEG, NEG, ALU.mult, ALU.add), [R], [R])
            elm = R[:, 2, 0:32]
            dv(lambda: nc.vector.tensor_tensor(elm.rearrange("p (g e) -> p g e", g=4), el.rearrange("p (g e) -> p g e", g=4),
                                               R[:, 1, 12:16].unsqueeze(2).broadcast_to([128, 4, 8]), ALU.add), [R], [R])
            dv(lambda: nc.vector.reduce_max(S[:, 3:4], elm, axis=AX.X), [R], [S])
            oh1 = R[:, 3, 0:32]
            dv(lambda: nc.vector.tensor_scalar(oh1, elm, S[:, 3:4], None, ALU.is_equal), [R, S], [R])
            elm2 = R[:, 4, 0:32]
            dv(lambda: nc.vector.scalar_tensor_tensor(elm2, oh1, NEG, elm, ALU.mult, ALU.add), [R], [R])
            dv(lambda: nc.vector.reduce_max(S[:, 4:5], elm2, axis=AX.X), [R], [S])
            oh2 = R[:, 5, 0:32]
            dv(lambda: nc.vector.tensor_scalar(oh2, elm2, S[:, 4:5], None, ALU.is_equal), [R, S], [R])
            dv(lambda: nc.vector.tensor_tensor(S[:, 5:6], S[:, 4:5], S[:, 3:4], ALU.subtract), [S], [S])
            p.op("act", lambda: nc.scalar.activation(out=S[:, 6:7], in_=S[:, 5:6], func=AF.Exp), reads=[S], writes=[S])
            dv(lambda: nc.vector.tensor_scalar(S[:, 7:8], S[:, 6:7], 1.0, None, ALU.add), [S], [S])
            dv(lambda: nc.vector.reciprocal(S[:, 8:9], S[:, 7:8]), [S], [S])
            dv(lambda: nc.vector.tensor_tensor(S[:, 9:10], S[:, 8:9], S[:, 2:3], ALU.mult), [S], [S])
            dv(lambda: nc.vector.tensor_tensor(S[:, 10:11], S[:, 9:10], S[:, 6:7], ALU.mult), [S], [S])
            dv(lambda: nc.vector.tensor_scalar(G[:], oh1, S[:, 9:10], None, ALU.mult), [R, S], [G])
            dv(lambda: nc.vector.scalar_tensor_tensor(G[:], oh2, S[:, 10:11], G[:], ALU.mult, ALU.add), [R, S, G], [G])
            p.dma("sp", c.GATE_s[tt * 128:(tt + 1) * 128, :], G[:], reads=[G])
        p.barrier()


def phase_e(p, c, ps):
    nc = p.nc
    NP = 1024
    with ExitStack() as es:
        T = lambda name, shape, dt=F32: p.tile(es, name, shape, dt)
        fnw = T("fnw", [128, D])
        gates = T("gates", [128, 16, NE])
        hnT = T("hnT_e", [128, NDC, NP], BF16)
        yacc = T("yacc", [128, NP // 128, D])
        wgs = Rot([T(f"wg{i}", [128, NDC, 256], BF16) for i in range(2)])
        wus = Rot([T(f"wu{i}", [128, NDC, 256], BF16) for i in range(2)])
        wds = Rot([T(f"wd{i}", [128, 2, D], BF16) for i in range(2)])
        sils = Rot([T(f"sil{i}", [128, 512]) for i in range(2)])
        hms = Rot([T(f"hm{i}", [128, 2, NP], BF16) for i in range(2)])
        junk = T("junk_m", [128, D], BF16)
        st4 = Rot([T(f"st4m_{i}", [128, 4]) for i in range(2)])
        psr = Rot(ps)
        p.dma("sp", fnw[:], c.final_norm_w.partition_broadcast(128), writes=[fnw])
        p.dma("sp", gates[:], c.GATE_s.rearrange("(t p) e -> p t e", p=128), writes=[gates])
        for ps_i in range(NTOK // NP):
            t0 = ps_i * NP
            for kc in range(NDC):
                p.dma("sp", hnT[:, kc, :], c.HN_s[kc][:, t0:t0 + NP], writes=[hnT])
            for t in range(NP // 128):
                p.dma("sp", yacc[:, t, :], c.X2_s[t0 + t * 128:t0 + (t + 1) * 128, :], writes=[yacc])
            for e in range(NE):
                for fc in range(4):
                    wg = wgs.next(); wu = wus.next(); wd = wds.next(); hm = hms.next()
                    p.dma("pool", wg[:].rearrange("p a b -> p (a b)"), c.wg_t[e, fc], writes=[wg])
                    p.dma("pool", wu[:].rearrange("p a b -> p (a b)"), c.wu_t[e, fc], writes=[wu])
                    p.dma("pool", wd[:].rearrange("p a b -> p (a b)"), c.wd_t[e, fc], writes=[wd])
                    for j in range(2):
                        for ts in range(NP // 512):
                            tsl = slice(ts * 512, (ts + 1) * 512)
                            bg = psr.next(); bu = psr.next(); sil = sils.next()
                            for kc in range(NDC):
                                p.op("pe", lambda: nc.tensor.matmul(bg[:], wg[:, kc, j * 128:(j + 1) * 128], hnT[:, kc, tsl], start=(kc == 0), stop=(kc == NDC - 1)),
                                     reads=[wg, hnT], writes=[bg], mark=(kc == NDC - 1))
                            for kc in range(NDC):
                                p.op("pe", lambda: nc.tensor.matmul(bu[:], wu[:, kc, j * 128:(j + 1) * 128], hnT[:, kc, tsl], start=(kc == 0), stop=(kc == NDC - 1)),
                                     reads=[wu, hnT], writes=[bu], mark=(kc == NDC - 1))
                            p.op("act", lambda: nc.scalar.activation(out=sil[:], in_=bg[:], func=AF.Silu), reads=[bg], writes=[sil])
                            p.op("dve", lambda: nc.vector.tensor_tensor(hm[:, j, tsl], sil[:], bu[:], ALU.mult), reads=[sil, bu], writes=[hm])
                    for t in range(NP // 128):
                        gcol = gates[:, ps_i * (NP // 128) + t, e:e + 1]
                        for dtile in range(4):
                            dsl = slice(dtile * 512, (dtile + 1) * 512)
                            bk = psr.next()
                            for j in range(2):
                                p.op("pe", lambda: nc.tensor.matmul(bk[:], hm[:, j, t * 128:(t + 1) * 128], wd[:, j, dsl], start=(j == 0), stop=(j == 1)),
                                     reads=[hm, wd], writes=[bk], mark=(j == 1))
                            p.op("dve", lambda: nc.vector.scalar_tensor_tensor(yacc[:, t, dsl], bk[:], gcol, yacc[:, t, dsl], ALU.mult, ALU.add),
                                 reads=[bk, gates, yacc], writes=[yacc])
            for t in range(NP // 128):
                s4 = st4.next()
                p.op("act", lambda: nc.scalar.activation(out=junk[:], in_=yacc[:, t, :], func=AF.Square, accum_out=s4[:, 0:1]), reads=[yacc], writes=[junk, s4])
                p.op("dve", lambda: nc.vector.tensor_scalar(s4[:, 1:2], s4[:, 0:1], 1.0 / D, EPS, ALU.mult, ALU.add), reads=[s4], writes=[s4])
                p.op("act", lambda: nc.scalar.activation(out=s4[:, 2:3], in_=s4[:, 1:2], func=AF.Ln), reads=[s4], writes=[s4])
                p.op("act", lambda: nc.scalar.activation(out=s4[:, 3:4], in_=s4[:, 2:3], func=AF.Exp, scale=-0.5), reads=[s4], writes=[s4])
                p.op("dve", lambda: nc.vector.scalar_tensor_tensor(yacc[:, t, :], yacc[:, t, :], s4[:, 3:4], fnw[:], ALU.mult, ALU.mult),
                     reads=[yacc, s4, fnw], writes=[yacc])
                p.dma("sp", c.out[t0 + t * 128:t0 + (t + 1) * 128, :], yacc[:, t, :], reads=[yacc])
        p.barrier()


def host_common_de(inp, m):
    m["w_ssd_fm"] = tile_w(np.asarray(inp["w_ssd_out"])[0], 128)
    m["w_attn_fm"] = tile_w(np.asarray(inp["w_attn_out"])[0], 128)
    m["w_out_tm"] = tile_w(np.asarray(inp["w_out"])[0], 512)
    m["ffn_norm_w"] = np.ascontiguousarray(np.asarray(inp["ffn_norm_w"]).reshape(1, D))
    wr = np.concatenate([np.asarray(inp["w_group_router"])[0], np.asarray(inp["w_expert_router"])[0]], axis=1)
    m["w_router"] = tile_w(wr, 36)[0]
    m["b_router"] = np.ascontiguousarray(np.concatenate([np.asarray(inp["b_group_router"])[0], np.asarray(inp["b_expert_router"])[0]]).reshape(1, 36))
    m["final_norm_w"] = np.ascontiguousarray(np.asarray(inp["final_norm_w"]).reshape(1, D))
    return m


def build_program(debug=False, phases="abcdeE"):
    nc = bass.Bass("TRN2", target_bir_lowering=False)
    c = declare_io(nc, debug=debug, phases=phases)
    with ExitStack() as es:
        p = Prog(nc, es)
        ps = [Tile(f"ps{i}", es.enter_context(nc.psum_tensor(f"psum{i}", [128, 512], F32))) for i in range(8)]
        for b in ps:
            b.excl = True
        if "a" in phases:
            phase_a(p, c, ps)
        if "b" in phases:
            phase_b(p, c, ps)
        if "c" in phases:
            phase_c(p, c, ps)
        if "d" in phases:
            phase_d(p, c, ps)
        rt_ = route_tiles(p, es)
        if "e" in phases:
            phase_e0s(p, c, ps, rt_)
            phase_e1(p, c, ps, rt_)
        if "E" in phases:
            phase_es(p, c, ps, rt_)
            phase_ec(p, c, ps, rt_)
        p.barrier()
    return nc, p


def host_maps(inp):
    m = host_common(inp)
    host_common_b(inp, m)
    host_common_c(inp, m)
    host_common_de(inp, m)
    host_common_s(inp, m)
    x = np.asarray(inp["x"])
    maps = []
    zeros = np.zeros((NPREV, D), np.float32)
    for core in range(8):
        b, half = core // 2, core % 2
        mm = dict(m)
        mm["x_own"] = np.ascontiguousarray(x[b, half * NTOK:(half + 1) * NTOK])
        mm["x_prev"] = np.ascontiguousarray(x[b, 0:NPREV]) if half == 1 else zeros
        fl = np.array([[1.0, 0.0]], np.float32) if half == 1 else np.array([[0.0, NEG]], np.float32)
        mm["flag"] = np.ascontiguousarray(np.tile(fl, (128, 1)))
        maps.append(mm)
    return maps


_PROG = {}


def kernel(**inputs):
    if "nc" not in _PROG:
        _PROG["nc"], _ = build_program()
    nc = _PROG["nc"]
    maps = host_maps(inputs)
    res = run_bass_kernel_spmd(nc, maps, core_ids=list(range(8)))
    x = np.asarray(inputs["x"])
    out = np.empty(x.shape, np.float32)
    for core in range(8):
        b, half = core // 2, core % 2
        out[b, half * NTOK:(half + 1) * NTOK] = np.asarray(res.results[core]["out"], np.float32)
    return out


def route_tiles(p, es):
    r = Ctx()
    T = lambda name, shape, dt=F32: p.tile(es, name, shape, dt)
    r.OH1 = T("OH1", [128, 16, NE])
    r.OH2 = T("OH2", [128, 16, NE])
    r.PG = T("PG", [128, 16, 2])
    r.POSI = T("POSI", [128, 2, 16], I32)
    r.IDXI = T("IDXI", [128, NSLOT], I32)
    return r


def phase_e0s(p, c, ps, rt_):
    nc = p.nc
    with ExitStack() as es:
        T = lambda name, shape, dt=F32: p.tile(es, name, shape, dt)
        idf = T("idf_e", [128, 128])
        fw = T("fw_e", [128, D])
        wr = T("wr_e", [128, NDC, 36])
        br = T("br_e", [128, 36])
        zt = T("zt_e", [128, D], BF16)
        x2s = Rot([T(f"x2_{i}", [128, D]) for i in range(2)])
        hns = Rot([T(f"hn_{i}", [128, D]) for i in range(2)])
        hbs = Rot([T(f"hb_{i}", [128, D], BF16) for i in range(2)])
        junk = T("junk_e", [128, D], BF16)
        st4 = Rot([T(f"st4e_{i}", [128, 4]) for i in range(2)])
        h32 = Rot([T(f"h32_{i}", [128, NDC, 128]) for i in range(2)])
        rt = Rot([T(f"rt{i}", [128, 8, 36]) for i in range(2)])
        sc = Rot([T(f"rs{i}", [128, 16]) for i in range(2)])
        psr = Rot(ps)
        p.dma("sp", idf[:], c.ident, writes=[idf])
        p.dma("sp", fw[:], c.ffn_norm_w.partition_broadcast(128), writes=[fw])
        p.dma("sp", wr[:].rearrange("p a b -> p (a b)"), c.w_router, writes=[wr])
        p.dma("sp", br[:], c.b_router.partition_broadcast(128), writes=[br])
        p.op("dve", lambda: nc.vector.memset(zt[:], 0.0), writes=[zt])
        for i in range(NSLOT):
            p.dma("sp", c.XS_s[i * 128:(i + 1) * 128, :], zt[:], reads=[zt])
        for tt in range(16):
            x2 = x2s.next(); hn = hns.next(); hb = hbs.next(); s4 = st4.next(); a32 = h32.next()
            R = rt.next(); S = sc.next()
            p.dma("sp", x2[:], c.X2_s[tt * 128:(tt + 1) * 128, :], writes=[x2])
            p.op("act", lambda: nc.scalar.activation(out=junk[:], in_=x2[:], func=AF.Square, accum_out=s4[:, 0:1]), reads=[x2], writes=[junk, s4])
            p.op("dve", lambda: nc.vector.tensor_scalar(s4[:, 1:2], s4[:, 0:1], 1.0 / D, EPS, ALU.mult, ALU.add), reads=[s4], writes=[s4])
            p.op("act", lambda: nc.scalar.activation(out=s4[:, 2:3], in_=s4[:, 1:2], func=AF.Ln), reads=[s4], writes=[s4])
            p.op("act", lambda: nc.scalar.activation(out=s4[:, 3:4], in_=s4[:, 2:3], func=AF.Exp, scale=-0.5), reads=[s4], writes=[s4])
            p.op("dve", lambda: nc.vector.scalar_tensor_tensor(hn[:], x2[:], s4[:, 3:4], fw[:], ALU.mult, ALU.mult), reads=[x2, s4, fw], writes=[hn])
            p.op("act", lambda: nc.scalar.copy(hb[:], hn[:]), reads=[hn], writes=[hb])
            p.dma("sp", c.HNTM_s[tt * 128:(tt + 1) * 128, :], hb[:], reads=[hb])
            for q4 in range(4):
                bk = psr.next()
                for k in range(4):
                    dc = q4 * 4 + k
                    p.op("pe", lambda: nc.tensor.transpose(bk[:, k * 128:(k + 1) * 128], hn[:, dc * 128:(dc + 1) * 128], idf[:]),
                         reads=[hn, idf], writes=[bk], mark=(k == 3))
                if q4 % 2 == 0:
                    p.op("act", lambda: nc.scalar.copy(a32[:, q4 * 4:q4 * 4 + 4, :], bk[:].rearrange("p (a b) -> p a b", a=4)), reads=[bk], writes=[a32])
                else:
                    p.op("dve", lambda: nc.vector.tensor_copy(a32[:, q4 * 4:q4 * 4 + 4, :], bk[:].rearrange("p (a b) -> p a b", a=4)), reads=[bk], writes=[a32])
            bk = psr.next()
            for kc in range(NDC):
                p.op("pe", lambda: nc.tensor.matmul(bk[:, 0:36], a32[:, kc, :], wr[:, kc, :], start=(kc == 0), stop=(kc == NDC - 1)),
                     reads=[a32, wr], writes=[bk], mark=(kc == NDC - 1))
            L = R[:, 0, :]
            dv = lambda fn, reads, writes: p.op("dve", fn, reads=reads, writes=writes)
            dv(lambda: nc.vector.tensor_tensor(L, bk[:, 0:36], br[:], ALU.add), [bk, br], [R])
            gl = R[:, 0, 0:4]
            el = R[:, 0, 4:36]
            dv(lambda: nc.vector.reduce_max(S[:, 0:1], gl, axis=AX.X), [R], [S])
            dv(lambda: nc.vector.tensor_scalar(R[:, 1, 0:4], gl, S[:, 0:1], None, ALU.subtract), [R, S], [R])
            p.op("act", lambda: nc.scalar.activation(out=R[:, 1, 4:8], in_=R[:, 1, 0:4], func=AF.Exp, accum_out=S[:, 1:2]), reads=[R], writes=[R, S])
            dv(lambda: nc.vector.reciprocal(S[:, 2:3], S[:, 1:2]), [S], [S])
            dv(lambda: nc.vector.tensor_scalar(R[:, 1, 8:12], gl, S[:, 0:1], None, ALU.is_equal), [R, S], [R])
            dv(lambda: nc.vector.tensor_scalar(R[:, 1, 12:16], R[:, 1, 8:12], -NEG, NEG, ALU.mult, ALU.add), [R], [R])
            elm = R[:, 2, 0:32]
            dv(lambda: nc.vector.tensor_tensor(elm.rearrange("p (g e) -> p g e", g=4), el.rearrange("p (g e) -> p g e", g=4),
                                               R[:, 1, 12:16].unsqueeze(2).broadcast_to([128, 4, 8]), ALU.add), [R], [R])
            dv(lambda: nc.vector.reduce_max(S[:, 3:4], elm, axis=AX.X), [R], [S])
            oh1 = rt_.OH1[:, tt, :]
            dv(lambda: nc.vector.tensor_scalar(oh1, elm, S[:, 3:4], None, ALU.is_equal), [R, S], [rt_.OH1])
            elm2 = R[:, 4, 0:32]
            dv(lambda: nc.vector.scalar_tensor_tensor(elm2, oh1, NEG, elm, ALU.mult, ALU.add), [R, rt_.OH1], [R])
            dv(lambda: nc.vector.reduce_max(S[:, 4:5], elm2, axis=AX.X), [R], [S])
            oh2 = rt_.OH2[:, tt, :]
            dv(lambda: nc.vector.tensor_scalar(oh2, elm2, S[:, 4:5], None, ALU.is_equal), [R, S], [rt_.OH2])
            dv(lambda: nc.vector.tensor_tensor(S[:, 5:6], S[:, 4:5], S[:, 3:4], ALU.subtract), [S], [S])
            p.op("act", lambda: nc.scalar.activation(out=S[:, 6:7], in_=S[:, 5:6], func=AF.Exp), reads=[S], writes=[S])
            dv(lambda: nc.vector.tensor_scalar(S[:, 7:8], S[:, 6:7], 1.0, None, ALU.add), [S], [S])
            dv(lambda: nc.vector.reciprocal(S[:, 8:9], S[:, 7:8]), [S], [S])
            dv(lambda: nc.vector.tensor_tensor(rt_.PG[:, tt, 0:1], S[:, 8:9], S[:, 2:3], ALU.mult), [S], [rt_.PG])
            dv(lambda: nc.vector.tensor_tensor(rt_.PG[:, tt, 1:2], rt_.PG[:, tt, 0:1], S[:, 6:7], ALU.mult), [S, rt_.PG], [rt_.PG])
        p.barrier()


def phase_e1(p, c, ps, rt_):
    nc = p.nc
    with ExitStack() as es:
        T = lambda name, shape, dt=F32: p.tile(es, name, shape, dt)
        cst = T("cstE", [128, 328])
        SUTR, ONES, THR, C8 = cst[:, 0:128], cst[:, 128:256], cst[:, 256:320], cst[:, 320:328]
        OHS = T("OHS", [128, 16, NE])
        CUM = T("CUM", [128, 17, NE])
        RK = T("RK", [128, 16, NE])
        W = T("W_e1", [128, 12, NE])
        CMP = T("CMP", [128, NSLOT, NE])
        ET = T("ET", [128, 2, NSLOT])
        IDXF = T("IDXF", [128, NSLOT])
        TT = T("TT_e1", [128, 16, NE])
        MM = T("MM_e1", [128, 16, NE])
        POSF = T("POSF", [128, 2, 16])
        hbs = Rot([T(f"hb1_{i}", [128, D], BF16) for i in range(3)])
        psr = Rot(ps)
        dv = lambda fn, reads, writes: p.op("dve", fn, reads=reads, writes=writes)
        p.dma("sp", cst[:], c.cstE, writes=[cst])
        dv(lambda: nc.vector.tensor_tensor(OHS[:], rt_.OH1[:], rt_.OH2[:], ALU.add), [rt_.OH1, rt_.OH2], [OHS])
        dv(lambda: nc.vector.memset(CUM[:, 0, :], 0.0), [], [CUM])
        for i in range(16):
            dv(lambda: nc.vector.tensor_tensor(CUM[:, i + 1, :], CUM[:, i, :], OHS[:, i, :], ALU.add), [CUM, OHS], [CUM])
        for i in range(16):
            bk = psr.next()
            p.op("pe", lambda: nc.tensor.matmul(bk[:, 0:NE], SUTR, OHS[:, i, :], start=True, stop=False), reads=[cst, OHS], writes=[bk], mark=False)
            p.op("pe", lambda: nc.tensor.matmul(bk[:, 0:NE], ONES, CUM[:, i, :], start=False, stop=True), reads=[cst, CUM], writes=[bk])
            p.op("act", lambda: nc.scalar.copy(RK[:, i, :], bk[:, 0:NE]), reads=[bk], writes=[RK])
        bk = psr.next()
        p.op("pe", lambda: nc.tensor.matmul(bk[:, 0:NE], ONES, CUM[:, 16, :], start=True, stop=True), reads=[cst, CUM], writes=[bk])
        cnt, r_, pf, pad, off = W[:, 0, :], W[:, 1, :], W[:, 2, :], W[:, 3, :], W[:, 6, :]
        p.op("act", lambda: nc.scalar.copy(cnt, bk[:, 0:NE]), reads=[bk], writes=[W])
        C2 = CMP[:, 0:NE, 0:16]
        dv(lambda: nc.vector.tensor_tensor(C2, cnt.unsqueeze(2).broadcast_to([128, NE, 16]), THR[:, 0:16].unsqueeze(1).broadcast_to([128, NE, 16]), ALU.is_gt),
           [W, cst], [CMP])
        dv(lambda: nc.vector.reduce_sum(pf, C2, axis=AX.X), [CMP], [W])
        dv(lambda: nc.vector.tensor_scalar(pad, pf, 128.0, None, ALU.mult), [W], [W])
        a, b = 4, 5
        dv(lambda: nc.vector.tensor_copy(W[:, a, :], pad), [W], [W])
        for sft in (1, 2, 4, 8, 16):
            dv(lambda: nc.vector.tensor_copy(W[:, b, 0:sft], W[:, a, 0:sft]), [W], [W])
            dv(lambda: nc.vector.tensor_tensor(W[:, b, sft:NE], W[:, a, sft:NE], W[:, a, 0:NE - sft], ALU.add), [W], [W])
            a, b = b, a
        END = W[:, a, :]
        dv(lambda: nc.vector.tensor_tensor(off, END, pad, ALU.subtract), [W], [W])
        dv(lambda: nc.vector.tensor_tensor(CMP[:], END.unsqueeze(1).broadcast_to([128, NSLOT, NE]), THR.unsqueeze(2).broadcast_to([128, NSLOT, NE]), ALU.is_le),
           [W, cst], [CMP])
        dv(lambda: nc.vector.reduce_sum(ET[:, 0, :], CMP[:], axis=AX.X), [CMP], [ET])
        dv(lambda: nc.vector.tensor_scalar(ET[:, 1, :], ET[:, 0, :], float(NE - 1), 128.0, ALU.min, ALU.mult), [ET], [ET])
        dv(lambda: nc.vector.tensor_scalar(IDXF[:], ET[:, 1, :], C8[:, 0:1], None, ALU.add), [ET, cst], [IDXF])
        dv(lambda: nc.vector.tensor_copy(rt_.IDXI[:], IDXF[:]), [IDXF], [rt_.IDXI])
        dv(lambda: nc.vector.tensor_tensor(TT[:], RK[:], off.unsqueeze(1).broadcast_to([128, 16, NE]), ALU.add), [RK, W], [TT])
        for k, OH in enumerate((rt_.OH1, rt_.OH2)):
            dv(lambda: nc.vector.tensor_tensor(MM[:], TT[:], OH[:], ALU.mult), [TT, OH], [MM])
            dv(lambda: nc.vector.reduce_sum(POSF[:, k, :], MM[:], axis=AX.X), [MM], [POSF])
        dv(lambda: nc.vector.tensor_copy(rt_.POSI[:], POSF[:]), [POSF], [rt_.POSI])
        xsb = Buf("XS_s")
        for tt in range(16):
            hb = hbs.next()
            p.dma("sp", hb[:], c.HNTM_s[tt * 128:(tt + 1) * 128, :], writes=[hb])
            for k in range(2):
                p.idma(c.XS_s, bass.IndirectOffsetOnAxis(rt_.POSI[:, k, tt:tt + 1], 0), hb[:], None, reads=[hb, rt_.POSI], writes=[xsb])
        p.barrier()


def phase_es(p, c, ps, rt_):
    nc = p.nc
    with ExitStack() as es:
        T = lambda name, shape, dt=F32: p.tile(es, name, shape, dt)
        idb = T("idb_s", [128, 128], BF16)
        xss = Rot([T(f"xs_s{i}", [128, D], BF16) for i in range(2)])
        xTs = Rot([T(f"xT_s{i}", [128, NDC, 128], BF16) for i in range(2)])
        units = Rot([T(f"wU{i}", [128, 3, 4096], BF16) for i in range(5)])
        sils = Rot([T(f"silS{i}", [128, 256]) for i in range(2)])
        hms = Rot([T(f"hmS{i}", [128, 8, 128], BF16) for i in range(2)])
        yts = Rot([T(f"ytS{i}", [128, D]) for i in range(2)])
        ybanks = ps[0:4]
        psr = Rot(ps[4:8])
        p.dma("pool", idb[:], c.ident, writes=[idb])
        precast_step(p, c, 1000)
        for tk in c.precast_toks:
            p._wait("pool", tk)
        pending = []

        def down(i, fc, w, hm, yt):
            for dtile in range(4):
                dsl = slice(dtile * 512, (dtile + 1) * 512)
                for j in range(2):
                    p.op("pe", lambda: nc.tensor.matmul(ybanks[dtile][:], hm[:, fc * 2 + j, :], w[:, 2, :].rearrange("p (a b) -> p a b", a=2)[:, j, dsl],
                                                        start=(fc == 0 and j == 0), stop=(fc == 3 and j == 1)),
                         reads=[hm, w], writes=[ybanks[dtile]], mark=(dtile == 3 and j == 1))
            if fc == 3:
                for dtile in range(4):
                    dsl = slice(dtile * 512, (dtile + 1) * 512)
                    if dtile % 2 == 0:
                        p.op("act", lambda: nc.scalar.copy(yt[:, dsl], ybanks[dtile][:]), reads=[ybanks[dtile]], writes=[yt])
                    else:
                        p.op("dve", lambda: nc.vector.tensor_copy(yt[:, dsl], ybanks[dtile][:]), reads=[ybanks[dtile]], writes=[yt])
                p.dma("sp", c.YP_s[i * 128:(i + 1) * 128, :], yt[:], reads=[yt])

        for i in range(NSLOT):
            xs_ = xss.next(); xT = xTs.next(); hm = hms.next(); yt = yts.next()
            p.dma("sp", xs_[:], c.XS_s[i * 128:(i + 1) * 128, :], writes=[xs_])
            for half in range(2):
                bank = psr.next()
                pb = bank[:].bitcast(BF16)
                for k in range(8):
                    dc = half * 8 + k
                    p.op("pe", lambda: nc.tensor.transpose(pb[:, k * 128:(k + 1) * 128], xs_[:, dc * 128:(dc + 1) * 128], idb[:]),
                         reads=[xs_, idb], writes=[bank], mark=(k == 7))
                if half == 0:
                    p.op("act", lambda: nc.scalar.copy(xT[:, 0:8, :], pb.rearrange("p (a b) -> p a b", a=8)), reads=[bank], writes=[xT])
                else:
                    p.op("dve", lambda: nc.vector.tensor_copy(xT[:, 8:16, :], pb.rearrange("p (a b) -> p a b", a=8)), reads=[bank], writes=[xT])
            for fc in range(4):
                w = units.next(); sil = sils.next()
                off = bass.IndirectOffsetOnAxis(rt_.IDXI[:, i:i + 1], 0)
                p.idma(w[:].rearrange("p a b -> p (a b)"), None, c.WB_s[fc], off, reads=[rt_.IDXI], writes=[w])
                bg = psr.next(); bu = psr.next()
                for (bank, m_) in ((bg, 0), (bu, 1)):
                    for j in range(2):
                        for kc in range(NDC):
                            lhs = w[:, m_, :].rearrange("p (h a b) -> p h a b", h=2, a=8)[:, kc // 8, kc % 8, j * 128:(j + 1) * 128]
                            p.op("pe", lambda: nc.tensor.matmul(bank[:, j * 128:(j + 1) * 128], lhs, xT[:, kc, :], start=(kc == 0), stop=(kc == NDC - 1)),
                                 reads=[w, xT], writes=[bank], mark=(kc == NDC - 1 and j == 1))
                p.op("act", lambda: nc.scalar.activation(out=sil[:], in_=bg[:, 0:256], func=AF.Silu), reads=[bg], writes=[sil])
                p.op("dve", lambda: nc.vector.tensor_tensor(hm[:, fc * 2:fc * 2 + 2, :], sil[:].rearrange("p (a b) -> p a b", a=2),
                                                            bu[:, 0:256].rearrange("p (a b) -> p a b", a=2), ALU.mult), reads=[sil, bu], writes=[hm])
                for fn in pending:
                    fn()
                pending = [lambda i=i, fc=fc, w=w, hm=hm, yt=yt: down(i, fc, w, hm, yt)]
        for fn in pending:
            fn()
        p.barrier()


def phase_ec(p, c, ps, rt_):
    nc = p.nc
    with ExitStack() as es:
        T = lambda name, shape, dt=F32: p.tile(es, name, shape, dt)
        fnw = T("fnw", [128, D])
        y1s = Rot([T(f"y1_{i}", [128, D]) for i in range(2)])
        y2s = Rot([T(f"y2_{i}", [128, D]) for i in range(2)])
        x2s = Rot([T(f"x2c_{i}", [128, D]) for i in range(2)])
        junk = T("junk_c", [128, D], BF16)
        st4 = Rot([T(f"st4c_{i}", [128, 4]) for i in range(2)])
        p.dma("sp", fnw[:], c.final_norm_w.partition_broadcast(128), writes=[fnw])
        for tt in range(16):
            y1 = y1s.next(); y2 = y2s.next(); x2 = x2s.next(); s4 = st4.next()
            p.dma("sp", x2[:], c.X2_s[tt * 128:(tt + 1) * 128, :], writes=[x2])
            p.idma(y1[:], None, c.YP_s, bass.IndirectOffsetOnAxis(rt_.POSI[:, 0, tt:tt + 1], 0), reads=[rt_.POSI], writes=[y1])
            p.idma(y2[:], None, c.YP_s, bass.IndirectOffsetOnAxis(rt_.POSI[:, 1, tt:tt + 1], 0), reads=[rt_.POSI], writes=[y2])
            p.op("dve", lambda: nc.vector.scalar_tensor_tensor(x2[:], y1[:], rt_.PG[:, tt, 0:1], x2[:], ALU.mult, ALU.add), reads=[y1, rt_.PG, x2], writes=[x2])
            p.op("dve", lambda: nc.vector.scalar_tensor_tensor(x2[:], y2[:], rt_.PG[:, tt, 1:2], x2[:], ALU.mult, ALU.add), reads=[y2, rt_.PG, x2], writes=[x2])
            p.op("act", lambda: nc.scalar.activation(out=junk[:], in_=x2[:], func=AF.Square, accum_out=s4[:, 0:1]), reads=[x2], writes=[junk, s4])
            p.op("dve", lambda: nc.vector.tensor_scalar(s4[:, 1:2], s4[:, 0:1], 1.0 / D, EPS, ALU.mult, ALU.add), reads=[s4], writes=[s4])
            p.op("act", lambda: nc.scalar.activation(out=s4[:, 2:3], in_=s4[:, 1:2], func=AF.Ln), reads=[s4], writes=[s4])
            p.op("act", lambda: nc.scalar.activation(out=s4[:, 3:4], in_=s4[:, 2:3], func=AF.Exp, scale=-0.5), reads=[s4], writes=[s4])
            p.op("dve", lambda: nc.vector.scalar_tensor_tensor(y1[:], x2[:], s4[:, 3:4], fnw[:], ALU.mult, ALU.mult), reads=[x2, s4, fnw], writes=[y1])
            p.dma("sp", c.out[tt * 128:(tt + 1) * 128, :], y1[:], reads=[y1])
        p.barrier()


def host_common_s(inp, m):
    for k in ("wg_t", "wu_t", "wd_t"):
        m.pop(k, None)
    wg = np.asarray(inp["w_exp_gate"])[0]
    wu = np.asarray(inp["w_exp_up"])[0]
    wd = np.asarray(inp["w_exp_down"])[0]
    tg = lambda w: np.ascontiguousarray(w.reshape(NE, 2, 8, 128, 4, 256).transpose(4, 0, 3, 1, 2, 5)).reshape(4, NE * 128, 4096)
    m["wg_r"] = tg(wg)
    m["wu_r"] = tg(wu)
    m["wd_r"] = np.ascontiguousarray(wd.reshape(NE, 4, 2, 128, D).transpose(1, 0, 3, 2, 4)).reshape(4, NE * 128, 4096)
    l = np.arange(128)
    sutr = (l[:, None] < l[None, :]).astype(np.float32)
    ones = np.ones((128, 128), np.float32)
    thr = np.tile((128.0 * np.arange(NSLOT, dtype=np.float32))[None, :], (128, 1))
    c8 = (np.arange(8, dtype=np.float32)[None, :] * 128.0 + l[:, None].astype(np.float32))
    m["cstE"] = np.ascontiguousarray(np.concatenate([sutr, ones, thr, c8], axis=1).astype(np.float32))
    return m
```

```python
import math
from contextlib import ExitStack

import numpy as np
import concourse.bass as bass
import concourse.mybir as mybir
from concourse.bass_utils import run_bass_kernel_spmd

F32 = mybir.dt.float32
BF16 = mybir.dt.bfloat16
AF = mybir.ActivationFunctionType
ALU = mybir.AluOpType
AX = mybir.AxisListType

D = 2048
NTOK = 2048
NPREV = 2048
NDC = D // 128
DI = 2048
NH = 32
HP = 64
NG = 8
DS = 128
CONV_DIM = DI + 2 * NG * DS
AH = 12
AE = 128
IN_COLS = 14880
C_Z, C_XBC, C_DT, C_Q, C_K, C_V, C_G = 0, 2048, 6144, 6176, 7712, 9248, 10784
DIL = (1, 4, 16)
NE = 32
DFF = 1024
EPS = 1e-6
NEG = -30000.0
NSLOT = 64
I32 = mybir.dt.int32


class Tok:
    __slots__ = ("sem", "val")

    def __init__(self, sem, val):
        self.sem = sem
        self.val = val


class Buf:
    def __init__(self, name=""):
        self.name = name
        self.w = []
        self.r = {}


class Tile(Buf):
    def __init__(self, name, t):
        super().__init__(name)
        self.t = t

    def __getitem__(self, k):
        return self.t[k]


def _flat(bs):
    out = []
    for b in bs:
        if b is None:
            continue
        if isinstance(b, (list, tuple)):
            out.extend(_flat(b))
        else:
            out.append(b)
    return out


class Prog:
    ENG = ("pe", "act", "dve", "pool", "sp")

    def __init__(self, nc, es):
        self.nc = nc
        self.es = es
        self.eng = {"pe": nc.tensor, "act": nc.scalar, "dve": nc.vector, "pool": nc.gpsimd, "sp": nc.sync}
        self.sem = {e: es.enter_context(nc.semaphore("s_" + e)) for e in self.ENG}
        self.cnt = {e: 0 for e in self.ENG}
        self.waited = {e: {} for e in self.ENG}
        nslots = {"sp": 28, "pool": 24, "act": 20, "bg": 14}
        self.slots = {q: [[es.enter_context(nc.semaphore(f"d_{q}{i}")), 0] for i in range(n)]
                      for q, n in nslots.items()}
        self.rr = {q: 0 for q in nslots}
        self.n_inst = 0

    def _wait(self, e, tok):
        if tok is None:
            return
        key = tok.sem.name
        if e == "pe" and tok.sem is self.sem["pe"]:
            return
        if self.waited[e].get(key, 0) >= tok.val:
            return
        self.eng[e].wait_ge(tok.sem, tok.val)
        self.waited[e][key] = tok.val
        self.n_inst += 1

    def _deps(self, e, reads, writes):
        for b in reads:
            for t in b.w:
                self._wait(e, t)
        for b in writes:
            for t in b.w:
                self._wait(e, t)
            for t in b.r.values():
                self._wait(e, t)

    def _commit(self, e, tok, reads, writes):
        for b in writes:
            b.w = [tok]
            b.r = {}
        for b in reads:
            if b not in writes:
                b.r[e] = tok

    def op(self, e, fn, reads=(), writes=(), mark=True):
        reads = _flat(reads)
        writes = _flat(writes)
        ex = [b for b in reads if getattr(b, "excl", False)]
        if ex:
            reads = [b for b in reads if b not in ex]
            writes = writes + [b for b in ex if b not in writes]
        self._deps(e, reads, writes)
        ins = fn()
        self.n_inst += 1
        if mark:
            self.cnt[e] += 1
            ins.then_inc(self.sem[e], 1)
            tok = Tok(self.sem[e], self.cnt[e])
        else:
            assert e == "pe"
            tok = Tok(self.sem[e], self.cnt[e] + 1)
        self._commit(e, tok, reads, writes)
        return tok

    def dma(self, q, out, in_, reads=(), writes=(), bg=False):
        reads = _flat(reads)
        writes = _flat(writes)
        if q == "sp" and reads and not writes:
            q = "act"
        sk = "bg" if bg else q
        slot = self.slots[sk][self.rr[sk]]
        self.rr[sk] = (self.rr[sk] + 1) % len(self.slots[sk])
        if slot[1] > 0:
            self._wait(q, Tok(slot[0], slot[1]))
        self._deps(q, reads, writes)
        ins = self.eng[q].dma_start(out=out, in_=in_)
        self.n_inst += 1
        slot[1] += 16
        ins.then_inc(slot[0], 16)
        tok = Tok(slot[0], slot[1])
        self._commit(q, tok, reads, writes)
        return tok

    def idma(self, out, out_off, in_, in_off, reads=(), writes=()):
        q = "pool"
        reads = _flat(reads)
        writes = _flat(writes)
        slot = self.slots[q][self.rr[q]]
        self.rr[q] = (self.rr[q] + 1) % len(self.slots[q])
        if slot[1] > 0:
            self._wait(q, Tok(slot[0], slot[1]))
        self._deps(q, reads, writes)
        ins = self.nc.gpsimd.indirect_dma_start(out, out_off, in_, in_off)
        self.n_inst += 1
        slot[1] += 16
        ins.then_inc(slot[0], 16)
        tok = Tok(slot[0], slot[1])
        self._commit(q, tok, reads, writes)
        return tok

    def barrier(self):
        toks = [Tok(self.sem[e], self.cnt[e]) for e in self.ENG if self.cnt[e] > 0]
        for q, sl in self.slots.items():
            if q == "bg":
                continue
            for s, c in sl:
                if c > 0:
                    toks.append(Tok(s, c))
        for e in self.ENG:
            for t in toks:
                if t.sem is self.sem[e]:
                    continue
                self._wait(e, t)

    def tile(self, es, name, shape, dtype):
        self.n_tiles = getattr(self, "n_tiles", 0) + 1
        t = es.enter_context(self.nc.sbuf_tensor(f"sb{self.n_tiles}_{name}", list(shape), dtype))
        return Tile(name, t)


class Rot:
    def __init__(self, tiles):
        self.tiles = tiles
        self.i = 0

    def next(self):
        t = self.tiles[self.i]
        self.i = (self.i + 1) % len(self.tiles)
        return t


def tile_w(w2d, cw):
    K, C = w2d.shape
    assert C % cw == 0 and K % 128 == 0
    kc = K // 128
    a = w2d.reshape(kc, 128, C // cw, cw).transpose(2, 1, 0, 3)
    return np.ascontiguousarray(a).reshape(C // cw, 128, kc * cw)


class Ctx:
    pass


def declare_io(nc, debug=False, phases="abcdeE"):
    c = Ctx()
    di = lambda name, shape, dt=F32: nc.dram_tensor(name, list(shape), dt, kind="ExternalInput").ap()
    c.x_own = di("x_own", [NTOK, D])
    c.x_prev = di("x_prev", [NPREV, D])
    c.w_fm = di("w_fm", [88, 128, NDC * 128])
    c.w_tm = di("w_tm", [7, 128, NDC * 512])
    c.w_dt = di("w_dt", [128, NDC * 32])
    c.attn_norm_w = di("attn_norm_w", [1, D])
    c.b_gate = di("b_gate", [128, 32])
    c.ident = di("ident", [128, 128])
    c.cstB = di("cstB", [128, 1024])
    c.conv_wb = di("conv_wb", [128, 32 * 5])
    c.vec32 = di("vec32", [3, NH])
    c.ssd_norm_w = di("ssd_norm_w", [1, DI])
    c.flag = di("flag", [128, 2])
    c.attn_bias = di("attn_bias", [3, 128, 1024])
    c.w_ssd_fm = di("w_ssd_fm", [16, 128, NDC * 128])
    c.w_attn_fm = di("w_attn_fm", [16, 128, 4 * 128])
    c.w_out_tm = di("w_out_tm", [4, 128, NDC * 512])
    c.ffn_norm_w = di("ffn_norm_w", [1, D])
    c.w_router = di("w_router", [128, NDC * 36])
    c.b_router = di("b_router", [1, 36])
    if "E" in phases:
        c.wg_r = di("wg_r", [4, NE * 128, 4096])
        c.wu_r = di("wu_r", [4, NE * 128, 4096])
        c.wd_r = di("wd_r", [4, NE * 128, 4096])
        c.WB_s = [nc.dram_tensor(f"WB_s{fc}", [NE * 128, 3 * 4096], BF16).ap() for fc in range(4)]
        c.precast = [(fc, k, src, r0) for fc in range(4) for r0 in range(0, NE * 128, 512) for (k, src) in ((0, c.wg_r), (1, c.wu_r), (2, c.wd_r))]
        c.precast_toks = []
    c.cstE = di("cstE", [128, 128 + 128 + 64 + 8])
    c.final_norm_w = di("final_norm_w", [1, D])
    c.out = nc.dram_tensor("out", [NTOK, D], F32, kind="ExternalOutput").ap()
    kind = "ExternalOutput" if debug else "Internal"
    ds = lambda name, shape, dt=F32: nc.dram_tensor(name, list(shape), dt, kind=kind).ap()
    c.XBC_s = ds("XBC_s", [CONV_DIM, 3 + NPREV + NTOK])
    c.Z_s = ds("Z_s", [NTOK, DI])
    c.DT_s = ds("DT_s", [NPREV + NTOK, NH])
    c.QT_s = ds("QT_s", [AH, 128, NTOK], BF16)
    c.KT_s = ds("KT_s", [AH, 128, NPREV + NTOK], BF16)
    c.V_s = ds("V_s", [3, 32, 128, 512], BF16)
    c.G_s = ds("G_s", [2 * D, NTOK])
    c.YN_s = ds("YN_s", [NDC, 128, NTOK], BF16)
    c.O_s = ds("O_s", [3, NTOK, 4 * 130])
    c.OT_s = ds("OT_s", [4, 128, NTOK], BF16)
    c.M_s = ds("M_s", [NDC, 128, NTOK], BF16)
    c.X2_s = ds("X2_s", [NTOK, D])
    c.HN_s = ds("HN_s", [NDC, 128, NTOK], BF16)
    c.GATE_s = ds("GATE_s", [NTOK, NE])
    c.HNTM_s = ds("HNTM_s", [NTOK, D], BF16)
    c.XS_s = ds("XS_s", [NSLOT * 128, D], BF16)
    c.YP_s = ds("YP_s", [NSLOT * 128, D])
    return c


def precast_step(p, c, n=1):
    for _ in range(n):
        if not getattr(c, "precast", None):
            return
        fc, k, src, r0 = c.precast.pop(0)
        dst = c.WB_s[fc][r0:r0 + 512, k * 4096:(k + 1) * 4096].rearrange("r (a b) -> r a b", a=2)
        c.precast_toks.append(p.dma("pool", dst, src[fc, r0:r0 + 512, :].rearrange("r (a b) -> r a b", a=2), bg=True))


def cls_view(ap2d, d):
    if d == 1:
        return ap2d.unsqueeze(1)
    return ap2d.rearrange("p (j r) -> p r j", r=d)


def phase_a(p, c, ps):
    nc = p.nc
    with ExitStack() as es:
        hT = p.tile(es, "hT", [128, NDC, 2048], BF16)
        nw = p.tile(es, "nw_bc", [128, D], F32)
        bg = p.tile(es, "bg", [128, 32], F32)
        idb = p.tile(es, "idb", [128, 128], BF16)
        zero = p.tile(es, "zero", [128, 4], F32)
        xts = Rot([p.tile(es, f"xt{i}", [128, D], F32) for i in range(2)])
        hbs = Rot([p.tile(es, f"hb{i}", [128, D], BF16) for i in range(2)])
        junk = p.tile(es, "junk", [128, D], BF16)
        sts = Rot([p.tile(es, f"st{i}", [128, 4], F32) for i in range(2)])
        wfm = Rot([p.tile(es, f"wfm{i}", [128, NDC, 128], BF16) for i in range(6)])
        wtm = Rot([p.tile(es, f"wtm{i}", [128, NDC, 512], BF16) for i in range(2)])
        wdt = p.tile(es, "wdt", [128, NDC, 32], BF16)
        so32 = Rot([p.tile(es, f"so32_{i}", [128, 512], F32) for i in range(4)])
        so16 = Rot([p.tile(es, f"so16_{i}", [128, 512], BF16) for i in range(4)])
        psr = Rot(ps)
        ev = [0]

        p.dma("sp", nw[:], c.attn_norm_w.partition_broadcast(128), writes=[nw])
        p.dma("sp", bg[:], c.b_gate, writes=[bg])
        p.dma("pool", idb[:], c.ident, writes=[idb])
        p.dma("pool", wdt[:].rearrange("p a b -> p (a b)"), c.w_dt, writes=[wdt])
        p.op("dve", lambda: nc.vector.memset(zero[:], 0.0), writes=[zero])
        for r0 in range(0, CONV_DIM, 128):
            p.dma("sp", c.XBC_s[r0:r0 + 128, 0:3], zero[:, 0:3], reads=[zero])

        def evac_copy(dst_ap, src_ap, reads, writes, scale=None):
            e = "act" if ev[0] % 2 == 0 else "dve"
            ev[0] += 1
            if e == "act":
                if scale is None:
                    return p.op("act", lambda: nc.scalar.copy(dst_ap, src_ap), reads=reads, writes=writes)
                return p.op("act", lambda: nc.scalar.activation(out=dst_ap, in_=src_ap, func=AF.Copy, scale=scale),
                            reads=reads, writes=writes)
            if scale is None:
                return p.op("dve", lambda: nc.vector.tensor_copy(dst_ap, src_ap), reads=reads, writes=writes)
            return p.op("dve", lambda: nc.vector.tensor_scalar(dst_ap, src_ap, scale, None, ALU.mult),
                        reads=reads, writes=writes)

        def build_hT(xsrc):
            for tt in range(16):
                xt = xts.next(); hb = hbs.next(); st = sts.next()
                p.dma("sp", xt[:], xsrc[tt * 128:(tt + 1) * 128, :], writes=[xt])
                p.op("act", lambda: nc.scalar.activation(out=junk[:], in_=xt[:], func=AF.Square, accum_out=st[:, 0:1]),
                     reads=[xt], writes=[junk, st])
                p.op("dve", lambda: nc.vector.tensor_scalar(st[:, 1:2], st[:, 0:1], 1.0 / D, EPS, ALU.mult, ALU.add),
                     reads=[st], writes=[st])
                p.op("act", lambda: nc.scalar.activation(out=st[:, 2:3], in_=st[:, 1:2], func=AF.Ln), reads=[st], writes=[st])
                p.op("act", lambda: nc.scalar.activation(out=st[:, 3:4], in_=st[:, 2:3], func=AF.Exp, scale=-0.5),
                     reads=[st], writes=[st])
                p.op("dve", lambda: nc.vector.scalar_tensor_tensor(hb[:], xt[:], st[:, 3:4], nw[:], ALU.mult, ALU.mult),
                     reads=[xt, st, nw], writes=[hb])
                for half in range(2):
                    bank = psr.next()
                    pb = bank[:].bitcast(BF16)
                    for k in range(8):
                        dc = half * 8 + k
                        p.op("pe", lambda: nc.tensor.transpose(pb[:, k * 128:(k + 1) * 128], hb[:, dc * 128:(dc + 1) * 128], idb[:]),
                             reads=[hb, idb], writes=[bank], mark=(k == 7))
                    evac_copy(hT[:, half * 8:half * 8 + 8, tt * 128:(tt + 1) * 128],
                              pb.rearrange("p (a b) -> p a b", a=8), [bank], [hT])

        def mm_group(bank, out_ap, lhs_fn, rhs_fn, reads):
            for dc in range(NDC):
                p.op("pe", lambda: nc.tensor.matmul(out_ap, lhs_fn(dc), rhs_fn(dc), start=(dc == 0), stop=(dc == NDC - 1)),
                     reads=reads, writes=[bank], mark=(dc == NDC - 1))

        nfm = [0]

        def fm_chunk(cc, jobs):
            nfm[0] += 1
            if nfm[0] % 4 == 0:
                precast_step(p, c)
            wb = wfm.next()
            p.dma("pool", wb[:].rearrange("p a b -> p (a b)"), c.w_fm[cc], writes=[wb])
            for rhs_fn, n, epi in jobs:
                bank = psr.next()
                mm_group(bank, bank[:, 0:n], lambda dc: wb[:, dc, :], rhs_fn, [wb, hT])
                epi(bank, n)

        def nat_rhs(st):
            return lambda dc: hT[:, dc, st * 512:(st + 1) * 512]

        def epi_store32(dst_ap):
            def f(bank, n):
                so = so32.next()
                evac_copy(so[:, 0:n], bank[:, 0:n], [bank], [so])
                p.dma("sp", dst_ap, so[:, 0:n], reads=[so])
            return f

        def epi_store16(dst_ap, scale=None, n3=None):
            def f(bank, n):
                so = so16.next()
                evac_copy(so[:, 0:n], bank[:, 0:n], [bank], [so], scale=scale)
                src = so[:, 0:n]
                if n3 is not None:
                    src = src.rearrange("p (a b) -> p a b", a=n3)
                p.dma("sp", dst_ap, src, reads=[so])
            return f

        def epi_gate(cc, dst_ap):
            def f(bank, n):
                so = so32.next()
                p.op("act", lambda: nc.scalar.activation(out=so[:, 0:n], in_=bank[:, 0:n], func=AF.Sigmoid, bias=bg[:, cc:cc + 1]),
                     reads=[bank, bg], writes=[so])
                p.dma("sp", dst_ap, so[:, 0:n], reads=[so])
            return f

        def kt_view(h, d):
            return c.KT_s[h].rearrange("e (r n j) -> e r n j", r=d, j=128)

        def tm_tile(wt_ap_fn, ncols, lhs_list, dst_list, dtype32=True):
            for lhs_fn, dst in zip(lhs_list, dst_list):
                bank = psr.next()
                mm_group(bank, bank[:, 0:ncols], lhs_fn, wt_ap_fn, [hT] + tm_reads[0])
                if dtype32:
                    so = so32.next()
                else:
                    so = so16.next()
                evac_copy(so[:, 0:ncols], bank[:, 0:ncols], [bank], [so])
                p.dma("sp", dst, so[:, 0:ncols], reads=[so])

        tm_reads = [[]]

        def v_blocks(g, prev):
            d = DIL[g]
            span = 128 * d
            nprevb = NPREV // span
            ncls = 4096 // span
            lhs, dst = [], []
            if prev:
                blocks = [(r, nprevb - 1) for r in range(d)]
            else:
                blocks = [(r, n) for r in range(d) for n in range(NTOK // span)]
            for r, n in blocks:
                def lf(dc, r=r, n=n):
                    return cls_view(hT[:, dc, :], d)[:, r, n * 128:(n + 1) * 128]
                lhs.append(lf)
                ng = n if prev else nprevb + n
                dst.append(c.V_s[g, r * ncls + ng])
            return lhs, dst

        build_hT(c.x_prev)
        for cc in range(24):
            fm_chunk(cc, [(nat_rhs(st), 512, epi_store32(c.XBC_s[cc * 128:(cc + 1) * 128, 3 + st * 512: 3 + (st + 1) * 512]))
                          for st in range(4)])
        for cc in range(24, 32):
            fm_chunk(cc, [(nat_rhs(3), 512, epi_store32(c.XBC_s[cc * 128:(cc + 1) * 128, 3 + 3 * 512: 3 + 4 * 512]))])
        for h in range(AH):
            g = h // 4
            d = DIL[g]
            cc = 44 + h
            if g == 0:
                jobs = [(lambda dc: hT[:, dc, 1920:2048], 128, epi_store16(c.KT_s[h][:, 15 * 128:16 * 128]))]
            elif g == 1:
                jobs = [(lambda dc: cls_view(hT[:, dc, :], 4)[:, :, 384:512], 512,
                         epi_store16(kt_view(h, 4)[:, :, 3, :], n3=4))]
            else:
                jobs = [((lambda dc, st=st: cls_view(hT[:, dc, :], 16)[:, 4 * st:4 * st + 4, :]), 512,
                         epi_store16(kt_view(h, 16)[:, 4 * st:4 * st + 4, 0, :], n3=4)) for st in range(4)]
            fm_chunk(cc, jobs)
        tm_reads[0] = [wdt]
        tm_tile(lambda dc: wdt[:, dc, :], 32,
                [(lambda dc, tt=tt: hT[:, dc, tt * 128:(tt + 1) * 128]) for tt in range(16)],
                [c.DT_s[tt * 128:(tt + 1) * 128, :] for tt in range(16)])
        for g in range(3):
            wt = wtm.next()
            p.dma("pool", wt[:].rearrange("p a b -> p (a b)"), c.w_tm[4 + g], writes=[wt])
            tm_reads[0] = [wt]
            lhs, dst = v_blocks(g, True)
            tm_tile(lambda dc: wt[:, dc, :], 512, lhs, dst, dtype32=False)

        build_hT(c.x_own)
        for cc in range(32):
            fm_chunk(cc, [(nat_rhs(st), 512,
                           epi_store32(c.XBC_s[cc * 128:(cc + 1) * 128, 3 + NPREV + st * 512: 3 + NPREV + (st + 1) * 512]))
                          for st in range(4)])
        inv = 1.0 / math.sqrt(AE)
        for h in range(AH):
            g = h // 4
            d = DIL[g]
            if g == 0:
                jobs = [(nat_rhs(st), 512, epi_store16(c.QT_s[h][:, st * 512:(st + 1) * 512], scale=inv)) for st in range(4)]
            elif g == 1:
                jobs = [((lambda dc, r=r: cls_view(hT[:, dc, :], 4)[:, r, :]), 512,
                         epi_store16(c.QT_s[h][:, r * 512:(r + 1) * 512], scale=inv)) for r in range(4)]
            else:
                jobs = [((lambda dc, st=st: cls_view(hT[:, dc, :], 16)[:, 4 * st:4 * st + 4, :]), 512,
                         epi_store16(c.QT_s[h][:, st * 512:(st + 1) * 512], scale=inv)) for st in range(4)]
            fm_chunk(32 + h, jobs)
            if g == 0:
                jobs = [(nat_rhs(st), 512, epi_store16(c.KT_s[h][:, (16 + 4 * st) * 128:(20 + 4 * st) * 128])) for st in range(4)]
            elif g == 1:
                jobs = [((lambda dc, r=r: cls_view(hT[:, dc, :], 4)[:, r, :]), 512,
                         epi_store16(c.KT_s[h][:, r * 1024 + 512:(r + 1) * 1024])) for r in range(4)]
            else:
                jobs = [((lambda dc, st=st: cls_view(hT[:, dc, :], 16)[:, 4 * st:4 * st + 4, :]), 512,
                         epi_store16(kt_view(h, 16)[:, 4 * st:4 * st + 4, 1, :], n3=4)) for st in range(4)]
            fm_chunk(44 + h, jobs)
        for cc in range(32):
            fm_chunk(56 + cc, [(nat_rhs(st), 512, epi_gate(cc, c.G_s[cc * 128:(cc + 1) * 128, st * 512:(st + 1) * 512]))
                               for st in range(4)])
        for ct in range(4):
            wt = wtm.next()
            p.dma("pool", wt[:].rearrange("p a b -> p (a b)"), c.w_tm[ct], writes=[wt])
            tm_reads[0] = [wt]
            tm_tile(lambda dc: wt[:, dc, :], 512,
                    [(lambda dc, tt=tt: hT[:, dc, tt * 128:(tt + 1) * 128]) for tt in range(16)],
                    [c.Z_s[tt * 128:(tt + 1) * 128, ct * 512:(ct + 1) * 512] for tt in range(16)])
        tm_reads[0] = [wdt]
        tm_tile(lambda dc: wdt[:, dc, :], 32,
                [(lambda dc, tt=tt: hT[:, dc, tt * 128:(tt + 1) * 128]) for tt in range(16)],
                [c.DT_s[NPREV + tt * 128:NPREV + (tt + 1) * 128, :] for tt in range(16)])
        for g in range(3):
            wt = wtm.next()
            p.dma("pool", wt[:].rearrange("p a b -> p (a b)"), c.w_tm[4 + g], writes=[wt])
            tm_reads[0] = [wt]
            lhs, dst = v_blocks(g, False)
            tm_tile(lambda dc: wt[:, dc, :], 512, lhs, dst, dtype32=False)
        p.barrier()


def host_common(inp):
    w_in = np.asarray(inp["w_in"])[0]
    fm_cols = np.concatenate([w_in[:, C_XBC:C_DT], w_in[:, C_Q:C_K], w_in[:, C_K:C_V], w_in[:, C_G:IN_COLS]], axis=1)
    tm_cols = np.concatenate([w_in[:, C_Z:C_XBC], w_in[:, C_V:C_G]], axis=1)
    m = {}
    m["w_fm"] = tile_w(fm_cols, 128)
    m["w_tm"] = tile_w(tm_cols, 512)
    m["w_dt"] = tile_w(w_in[:, C_DT:C_Q], 32)[0]
    m["attn_norm_w"] = np.ascontiguousarray(np.asarray(inp["attn_norm_w"]).reshape(1, D))
    m["b_gate"] = np.ascontiguousarray(np.asarray(inp["b_gate"]).reshape(32, 128).T)
    m["ident"] = np.eye(128, dtype=np.float32)
    return m


def phase_b(p, c, ps):
    nc = p.nc
    with ExitStack() as es:
        T = lambda name, shape, dt=F32: p.tile(es, name, shape, dt)
        cst = T("cstB", [128, 1024])
        TRI, SU, ONES, IDF, NEGM = (cst[:, 0:128], cst[:, 128:256], cst[:, 256:384], cst[:, 384:512], cst[:, 512:1024])
        idb = T("idbB", [128, 128], BF16)
        cwb = T("cwb", [128, 32, 5])
        v32 = T("v32", [128, 3, NH])
        A_bc = T("A_bc", [128, NH])
        nws = T("nws", [128, DI])
        flag = T("flagB", [128, 2])
        us = Rot([T(f"u{i}", [128, 515]) for i in range(3)])
        accs = Rot([T(f"acc{i}", [128, 512]) for i in range(2)])
        xcs = Rot([T(f"xc{i}", [128, 512]) for i in range(2)])
        xs_tm = T("xs_tm", [128, 4, DI])
        B_tm = T("B_tm", [128, 4, NG * DS], BF16)
        BT = T("BT", [128, NG, 512], BF16)
        CT = T("CT", [128, NG, 512], BF16)
        dtr = Rot([T(f"dtr{i}", [128, NH]) for i in range(2)])
        sm = Rot([T(f"sm{i}", [128, 8, NH]) for i in range(2)])
        Ex = Rot([T(f"Ex{i}", [128, 3, NH]) for i in range(2)])
        L_all = T("L_all", [128, NH, 128])
        x_dt = T("x_dt", [128, DI], BF16)
        xw = T("xw", [128, DI], BF16)
        decs = Rot([T(f"dec{i}", [128, 512]) for i in range(2)])
        MTs = Rot([T(f"MT{i}", [128, 512], BF16) for i in range(2)])
        y = T("y", [128, DI])
        tmp = T("tmpB", [128, DI])
        zts = Rot([T(f"zt{i}", [128, DI]) for i in range(2)])
        yn = T("yn", [128, DI], BF16)
        junk = T("junkB", [128, DI], BF16)
        st4 = Rot([T(f"st4_{i}", [128, 4]) for i in range(2)])
        S = T("S", [128, DI])
        S_bf = T("S_bf", [128, DI], BF16)
        ynT = Rot([T(f"ynT{i}", [128, NDC, 128], BF16) for i in range(2)])
        psr = Rot(ps)
        S_g = [Buf(f"S{g}") for g in range(NG)]
        Sbf_g = [Buf(f"Sbf{g}") for g in range(NG)]
        y_g = [Buf(f"y{g}") for g in range(NG)]
        tmp_g = [Buf(f"tmp{g}") for g in range(NG)]
        x_dts = Rot([x_dt, T("x_dt2", [128, DI], BF16)])
        xws = Rot([xw, T("xw2", [128, DI], BF16)])

        p.dma("sp", cst[:], c.cstB, writes=[cst])
        p.dma("pool", idb[:], c.ident, writes=[idb])
        p.dma("sp", cwb[:].rearrange("p a b -> p (a b)"), c.conv_wb, writes=[cwb])
        p.dma("sp", v32[:].rearrange("p a b -> p (a b)"), c.vec32.rearrange("a b -> (a b)").partition_broadcast(128), writes=[v32])
        p.dma("sp", nws[:], c.ssd_norm_w.partition_broadcast(128), writes=[nws])
        p.dma("sp", flag[:], c.flag, writes=[flag])
        p.op("act", lambda: nc.scalar.activation(out=A_bc[:], in_=v32[:, 1, :], func=AF.Exp), reads=[v32], writes=[A_bc])
        p.op("dve", lambda: nc.vector.tensor_scalar(A_bc[:], A_bc[:], -1.0, None, ALU.mult), reads=[A_bc], writes=[A_bc])
        p.op("dve", lambda: nc.vector.memset(S[:], 0.0), writes=[S_g])
        p.op("dve", lambda: nc.vector.memset(S_bf[:], 0.0), writes=[Sbf_g])
        DTB = v32[:, 0, :]
        D_bc = v32[:, 2, :]
        bc = lambda ap, n: ap.unsqueeze(2).broadcast_to([128, ap.shape[1], n])

        def conv_supertile(t0, own):
            ncc = 32 if own else 24
            for cc in range(ncc):
                u = us.next(); acc = accs.next(); xc = xcs.next()
                p.dma("sp", u[:], c.XBC_s[cc * 128:(cc + 1) * 128, t0:t0 + 515], writes=[u])
                p.op("dve", lambda: nc.vector.tensor_scalar(acc[:], u[:, 0:512], cwb[:, cc, 0:1], cwb[:, cc, 4:5], ALU.mult, ALU.add),
                     reads=[u, cwb], writes=[acc])
                for w in range(1, 4):
                    p.op("dve", lambda: nc.vector.scalar_tensor_tensor(acc[:], u[:, w:w + 512], cwb[:, cc, w:w + 1], acc[:], ALU.mult, ALU.add),
                         reads=[u, cwb, acc], writes=[acc])
                if cc < 16:
                    p.op("act", lambda: nc.scalar.activation(out=xc[:], in_=acc[:], func=AF.Silu), reads=[acc], writes=[xc])
                    bank = psr.next()
                    for k in range(4):
                        p.op("pe", lambda: nc.tensor.transpose(bank[:, k * 128:(k + 1) * 128], xc[:, k * 128:(k + 1) * 128], IDF),
                             reads=[xc, cst], writes=[bank], mark=(k == 3))
                    p.op("dve", lambda: nc.vector.tensor_copy(xs_tm[:, :, cc * 128:(cc + 1) * 128], bank[:].rearrange("p (a b) -> p a b", a=4)),
                         reads=[bank], writes=[xs_tm])
                elif cc < 24:
                    g = cc - 16
                    p.op("act", lambda: nc.scalar.activation(out=BT[:, g, :], in_=acc[:], func=AF.Silu), reads=[acc], writes=[BT])
                    bank = psr.next()
                    pb = bank[:].bitcast(BF16)
                    for k in range(4):
                        p.op("pe", lambda: nc.tensor.transpose(pb[:, k * 128:(k + 1) * 128], BT[:, g, k * 128:(k + 1) * 128], idb[:]),
                             reads=[BT, idb], writes=[bank], mark=(k == 3))
                    p.op("dve", lambda: nc.vector.tensor_copy(B_tm[:, :, g * 128:(g + 1) * 128], pb[:, 0:512].rearrange("p (a b) -> p a b", a=4)),
                         reads=[bank], writes=[B_tm])
                else:
                    g = cc - 24
                    p.op("act", lambda: nc.scalar.activation(out=CT[:, g, :], in_=acc[:], func=AF.Silu), reads=[acc], writes=[CT])

        def chunk(ci, k, own):
            precast_step(p, c, 2)
            dr = dtr.next(); s = sm.next(); E = Ex.next()
            x_dt = x_dts.next(); xw = xws.next()
            p.dma("sp", dr[:], c.DT_s[ci * 128:(ci + 1) * 128, :], writes=[dr])
            p.op("dve", lambda: nc.vector.tensor_tensor(s[:, 0, :], dr[:], DTB, ALU.add), reads=[dr, v32], writes=[s])
            p.op("act", lambda: nc.scalar.activation(out=s[:, 1, :], in_=s[:, 0, :], func=AF.Exp), reads=[s], writes=[s])
            p.op("act", lambda: nc.scalar.activation(out=s[:, 2, :], in_=s[:, 1, :], func=AF.Ln, bias=1.0), reads=[s], writes=[s])
            p.op("dve", lambda: nc.vector.tensor_tensor(s[:, 3, :], s[:, 2, :], A_bc[:], ALU.mult), reads=[s, A_bc], writes=[s])
            bk = psr.next()
            p.op("pe", lambda: nc.tensor.matmul(bk[:, 0:NH], TRI, s[:, 3, :], start=True, stop=True), reads=[cst, s], writes=[bk], mark=False)
            p.op("pe", lambda: nc.tensor.matmul(bk[:, NH:2 * NH], ONES, s[:, 3, :], start=True, stop=True), reads=[cst, s], writes=[bk])
            p.op("act", lambda: nc.scalar.copy(s[:, 4:6, :].rearrange("p a b -> p (a b)"), bk[:, 0:2 * NH]), reads=[bk], writes=[s])
            p.op("dve", lambda: nc.vector.tensor_tensor(s[:, 6, :], s[:, 5, :], s[:, 4, :], ALU.subtract), reads=[s], writes=[s])
            p.op("act", lambda: nc.scalar.activation(out=E[:].rearrange("p a b -> p (a b)"), in_=s[:, 4:7, :].rearrange("p a b -> p (a b)"), func=AF.Exp),
                 reads=[s], writes=[E])
            p.op("dve", lambda: nc.vector.tensor_tensor(s[:, 7, :], s[:, 2, :], E[:, 2, :], ALU.mult), reads=[s, E], writes=[s])
            xs3 = xs_tm[:, k, :].rearrange("p (h q) -> p h q", h=NH)
            p.op("dve", lambda: nc.vector.tensor_tensor(xw[:].rearrange("p (h q) -> p h q", h=NH), xs3, bc(s[:, 7, :], HP), ALU.mult),
                 reads=[xs_tm, s], writes=[xw])
            if own:
                p.op("dve", lambda: nc.vector.tensor_tensor(x_dt[:].rearrange("p (h q) -> p h q", h=NH), xs3, bc(s[:, 2, :], HP), ALU.mult),
                     reads=[xs_tm, s], writes=[x_dt])
                p.op("dve", lambda: nc.vector.tensor_tensor(L_all[:], SU.unsqueeze(1).broadcast_to([128, NH, 128]), bc(s[:, 3, :], 128), ALU.mult),
                     reads=[cst, s], writes=[L_all])
            for g in range(NG):
                hs = slice(g * 4, g * 4 + 4)
                cs = slice(g * 256, (g + 1) * 256)
                if own:
                    bseg = psr.next()
                    for hh in range(4):
                        p.op("pe", lambda: nc.tensor.matmul(bseg[:, hh * 128:(hh + 1) * 128], L_all[:, g * 4 + hh, :], TRI, start=True, stop=False),
                             reads=[L_all, cst], writes=[bseg], mark=False)
                        p.op("pe", lambda: nc.tensor.matmul(bseg[:, hh * 128:(hh + 1) * 128], IDF, NEGM[:, 0:128], start=False, stop=True),
                             reads=[cst], writes=[bseg], mark=(hh == 3))
                    dec = decs.next(); MT = MTs.next()
                    p.op("act", lambda: nc.scalar.activation(out=dec[:], in_=bseg[:], func=AF.Exp), reads=[bseg], writes=[dec])
                    bcb = psr.next()
                    p.op("pe", lambda: nc.tensor.matmul(bcb[:, 0:128], BT[:, g, k * 128:(k + 1) * 128], CT[:, g, k * 128:(k + 1) * 128], start=True, stop=True),
                         reads=[BT, CT], writes=[bcb])
                    p.op("dve", lambda: nc.vector.tensor_tensor(MT[:].rearrange("p (h l) -> p h l", h=4), dec[:].rearrange("p (h l) -> p h l", h=4),
                                                                bcb[:, 0:128].unsqueeze(1).broadcast_to([128, 4, 128]), ALU.mult),
                         reads=[dec, bcb], writes=[MT])
                    by = psr.next()
                    for hh in range(4):
                        h = g * 4 + hh
                        p.op("pe", lambda: nc.tensor.matmul(by[:, hh * 64:(hh + 1) * 64], MT[:, hh * 128:(hh + 1) * 128], x_dt[:, h * 64:(h + 1) * 64], start=True, stop=True),
                             reads=[MT, x_dt], writes=[by], mark=False)
                    p.op("pe", lambda: nc.tensor.matmul(by[:, 256:512], CT[:, g, k * 128:(k + 1) * 128], S_bf[:, cs], start=True, stop=True),
                         reads=[CT, Sbf_g[g]], writes=[by])
                    p.op("dve", lambda: nc.vector.tensor_tensor(tmp[:, cs].rearrange("p (h q) -> p h q", h=4), by[:, 256:512].rearrange("p (h q) -> p h q", h=4),
                                                                bc(E[:, 0, hs], HP), ALU.mult), reads=[by, E], writes=[tmp_g[g]])
                    p.op("dve", lambda: nc.vector.tensor_tensor(y[:, cs], tmp[:, cs], by[:, 0:256], ALU.add), reads=[tmp_g[g], by], writes=[y_g[g]])
                bs_ = psr.next()
                p.op("pe", lambda: nc.tensor.matmul(bs_[:, 0:256], B_tm[:, k, g * 128:(g + 1) * 128], xw[:, cs], start=True, stop=True),
                     reads=[B_tm, xw], writes=[bs_])
                p.op("dve", lambda: nc.vector.tensor_tensor(S[:, cs].rearrange("p (h q) -> p h q", h=4), S[:, cs].rearrange("p (h q) -> p h q", h=4),
                                                            bc(E[:, 1, hs], HP), ALU.mult), reads=[S_g[g], E], writes=[S_g[g]])
                p.op("dve", lambda: nc.vector.tensor_tensor(S[:, cs], S[:, cs], bs_[:, 0:256], ALU.add), reads=[S_g[g], bs_], writes=[S_g[g]])
                p.op("act", lambda: nc.scalar.copy(S_bf[:, cs], S[:, cs]), reads=[S_g[g]], writes=[Sbf_g[g]])
            if not own:
                return
            tt = ci - 16
            p.op("dve", lambda: nc.vector.tensor_tensor(tmp[:].rearrange("p (h q) -> p h q", h=NH), xs3, bc(D_bc, HP), ALU.mult),
                 reads=[xs_tm, v32], writes=[tmp_g])
            p.op("dve", lambda: nc.vector.tensor_tensor(y[:], y[:], tmp[:], ALU.add), reads=[y_g, tmp_g], writes=[y_g])
            zt = zts.next(); s4 = st4.next()
            p.dma("sp", zt[:], c.Z_s[tt * 128:(tt + 1) * 128, :], writes=[zt])
            p.op("act", lambda: nc.scalar.activation(out=zt[:], in_=zt[:], func=AF.Silu), reads=[zt], writes=[zt])
            p.op("dve", lambda: nc.vector.tensor_tensor(y[:], y[:], zt[:], ALU.mult), reads=[y_g, zt], writes=[y_g])
            p.op("act", lambda: nc.scalar.activation(out=junk[:], in_=y[:], func=AF.Square, accum_out=s4[:, 0:1]), reads=[y_g], writes=[junk, s4])
            p.op("dve", lambda: nc.vector.tensor_scalar(s4[:, 1:2], s4[:, 0:1], 1.0 / DI, EPS, ALU.mult, ALU.add), reads=[s4], writes=[s4])
            p.op("act", lambda: nc.scalar.activation(out=s4[:, 2:3], in_=s4[:, 1:2], func=AF.Ln), reads=[s4], writes=[s4])
            p.op("act", lambda: nc.scalar.activation(out=s4[:, 3:4], in_=s4[:, 2:3], func=AF.Exp, scale=-0.5), reads=[s4], writes=[s4])
            p.op("dve", lambda: nc.vector.scalar_tensor_tensor(yn[:], y[:], s4[:, 3:4], nws[:], ALU.mult, ALU.mult), reads=[y_g, s4, nws], writes=[yn])
            yT = ynT.next()
            for half in range(2):
                bank = psr.next()
                pb = bank[:].bitcast(BF16)
                for kk in range(8):
                    dc = half * 8 + kk
                    p.op("pe", lambda: nc.tensor.transpose(pb[:, kk * 128:(kk + 1) * 128], yn[:, dc * 128:(dc + 1) * 128], idb[:]),
                         reads=[yn, idb], writes=[bank], mark=(kk == 7))
                if half == 0:
                    p.op("act", lambda: nc.scalar.copy(yT[:, 0:8, :], pb.rearrange("p (a b) -> p a b", a=8)), reads=[bank], writes=[yT])
                else:
                    p.op("dve", lambda: nc.vector.tensor_copy(yT[:, 8:16, :], pb.rearrange("p (a b) -> p a b", a=8)), reads=[bank], writes=[yT])
            p.dma("sp", c.YN_s[:, :, tt * 128:(tt + 1) * 128].rearrange("a p t -> p a t"), yT[:], reads=[yT])

        for sti in range(8):
            own = sti >= 4
            conv_supertile(sti * 512, own)
            for k in range(4):
                chunk(sti * 4 + k, k, own)
            if sti == 3:
                p.op("dve", lambda: nc.vector.tensor_scalar(S[:], S[:], flag[:, 0:1], None, ALU.mult), reads=[S_g, flag], writes=[S_g])
                p.op("act", lambda: nc.scalar.copy(S_bf[:], S[:]), reads=[S_g], writes=[Sbf_g])
        p.barrier()


def host_common_b(inp, m):
    l = np.arange(128)
    tri = (l[:, None] <= l[None, :]).astype(np.float32)
    su = (l[:, None] > l[None, :]).astype(np.float32)
    ones = np.ones((128, 128), np.float32)
    idf = np.eye(128, dtype=np.float32)
    negm = np.where(l[None, :] < l[:, None], NEG, 0.0).astype(np.float32)
    m["cstB"] = np.ascontiguousarray(np.concatenate([tri, su, ones, idf, np.tile(negm, (1, 4))], axis=1))
    cw = np.asarray(inp["conv_w"])[0]
    cb = np.asarray(inp["conv_b"])[0]
    wb = np.concatenate([cw, cb[None, :]], axis=0)
    m["conv_wb"] = np.ascontiguousarray(wb.reshape(5, 32, 128).transpose(2, 1, 0)).reshape(128, 160)
    m["vec32"] = np.ascontiguousarray(np.stack([np.asarray(inp["dt_bias"])[0], np.asarray(inp["a_log"])[0], np.asarray(inp["d_skip"])[0]]))
    m["ssd_norm_w"] = np.ascontiguousarray(np.asarray(inp["ssd_norm_w"]).reshape(1, DI))
    return m


def phase_c(p, c, ps):
    nc = p.nc
    with ExitStack() as es:
        T = lambda name, shape, dt=F32: p.tile(es, name, shape, dt)
        idb = T("idbC", [128, 128], BF16)
        flag = T("flagC", [128, 2])
        bmid = T("bmid", [128, 4, 256])
        bfirst = T("bfirst", [128, 4, 256])
        qts = Rot([T(f"qt{i}", [128, 4, 128], BF16) for i in range(3)])
        kts = Rot([T(f"kt{i}", [128, 4, 256], BF16) for i in range(3)])
        vts = Rot([T(f"vt{i}", [128, 2, 512], BF16) for i in range(3)])
        scs = Rot([T(f"sc{i}", [128, 4, 256]) for i in range(2)])
        Ps = Rot([T(f"P{i}", [128, 4, 256], BF16) for i in range(2)])
        PTs = Rot([T(f"PT{i}", [128, 8, 128], BF16) for i in range(2)])
        mss = Rot([T(f"ms{i}", [128, 2, 4]) for i in range(2)])
        stg = Rot([T(f"stg{i}", [128, 4, 130]) for i in range(2)])
        psr = Rot(ps)
        p.dma("pool", idb[:], c.ident, writes=[idb])
        p.dma("sp", flag[:], c.flag, writes=[flag])

        for g in range(3):
            d = DIL[g]
            span = 128 * d
            nprevb = NPREV // span
            ncls = 4096 // span
            nown = NTOK // span
            p.dma("sp", bmid[:].rearrange("p a b -> p (a b)"), c.attn_bias[g], writes=[bmid])
            p.op("dve", lambda: nc.vector.tensor_copy(bfirst[:], bmid[:]), reads=[bmid], writes=[bfirst])
            p.op("dve", lambda: nc.vector.tensor_scalar(bfirst[:, :, 0:128], bfirst[:, :, 0:128], flag[:, 1:2], None, ALU.add),
                 reads=[bfirst, flag], writes=[bfirst])
            og = c.O_s[g].rearrange("(j r) c -> r j c", r=d)
            for r in range(d):
                for n in range(nown):
                    qt = qts.next(); kt = kts.next(); vt = vts.next(); sc = scs.next(); P = Ps.next(); PT = PTs.next()
                    ms = mss.next(); sg = stg.next()
                    pos0 = r * (NTOK // d) + n * 128
                    kpos = r * (4096 // d) + (nprevb + n - 1) * 128
                    p.dma("sp", qt[:], c.QT_s[4 * g:4 * g + 4, :, pos0:pos0 + 128].rearrange("h e q -> e h q"), writes=[qt])
                    p.dma("sp", kt[:], c.KT_s[4 * g:4 * g + 4, :, kpos:kpos + 256].rearrange("h e k -> e h k"), writes=[kt])
                    vb = r * ncls + nprevb + n - 1
                    p.dma("sp", vt[:], c.V_s[g, vb:vb + 2].rearrange("b k e -> k b e"), writes=[vt])
                    bias = bfirst if n == 0 else bmid
                    banks = [psr.next(), psr.next()]
                    for hh in range(4):
                        bk = banks[hh // 2]
                        p.op("pe", lambda: nc.tensor.matmul(bk[:, (hh % 2) * 256:(hh % 2 + 1) * 256], qt[:, hh, :], kt[:, hh, :], start=True, stop=True),
                             reads=[qt, kt], writes=[bk], mark=(hh % 2 == 1))
                    for i in range(2):
                        p.op("dve", lambda: nc.vector.tensor_tensor(sc[:, 2 * i:2 * i + 2, :], banks[i][:].rearrange("p (a b) -> p a b", a=2),
                                                                    bias[:, 2 * i:2 * i + 2, :], ALU.add), reads=[banks[i], bias], writes=[sc])
                    p.op("dve", lambda: nc.vector.reduce_max(ms[:, 0, :], sc[:], axis=AX.X), reads=[sc], writes=[ms])
                    p.op("dve", lambda: nc.vector.tensor_tensor(sc[:], sc[:], ms[:, 0, :].unsqueeze(2).broadcast_to([128, 4, 256]), ALU.subtract),
                         reads=[sc, ms], writes=[sc])
                    p.op("act", lambda: nc.scalar.activation(out=P[:], in_=sc[:], func=AF.Exp), reads=[sc], writes=[P])
                    p.op("dve", lambda: nc.vector.reduce_sum(ms[:, 1, :], P[:], axis=AX.X), reads=[P], writes=[ms])
                    bt = psr.next()
                    pb = bt[:].bitcast(BF16)
                    for hh in range(4):
                        for kb in range(2):
                            i = hh * 2 + kb
                            p.op("pe", lambda: nc.tensor.transpose(pb[:, i * 128:(i + 1) * 128], P[:, hh, kb * 128:(kb + 1) * 128], idb[:]),
                                 reads=[P, idb], writes=[bt], mark=(i == 7))
                    p.op("act", lambda: nc.scalar.copy(PT[:], pb.rearrange("p (a b) -> p a b", a=8)), reads=[bt], writes=[PT])
                    bu = psr.next()
                    for hh in range(4):
                        for kb in range(2):
                            p.op("pe", lambda: nc.tensor.matmul(bu[:, hh * 128:(hh + 1) * 128], PT[:, hh * 2 + kb, :], vt[:, kb, hh * 128:(hh + 1) * 128],
                                                                start=(kb == 0), stop=(kb == 1)), reads=[PT, vt], writes=[bu], mark=(hh == 3 and kb == 1))
                    p.op("act", lambda: nc.scalar.copy(sg[:, :, 0:128], bu[:].rearrange("p (a b) -> p a b", a=4)), reads=[bu], writes=[sg])
                    p.op("dve", lambda: nc.vector.tensor_copy(sg[:, :, 128:130], ms[:].rearrange("p a b -> p b a")), reads=[ms], writes=[sg])
                    p.dma("sp", og[r, n * 128:(n + 1) * 128, :], sg[:].rearrange("p a b -> p (a b)"), reads=[sg])
        p.barrier()
        lds = Rot([T(f"ld{i}", [128, 3, 4, 130]) for i in range(2)])
        wk = Rot([T(f"wk{i}", [128, 8, 12]) for i in range(2)])
        ob = Rot([T(f"ob{i}", [128, 4, 128]) for i in range(2)])
        o16 = Rot([T(f"o16_{i}", [128, 4, 128], BF16) for i in range(2)])
        oT = Rot([T(f"oT{i}", [128, 4, 128], BF16) for i in range(2)])
        for tt in range(16):
            ld = lds.next(); w = wk.next(); o = ob.next(); ob16 = o16.next(); ot = oT.next()
            p.dma("sp", ld[:].rearrange("p g h c -> p g (h c)"), c.O_s[:, tt * 128:(tt + 1) * 128, :].rearrange("g t c -> t g c"), writes=[ld])
            mg = ld[:, :, :, 128]
            sgm = ld[:, :, :, 129]
            M = w[:, 0, 0:4]
            p.op("dve", lambda: nc.vector.tensor_tensor(M, mg[:, 0, :], mg[:, 1, :], ALU.max), reads=[ld], writes=[w])
            p.op("dve", lambda: nc.vector.tensor_tensor(M, M, mg[:, 2, :], ALU.max), reads=[ld, w], writes=[w])
            wg3 = w[:, 1, :].rearrange("p (g h) -> p g h", g=3)
            p.op("dve", lambda: nc.vector.tensor_tensor(wg3, mg, M.unsqueeze(1).broadcast_to([128, 3, 4]), ALU.subtract), reads=[ld, w], writes=[w])
            p.op("act", lambda: nc.scalar.activation(out=w[:, 2, :], in_=w[:, 1, :], func=AF.Exp), reads=[w], writes=[w])
            e3 = w[:, 2, :].rearrange("p (g h) -> p g h", g=3)
            ws3 = w[:, 3, :].rearrange("p (g h) -> p g h", g=3)
            p.op("dve", lambda: nc.vector.tensor_tensor(ws3, e3, sgm, ALU.mult), reads=[ld, w], writes=[w])
            den = w[:, 4, 0:4]
            p.op("dve", lambda: nc.vector.tensor_tensor(den, ws3[:, 0, :], ws3[:, 1, :], ALU.add), reads=[w], writes=[w])
            p.op("dve", lambda: nc.vector.tensor_tensor(den, den, ws3[:, 2, :], ALU.add), reads=[w], writes=[w])
            p.op("dve", lambda: nc.vector.reciprocal(w[:, 5, 0:4], den), reads=[w], writes=[w])
            wn3 = w[:, 6, :].rearrange("p (g h) -> p g h", g=3)
            p.op("dve", lambda: nc.vector.tensor_tensor(wn3, e3, w[:, 5, 0:4].unsqueeze(1).broadcast_to([128, 3, 4]), ALU.mult), reads=[w], writes=[w])
            bcw = lambda gi: wn3[:, gi, :].unsqueeze(2).broadcast_to([128, 4, 128])
            p.op("dve", lambda: nc.vector.tensor_tensor(o[:], ld[:, 0, :, 0:128], bcw(0), ALU.mult), reads=[ld, w], writes=[o])
            for gi in (1, 2):
                p.op("dve", lambda: nc.vector.tensor_tensor(ld[:, gi, :, 0:128], ld[:, gi, :, 0:128], bcw(gi), ALU.mult), reads=[ld, w], writes=[ld])
                dst = o[:] if gi == 1 else ob16[:]
                p.op("dve", lambda: nc.vector.tensor_tensor(dst, o[:], ld[:, gi, :, 0:128], ALU.add), reads=[ld, o], writes=[o, ob16])
            bt = psr.next()
            pb = bt[:].bitcast(BF16)
            for hh in range(4):
                p.op("pe", lambda: nc.tensor.transpose(pb[:, hh * 128:(hh + 1) * 128], ob16[:, hh, :], idb[:]), reads=[ob16, idb], writes=[bt], mark=(hh == 3))
            p.op("act", lambda: nc.scalar.copy(ot[:], pb[:, 0:512].rearrange("p (a b) -> p a b", a=4)), reads=[bt], writes=[ot])
            p.dma("sp", c.OT_s[:, :, tt * 128:(tt + 1) * 128].rearrange("h e t -> e h t"), ot[:], reads=[ot])
        p.barrier()


def host_common_c(inp, m):
    slopes = (2.0 ** (-8.0 * np.arange(1, AH + 1) / AH)).astype(np.float32)
    q = np.arange(128)[:, None]
    k = np.arange(256)[None, :]
    rel = (q + 128) - k
    valid = (rel >= 0) & (rel <= 128)
    ab = np.zeros((3, 128, 4, 256), np.float32)
    for g in range(3):
        for hh in range(4):
            bias = -slopes[4 * g + hh] * (rel * DIL[g]).astype(np.float32)
            ab[g, :, hh, :] = np.where(valid, bias, NEG)
    m["attn_bias"] = np.ascontiguousarray(ab.reshape(3, 128, 1024))
    return m


def phase_d(p, c, ps):
    nc = p.nc
    with ExitStack() as es:
        T = lambda name, shape, dt=F32: p.tile(es, name, shape, dt)
        ynT = T("ynT_d", [128, NDC, NTOK], BF16)
        oT = T("oT_d", [128, 4, NTOK], BF16)
        wss = Rot([T(f"wss{i}", [128, NDC, 128], BF16) for i in range(2)])
        was = Rot([T(f"was{i}", [128, 4, 128], BF16) for i in range(2)])
        g0s = Rot([T(f"g0_{i}", [128, 512]) for i in range(2)])
        g1s = Rot([T(f"g1_{i}", [128, 512]) for i in range(2)])
        t1s = Rot([T(f"t1_{i}", [128, 512]) for i in range(2)])
        mgs = Rot([T(f"mg{i}", [128, 512], BF16) for i in range(2)])
        psr = Rot(ps)
        for kc in range(NDC):
            p.dma("sp", ynT[:, kc, :], c.YN_s[kc], writes=[ynT])
        for kc in range(4):
            p.dma("sp", oT[:, kc, :], c.OT_s[kc], writes=[oT])
        for dmc in range(16):
            ws = wss.next(); wa = was.next()
            p.dma("pool", ws[:].rearrange("p a b -> p (a b)"), c.w_ssd_fm[dmc], writes=[ws])
            p.dma("pool", wa[:].rearrange("p a b -> p (a b)"), c.w_attn_fm[dmc], writes=[wa])
            for st in range(4):
                ts = slice(st * 512, (st + 1) * 512)
                g0 = g0s.next(); g1 = g1s.next(); t1 = t1s.next(); mg = mgs.next()
                p.dma("sp", g0[:], c.G_s[dmc * 128:(dmc + 1) * 128, ts], writes=[g0])
                p.dma("sp", g1[:], c.G_s[D + dmc * 128:D + (dmc + 1) * 128, ts], writes=[g1])
                b1 = psr.next()
                for kc in range(NDC):
                    p.op("pe", lambda: nc.tensor.matmul(b1[:], ws[:, kc, :], ynT[:, kc, ts], start=(kc == 0), stop=(kc == NDC - 1)),
                         reads=[ws, ynT], writes=[b1], mark=(kc == NDC - 1))
                b2 = psr.next()
                for kc in range(4):
                    p.op("pe", lambda: nc.tensor.matmul(b2[:], wa[:, kc, :], oT[:, kc, ts], start=(kc == 0), stop=(kc == 3)),
                         reads=[wa, oT], writes=[b2], mark=(kc == 3))
                p.op("dve", lambda: nc.vector.tensor_tensor(t1[:], g0[:], b1[:], ALU.mult), reads=[g0, b1], writes=[t1])
                p.op("dve", lambda: nc.vector.tensor_tensor(g1[:], g1[:], b2[:], ALU.mult), reads=[g1, b2], writes=[g1])
                p.op("dve", lambda: nc.vector.tensor_tensor(mg[:], t1[:], g1[:], ALU.add), reads=[t1, g1], writes=[mg])
                p.dma("sp", c.M_s[dmc][:, ts], mg[:], reads=[mg])
        p.barrier()
    with ExitStack() as es:
        T = lambda name, shape, dt=F32: p.tile(es, name, shape, dt)
        mT = T("mT_d", [128, NDC, NTOK], BF16)
        wos = Rot([T(f"wo{i}", [128, NDC, 512], BF16) for i in range(2)])
        xss = Rot([T(f"xs_d{i}", [128, 512]) for i in range(3)])
        psr = Rot(ps)
        for kc in range(NDC):
            p.dma("sp", mT[:, kc, :], c.M_s[kc], writes=[mT])
        for ct in range(4):
            wo = wos.next()
            p.dma("pool", wo[:].rearrange("p a b -> p (a b)"), c.w_out_tm[ct], writes=[wo])
            for tt in range(16):
                xs_ = xss.next()
                p.dma("sp", xs_[:], c.x_own[tt * 128:(tt + 1) * 128, ct * 512:(ct + 1) * 512], writes=[xs_])
                bk = psr.next()
                for kc in range(NDC):
                    p.op("pe", lambda: nc.tensor.matmul(bk[:], mT[:, kc, tt * 128:(tt + 1) * 128], wo[:, kc, :], start=(kc == 0), stop=(kc == NDC - 1)),
                         reads=[mT, wo], writes=[bk], mark=(kc == NDC - 1))
                p.op("dve", lambda: nc.vector.tensor_tensor(xs_[:], xs_[:], bk[:], ALU.add), reads=[xs_, bk], writes=[xs_])
                p.dma("sp", c.X2_s[tt * 128:(tt + 1) * 128, ct * 512:(ct + 1) * 512], xs_[:], reads=[xs_])
        p.barrier()


def phase_e0(p, c, ps):
    nc = p.nc
    with ExitStack() as es:
        T = lambda name, shape, dt=F32: p.tile(es, name, shape, dt)
        idf = T("idf_e", [128, 128])
        fw = T("fw_e", [128, D])
        wr = T("wr_e", [128, NDC, 36])
        br = T("br_e", [128, 36])
        x2s = Rot([T(f"x2_{i}", [128, D]) for i in range(2)])
        hns = Rot([T(f"hn_{i}", [128, D]) for i in range(2)])
        junk = T("junk_e", [128, D], BF16)
        st4 = Rot([T(f"st4e_{i}", [128, 4]) for i in range(2)])
        h32 = Rot([T(f"h32_{i}", [128, NDC, 128]) for i in range(2)])
        h16 = Rot([T(f"h16_{i}", [128, NDC, 128], BF16) for i in range(2)])
        rt = Rot([T(f"rt{i}", [128, 8, 36]) for i in range(2)])
        sc = Rot([T(f"rs{i}", [128, 16]) for i in range(2)])
        gt = Rot([T(f"gt{i}", [128, NE]) for i in range(2)])
        psr = Rot(ps)
        p.dma("sp", idf[:], c.ident, writes=[idf])
        p.dma("sp", fw[:], c.ffn_norm_w.partition_broadcast(128), writes=[fw])
        p.dma("sp", wr[:].rearrange("p a b -> p (a b)"), c.w_router, writes=[wr])
        p.dma("sp", br[:], c.b_router.partition_broadcast(128), writes=[br])
        for tt in range(16):
            x2 = x2s.next(); hn = hns.next(); s4 = st4.next(); a32 = h32.next(); a16 = h16.next()
            R = rt.next(); S = sc.next(); G = gt.next()
            p.dma("sp", x2[:], c.X2_s[tt * 128:(tt + 1) * 128, :], writes=[x2])
            p.op("act", lambda: nc.scalar.activation(out=junk[:], in_=x2[:], func=AF.Square, accum_out=s4[:, 0:1]), reads=[x2], writes=[junk, s4])
            p.op("dve", lambda: nc.vector.tensor_scalar(s4[:, 1:2], s4[:, 0:1], 1.0 / D, EPS, ALU.mult, ALU.add), reads=[s4], writes=[s4])
            p.op("act", lambda: nc.scalar.activation(out=s4[:, 2:3], in_=s4[:, 1:2], func=AF.Ln), reads=[s4], writes=[s4])
            p.op("act", lambda: nc.scalar.activation(out=s4[:, 3:4], in_=s4[:, 2:3], func=AF.Exp, scale=-0.5), reads=[s4], writes=[s4])
            p.op("dve", lambda: nc.vector.scalar_tensor_tensor(hn[:], x2[:], s4[:, 3:4], fw[:], ALU.mult, ALU.mult), reads=[x2, s4, fw], writes=[hn])
            for q4 in range(4):
                bk = psr.next()
                for k in range(4):
                    dc = q4 * 4 + k
                    p.op("pe", lambda: nc.tensor.transpose(bk[:, k * 128:(k + 1) * 128], hn[:, dc * 128:(dc + 1) * 128], idf[:]),
                         reads=[hn, idf], writes=[bk], mark=(k == 3))
                p.op("act", lambda: nc.scalar.copy(a32[:, q4 * 4:q4 * 4 + 4, :], bk[:].rearrange("p (a b) -> p a b", a=4)), reads=[bk], writes=[a32])
                p.op("dve", lambda: nc.vector.tensor_copy(a16[:, q4 * 4:q4 * 4 + 4, :], bk[:].rearrange("p (a b) -> p a b", a=4)), reads=[bk], writes=[a16])
            p.dma("sp", c.HN_s[:, :, tt * 128:(tt + 1) * 128].rearrange("a p t -> p a t"), a16[:], reads=[a16])
            bk = psr.next()
            for kc in range(NDC):
                p.op("pe", lambda: nc.tensor.matmul(bk[:, 0:36], a32[:, kc, :], wr[:, kc, :], start=(kc == 0), stop=(kc == NDC - 1)),
                     reads=[a32, wr], writes=[bk], mark=(kc == NDC - 1))
            L = R[:, 0, :]
            dv = lambda fn, reads, writes: p.op("dve", fn, reads=reads, writes=writes)
            dv(lambda: nc.vector.tensor_tensor(L, bk[:, 0:36], br[:], ALU.add), [bk, br], [R])
            gl = R[:, 0, 0:4]
            el = R[:, 0, 4:36]
            dv(lambda: nc.vector.reduce_max(S[:, 0:1], gl, axis=AX.X), [R], [S])
            dv(lambda: nc.vector.tensor_scalar(R[:, 1, 0:4], gl, S[:, 0:1], None, ALU.subtract), [R, S], [R])
            p.op("act", lambda: nc.scalar.activation(out=R[:, 1, 4:8], in_=R[:, 1, 0:4], func=AF.Exp, accum_out=S[:, 1:2]), reads=[R], writes=[R, S])
            dv(lambda: nc.vector.reciprocal(S[:, 2:3], S[:, 1:2]), [S], [S])
            dv(lambda: nc.vector.tensor_scalar(R[:, 1, 8:12], gl, S[:, 0:1], None, ALU.is_equal), [R, S], [R])
            dv(lambda: nc.vector.tensor_scalar(R[:, 1, 12:16], R[:, 1, 8:12], -NEG, NEG, ALU.mult, ALU.add), [R], [R])
            elm = R[:, 2, 0:32]
            dv(lambda: nc.vector.tensor_tensor(elm.rearrange("p (g e) -> p g e", g=4), el.rearrange("p (g e) -> p g e", g=4),
                                               R[:, 1, 12:16].unsqueeze(2).broadcast_to([128, 4, 8]), ALU.add), [R], [R])
            dv(lambda: nc.vector.reduce_max(S[:, 3:4], elm, axis=AX.X), [R], [S])
            oh1 = R[:, 3, 0:32]
            dv(lambda: nc.vector.tensor_scalar(oh1, elm, S[:, 3:4], None, ALU.is_equal), [R, S], [R])
            elm2 = R[:, 4, 0:32]
            dv(lambda: nc.vector.scalar_tensor_tensor(elm2, oh1, NEG, elm, ALU.mult, ALU.add), [R], [R])
            dv(lambda: nc.vector.reduce_max(S[:, 4:5], elm2, axis=AX.X), [R], [S])
            oh2 = R[:, 5, 0:32]
            dv(lambda: nc.vector.tensor_scalar(oh2, elm2, S[:, 4:5], None, ALU.is_equal), [R, S], [R])
            dv(lambda: nc.vector.tensor_tensor(S[:, 5:6], S[:, 4:5], S[:, 3:4], ALU.subtract), [S], [S])
            p.op("act", lambda: nc.scalar.activation(out=S[:, 6:7], in_=S[:, 5:6], func=AF.Exp), reads=[S], writes=[S])
            dv(lambda: nc.vector.tensor_scalar(S[:, 7:8], S[:, 6:7], 1.0, None, ALU.add), [S], [S])
            dv(lambda: nc.vector.reciprocal(S[:, 8:9], S[:, 7:8]), [S], [S])
            dv(lambda: nc.vector.tensor_tensor(S[:, 9:10], S[:, 8:9], S[:, 2:3], ALU.mult), [S], [S])
            dv(lambda: nc.vector.tensor_tensor(S[:, 10:11], S[:, 9:10], S[:, 6:7], ALU.mult), [S], [S])
            dv(lambda: nc.vector.tensor_scalar(G[:], oh1, S[:, 9:10], None, ALU.mult), [R, S], [G])
            dv(lambda: nc.vector.scalar_tensor_tensor(G[:], oh2, S[:, 10:11], G[:], ALU.mult, ALU.add), [R, S, G], [G])
            p.dma("sp", c.GATE_s[tt * 128:(tt + 1) * 128, :], G[:], reads=[G])
        p.barrier()


def phase_e(p, c, ps):
    nc = p.nc
    NP = 1024
    with ExitStack() as es:
        T = lambda name, shape, dt=F32: p.tile(es, name, shape, dt)
        fnw = T("fnw", [128, D])
        gates = T("gates", [128, 16, NE])
        hnT = T("hnT_e", [128, NDC, NP], BF16)
        yacc = T("yacc", [128, NP // 128, D])
        wgs = Rot([T(f"wg{i}", [128, NDC, 256], BF16) for i in range(2)])
        wus = Rot([T(f"wu{i}", [128, NDC, 256], BF16) for i in range(2)])
        wds = Rot([T(f"wd{i}", [128, 2, D], BF16) for i in range(2)])
        sils = Rot([T(f"sil{i}", [128, 512]) for i in range(2)])
        hms = Rot([T(f"hm{i}", [128, 2, NP], BF16) for i in range(2)])
        junk = T("junk_m", [128, D], BF16)
        st4 = Rot([T(f"st4m_{i}", [128, 4]) for i in range(2)])
        psr = Rot(ps)
        p.dma("sp", fnw[:], c.final_norm_w.partition_broadcast(128), writes=[fnw])
        p.dma("sp", gates[:], c.GATE_s.rearrange("(t p) e -> p t e", p=128), writes=[gates])
        for ps_i in range(NTOK // NP):
            t0 = ps_i * NP
            for kc in range(NDC):
                p.dma("sp", hnT[:, kc, :], c.HN_s[kc][:, t0:t0 + NP], writes=[hnT])
            for t in range(NP // 128):
                p.dma("sp", yacc[:, t, :], c.X2_s[t0 + t * 128:t0 + (t + 1) * 128, :], writes=[yacc])
            for e in range(NE):
                for fc in range(4):
                    wg = wgs.next(); wu = wus.next(); wd = wds.next(); hm = hms.next()
                    p.dma("pool", wg[:].rearrange("p a b -> p (a b)"), c.wg_t[e, fc], writes=[wg])
                    p.dma("pool", wu[:].rearrange("p a b -> p (a b)"), c.wu_t[e, fc], writes=[wu])
                    p.dma("pool", wd[:].rearrange("p a b -> p (a b)"), c.wd_t[e, fc], writes=[wd])
                    for j in range(2):
                        for ts in range(NP // 512):
                            tsl = slice(ts * 512, (ts + 1) * 512)
                            bg = psr.next(); bu = psr.next(); sil = sils.next()
                            for kc in range(NDC):
                                p.op("pe", lambda: nc.tensor.matmul(bg[:], wg[:, kc, j * 128:(j + 1) * 128], hnT[:, kc, tsl], start=(kc == 0), stop=(kc == NDC - 1)),
                                     reads=[wg, hnT], writes=[bg], mark=(kc == NDC - 1))
                            for kc in range(NDC):
                                p.op("pe", lambda: nc.tensor.matmul(bu[:], wu[:, kc, j * 128:(j + 1) * 128], hnT[:, kc, tsl], start=(kc == 0), stop=(kc == NDC - 1)),
                                     reads=[wu, hnT], writes=[bu], mark=(kc == NDC - 1))
                            p.op("act", lambda: nc.scalar.activation(out=sil[:], in_=bg[:], func=AF.Silu), reads=[bg], writes=[sil])
                            p.op("dve", lambda: nc.vector.tensor_tensor(hm[:, j, tsl], sil[:], bu[:], ALU.mult), reads=[sil, bu], writes=[hm])
                    for t in range(NP // 128):
                        gcol = gates[:, ps_i * (NP // 128) + t, e:e + 1]
                        for dtile in range(4):
                            dsl = slice(dtile * 512, (dtile + 1) * 512)
                            bk = psr.next()
                            for j in range(2):
                                p.op("pe", lambda: nc.tensor.matmul(bk[:], hm[:, j, t * 128:(t + 1) * 128], wd[:, j, dsl], start=(j == 0), stop=(j == 1)),
                                     reads=[hm, wd], writes=[bk], mark=(j == 1))
                            p.op("dve", lambda: nc.vector.scalar_tensor_tensor(yacc[:, t, dsl], bk[:], gcol, yacc[:, t, dsl], ALU.mult, ALU.add),
                                 reads=[bk, gates, yacc], writes=[yacc])
            for t in range(NP // 128):
                s4 = st4.next()
                p.op("act", lambda: nc.scalar.activation(out=junk[:], in_=yacc[:, t, :], func=AF.Square, accum_out=s4[:, 0:1]), reads=[yacc], writes=[junk, s4])
                p.op("dve", lambda: nc.vector.tensor_scalar(s4[:, 1:2], s4[:, 0:1], 1.0 / D, EPS, ALU.mult, ALU.add), reads=[s4], writes=[s4])
                p.op("act", lambda: nc.scalar.activation(out=s4[:, 2:3], in_=s4[:, 1:2], func=AF.Ln), reads=[s4], writes=[s4])
                p.op("act", lambda: nc.scalar.activation(out=s4[:, 3:4], in_=s4[:, 2:3], func=AF.Exp, scale=-0.5), reads=[s4], writes=[s4])
                p.op("dve", lambda: nc.vector.scalar_tensor_tensor(yacc[:, t, :], yacc[:, t, :], s4[:, 3:4], fnw[:], ALU.mult, ALU.mult),
                     reads=[yacc, s4, fnw], writes=[yacc])
                p.dma("sp", c.out[t0 + t * 128:t0 + (t + 1) * 128, :], yacc[:, t, :], reads=[yacc])
        p.barrier()


def host_common_de(inp, m):
    m["w_ssd_fm"] = tile_w(np.asarray(inp["w_ssd_out"])[0], 128)
    m["w_attn_fm"] = tile_w(np.asarray(inp["w_attn_out"])[0], 128)
    m["w_out_tm"] = tile_w(np.asarray(inp["w_out"])[0], 512)
    m["ffn_norm_w"] = np.ascontiguousarray(np.asarray(inp["ffn_norm_w"]).reshape(1, D))
    wr = np.concatenate([np.asarray(inp["w_group_router"])[0], np.asarray(inp["w_expert_router"])[0]], axis=1)
    m["w_router"] = tile_w(wr, 36)[0]
    m["b_router"] = np.ascontiguousarray(np.concatenate([np.asarray(inp["b_group_router"])[0], np.asarray(inp["b_expert_router"])[0]]).reshape(1, 36))
    m["final_norm_w"] = np.ascontiguousarray(np.asarray(inp["final_norm_w"]).reshape(1, D))
    return m


def build_program(debug=False, phases="abcdeE"):
    nc = bass.Bass("TRN2", target_bir_lowering=False)
    c = declare_io(nc, debug=debug, phases=phases)
    with ExitStack() as es:
        p = Prog(nc, es)
        ps = [Tile(f"ps{i}", es.enter_context(nc.psum_tensor(f"psum{i}", [128, 512], F32))) for i in range(8)]
        for b in ps:
            b.excl = True
        if "a" in phases:
            phase_a(p, c, ps)
        if "b" in phases:
            phase_b(p, c, ps)
        if "c" in phases:
            phase_c(p, c, ps)
        if "d" in phases:
            phase_d(p, c, ps)
        rt_ = route_tiles(p, es)
        if "e" in phases:
            phase_e0s(p, c, ps, rt_)
            phase_e1(p, c, ps, rt_)
        if "E" in phases:
            phase_es(p, c, ps, rt_)
            phase_ec(p, c, ps, rt_)
        p.barrier()
    return nc, p


def host_maps(inp):
    m = host_common(inp)
    host_common_b(inp, m)
    host_common_c(inp, m)
    host_common_de(inp, m)
    host_common_s(inp, m)
    x = np.asarray(inp["x"])
    maps = []
    zeros = np.zeros((NPREV, D), np.float32)
    for core in range(8):
        b, half = core // 2, core % 2
        mm = dict(m)
        mm["x_own"] = np.ascontiguousarray(x[b, half * NTOK:(half + 1) * NTOK])
        mm["x_prev"] = np.ascontiguousarray(x[b, 0:NPREV]) if half == 1 else zeros
        fl = np.array([[1.0, 0.0]], np.float32) if half == 1 else np.array([[0.0, NEG]], np.float32)
        mm["flag"] = np.ascontiguousarray(np.tile(fl, (128, 1)))
        maps.append(mm)
    return maps


_PROG = {}


def kernel(**inputs):
    if "nc" not in _PROG:
        _PROG["nc"], _ = build_program()
    nc = _PROG["nc"]
    maps = host_maps(inputs)
    res = run_bass_kernel_spmd(nc, maps, core_ids=list(range(8)))
    x = np.asarray(inputs["x"])
    out = np.empty(x.shape, np.float32)
    for core in range(8):
        b, half = core // 2, core % 2
        out[b, half * NTOK:(half + 1) * NTOK] = np.asarray(res.results[core]["out"], np.float32)
    return out


def route_tiles(p, es):
    r = Ctx()
    T = lambda name, shape, dt=F32: p.tile(es, name, shape, dt)
    r.OH1 = T("OH1", [128, 16, NE])
    r.OH2 = T("OH2", [128, 16, NE])
    r.PG = T("PG", [128, 16, 2])
    r.POSI = T("POSI", [128, 2, 16], I32)
    r.IDXI = T("IDXI", [128, NSLOT], I32)
    return r


def phase_e0s(p, c, ps, rt_):
    nc = p.nc
    with ExitStack() as es:
        T = lambda name, shape, dt=F32: p.tile(es, name, shape, dt)
        idf = T("idf_e", [128, 128])
        fw = T("fw_e", [128, D])
        wr = T("wr_e", [128, NDC, 36])
        br = T("br_e", [128, 36])
        zt = T("zt_e", [128, D], BF16)
        x2s = Rot([T(f"x2_{i}", [128, D]) for i in range(2)])
        hns = Rot([T(f"hn_{i}", [128, D]) for i in range(2)])
        hbs = Rot([T(f"hb_{i}", [128, D], BF16) for i in range(2)])
        junk = T("junk_e", [128, D], BF16)
        st4 = Rot([T(f"st4e_{i}", [128, 4]) for i in range(2)])
        h32 = Rot([T(f"h32_{i}", [128, NDC, 128]) for i in range(2)])
        rt = Rot([T(f"rt{i}", [128, 8, 36]) for i in range(2)])
        sc = Rot([T(f"rs{i}", [128, 16]) for i in range(2)])
        psr = Rot(ps)
        p.dma("sp", idf[:], c.ident, writes=[idf])
        p.dma("sp", fw[:], c.ffn_norm_w.partition_broadcast(128), writes=[fw])
        p.dma("sp", wr[:].rearrange("p a b -> p (a b)"), c.w_router, writes=[wr])
        p.dma("sp", br[:], c.b_router.partition_broadcast(128), writes=[br])
        p.op("dve", lambda: nc.vector.memset(zt[:], 0.0), writes=[zt])
        for i in range(NSLOT):
            p.dma("sp", c.XS_s[i * 128:(i + 1) * 128, :], zt[:], reads=[zt])
        for tt in range(16):
            x2 = x2s.next(); hn = hns.next(); hb = hbs.next(); s4 = st4.next(); a32 = h32.next()
            R = rt.next(); S = sc.next()
            p.dma("sp", x2[:], c.X2_s[tt * 128:(tt + 1) * 128, :], writes=[x2])
            p.op("act", lambda: nc.scalar.activation(out=junk[:], in_=x2[:], func=AF.Square, accum_out=s4[:, 0:1]), reads=[x2], writes=[junk, s4])
            p.op("dve", lambda: nc.vector.tensor_scalar(s4[:, 1:2], s4[:, 0:1], 1.0 / D, EPS, ALU.mult, ALU.add), reads=[s4], writes=[s4])
            p.op("act", lambda: nc.scalar.activation(out=s4[:, 2:3], in_=s4[:, 1:2], func=AF.Ln), reads=[s4], writes=[s4])
            p.op("act", lambda: nc.scalar.activation(out=s4[:, 3:4], in_=s4[:, 2:3], func=AF.Exp, scale=-0.5), reads=[s4], writes=[s4])
            p.op("dve", lambda: nc.vector.scalar_tensor_tensor(hn[:], x2[:], s4[:, 3:4], fw[:], ALU.mult, ALU.mult), reads=[x2, s4, fw], writes=[hn])
            p.op("act", lambda: nc.scalar.copy(hb[:], hn[:]), reads=[hn], writes=[hb])
            p.dma("sp", c.HNTM_s[tt * 128:(tt + 1) * 128, :], hb[:], reads=[hb])
            for q4 in range(4):
                bk = psr.next()
                for k in range(4):
                    dc = q4 * 4 + k
                    p.op("pe", lambda: nc.tensor.transpose(bk[:, k * 128:(k + 1) * 128], hn[:, dc * 128:(dc + 1) * 128], idf[:]),
                         reads=[hn, idf], writes=[bk], mark=(k == 3))
                if q4 % 2 == 0:
                    p.op("act", lambda: nc.scalar.copy(a32[:, q4 * 4:q4 * 4 + 4, :], bk[:].rearrange("p (a b) -> p a b", a=4)), reads=[bk], writes=[a32])
                else:
                    p.op("dve", lambda: nc.vector.tensor_copy(a32[:, q4 * 4:q4 * 4 + 4, :], bk[:].rearrange("p (a b) -> p a b", a=4)), reads=[bk], writes=[a32])
            bk = psr.next()
            for kc in range(NDC):
                p.op("pe", lambda: nc.tensor.matmul(bk[:, 0:36], a32[:, kc, :], wr[:, kc, :], start=(kc == 0), stop=(kc == NDC - 1)),
                     reads=[a32, wr], writes=[bk], mark=(kc == NDC - 1))
            L = R[:, 0, :]
            dv = lambda fn, reads, writes: p.op("dve", fn, reads=reads, writes=writes)
            dv(lambda: nc.vector.tensor_tensor(L, bk[:, 0:36], br[:], ALU.add), [bk, br], [R])
            gl = R[:, 0, 0:4]
            el = R[:, 0, 4:36]
            dv(lambda: nc.vector.reduce_max(S[:, 0:1], gl, axis=AX.X), [R], [S])
            dv(lambda: nc.vector.tensor_scalar(R[:, 1, 0:4], gl, S[:, 0:1], None, ALU.subtract), [R, S], [R])
            p.op("act", lambda: nc.scalar.activation(out=R[:, 1, 4:8], in_=R[:, 1, 0:4], func=AF.Exp, accum_out=S[:, 1:2]), reads=[R], writes=[R, S])
            dv(lambda: nc.vector.reciprocal(S[:, 2:3], S[:, 1:2]), [S], [S])
            dv(lambda: nc.vector.tensor_scalar(R[:, 1, 8:12], gl, S[:, 0:1], None, ALU.is_equal), [R, S], [R])
            dv(lambda: nc.vector.tensor_scalar(R[:, 1, 12:16], R[:, 1, 8:12], -NEG, NEG, ALU.mult, ALU.add), [R], [R])
            elm = R[:, 2, 0:32]
            dv(lambda: nc.vector.tensor_tensor(elm.rearrange("p (g e) -> p g e", g=4), el.rearrange("p (g e) -> p g e", g=4),
                                               R[:, 1, 12:16].unsqueeze(2).broadcast_to([128, 4, 8]), ALU.add), [R], [R])
            dv(lambda: nc.vector.reduce_max(S[:, 3:4], elm, axis=AX.X), [R], [S])
            oh1 = rt_.OH1[:, tt, :]
            dv(lambda: nc.vector.tensor_scalar(oh1, elm, S[:, 3:4], None, ALU.is_equal), [R, S], [rt_.OH1])
            elm2 = R[:, 4, 0:32]
            dv(lambda: nc.vector.scalar_tensor_tensor(elm2, oh1, NEG, elm, ALU.mult, ALU.add), [R, rt_.OH1], [R])
            dv(lambda: nc.vector.reduce_max(S[:, 4:5], elm2, axis=AX.X), [R], [S])
            oh2 = rt_.OH2[:, tt, :]
            dv(lambda: nc.vector.tensor_scalar(oh2, elm2, S[:, 4:5], None, ALU.is_equal), [R, S], [rt_.OH2])
            dv(lambda: nc.vector.tensor_tensor(S[:, 5:6], S[:, 4:5], S[:, 3:4], ALU.subtract), [S], [S])
            p.op("act", lambda: nc.scalar.activation(out=S[:, 6:7], in_=S[:, 5:6], func=AF.Exp), reads=[S], writes=[S])
            dv(lambda: nc.vector.tensor_scalar(S[:, 7:8], S[:, 6:7], 1.0, None, ALU.add), [S], [S])
            dv(lambda: nc.vector.reciprocal(S[:, 8:9], S[:, 7:8]), [S], [S])
            dv(lambda: nc.vector.tensor_tensor(rt_.PG[:, tt, 0:1], S[:, 8:9], S[:, 2:3], ALU.mult), [S], [rt_.PG])
            dv(lambda: nc.vector.tensor_tensor(rt_.PG[:, tt, 1:2], rt_.PG[:, tt, 0:1], S[:, 6:7], ALU.mult), [S, rt_.PG], [rt_.PG])
        p.barrier()


def phase_e1(p, c, ps, rt_):
    nc = p.nc
    with ExitStack() as es:
        T = lambda name, shape, dt=F32: p.tile(es, name, shape, dt)
        cst = T("cstE", [128, 328])
        SUTR, ONES, THR, C8 = cst[:, 0:128], cst[:, 128:256], cst[:, 256:320], cst[:, 320:328]
        OHS = T("OHS", [128, 16, NE])
        CUM = T("CUM", [128, 17, NE])
        RK = T("RK", [128, 16, NE])
        W = T("W_e1", [128, 12, NE])
        CMP = T("CMP", [128, NSLOT, NE])
        ET = T("ET", [128, 2, NSLOT])
        IDXF = T("IDXF", [128, NSLOT])
        TT = T("TT_e1", [128, 16, NE])
        MM = T("MM_e1", [128, 16, NE])
        POSF = T("POSF", [128, 2, 16])
        hbs = Rot([T(f"hb1_{i}", [128, D], BF16) for i in range(3)])
        psr = Rot(ps)
        dv = lambda fn, reads, writes: p.op("dve", fn, reads=reads, writes=writes)
        p.dma("sp", cst[:], c.cstE, writes=[cst])
        dv(lambda: nc.vector.tensor_tensor(OHS[:], rt_.OH1[:], rt_.OH2[:], ALU.add), [rt_.OH1, rt_.OH2], [OHS])
        dv(lambda: nc.vector.memset(CUM[:, 0, :], 0.0), [], [CUM])
        for i in range(16):
            dv(lambda: nc.vector.tensor_tensor(CUM[:, i + 1, :], CUM[:, i, :], OHS[:, i, :], ALU.add), [CUM, OHS], [CUM])
        for i in range(16):
            bk = psr.next()
            p.op("pe", lambda: nc.tensor.matmul(bk[:, 0:NE], SUTR, OHS[:, i, :], start=True, stop=False), reads=[cst, OHS], writes=[bk], mark=False)
            p.op("pe", lambda: nc.tensor.matmul(bk[:, 0:NE], ONES, CUM[:, i, :], start=False, stop=True), reads=[cst, CUM], writes=[bk])
            p.op("act", lambda: nc.scalar.copy(RK[:, i, :], bk[:, 0:NE]), reads=[bk], writes=[RK])
        bk = psr.next()
        p.op("pe", lambda: nc.tensor.matmul(bk[:, 0:NE], ONES, CUM[:, 16, :], start=True, stop=True), reads=[cst, CUM], writes=[bk])
        cnt, r_, pf, pad, off = W[:, 0, :], W[:, 1, :], W[:, 2, :], W[:, 3, :], W[:, 6, :]
        p.op("act", lambda: nc.scalar.copy(cnt, bk[:, 0:NE]), reads=[bk], writes=[W])
        C2 = CMP[:, 0:NE, 0:16]
        dv(lambda: nc.vector.tensor_tensor(C2, cnt.unsqueeze(2).broadcast_to([128, NE, 16]), THR[:, 0:16].unsqueeze(1).broadcast_to([128, NE, 16]), ALU.is_gt),
           [W, cst], [CMP])
        dv(lambda: nc.vector.reduce_sum(pf, C2, axis=AX.X), [CMP], [W])
        dv(lambda: nc.vector.tensor_scalar(pad, pf, 128.0, None, ALU.mult), [W], [W])
        a, b = 4, 5
        dv(lambda: nc.vector.tensor_copy(W[:, a, :], pad), [W], [W])
        for sft in (1, 2, 4, 8, 16):
            dv(lambda: nc.vector.tensor_copy(W[:, b, 0:sft], W[:, a, 0:sft]), [W], [W])
            dv(lambda: nc.vector.tensor_tensor(W[:, b, sft:NE], W[:, a, sft:NE], W[:, a, 0:NE - sft], ALU.add), [W], [W])
            a, b = b, a
        END = W[:, a, :]
        dv(lambda: nc.vector.tensor_tensor(off, END, pad, ALU.subtract), [W], [W])
        dv(lambda: nc.vector.tensor_tensor(CMP[:], END.unsqueeze(1).broadcast_to([128, NSLOT, NE]), THR.unsqueeze(2).broadcast_to([128, NSLOT, NE]), ALU.is_le),
           [W, cst], [CMP])
        dv(lambda: nc.vector.reduce_sum(ET[:, 0, :], CMP[:], axis=AX.X), [CMP], [ET])
        dv(lambda: nc.vector.tensor_scalar(ET[:, 1, :], ET[:, 0, :], float(NE - 1), 128.0, ALU.min, ALU.mult), [ET], [ET])
        dv(lambda: nc.vector.tensor_scalar(IDXF[:], ET[:, 1, :], C8[:, 0:1], None, ALU.add), [ET, cst], [IDXF])
        dv(lambda: nc.vector.tensor_copy(rt_.IDXI[:], IDXF[:]), [IDXF], [rt_.IDXI])
        dv(lambda: nc.vector.tensor_tensor(TT[:], RK[:], off.unsqueeze(1).broadcast_to([128, 16, NE]), ALU.add), [RK, W], [TT])
        for k, OH in enumerate((rt_.OH1, rt_.OH2)):
            dv(lambda: nc.vector.tensor_tensor(MM[:], TT[:], OH[:], ALU.mult), [TT, OH], [MM])
            dv(lambda: nc.vector.reduce_sum(POSF[:, k, :], MM[:], axis=AX.X), [MM], [POSF])
        dv(lambda: nc.vector.tensor_copy(rt_.POSI[:], POSF[:]), [POSF], [rt_.POSI])
        xsb = Buf("XS_s")
        for tt in range(16):
            hb = hbs.next()
            p.dma("sp", hb[:], c.HNTM_s[tt * 128:(tt + 1) * 128, :], writes=[hb])
            for k in range(2):
                p.idma(c.XS_s, bass.IndirectOffsetOnAxis(rt_.POSI[:, k, tt:tt + 1], 0), hb[:], None, reads=[hb, rt_.POSI], writes=[xsb])
        p.barrier()


def phase_es(p, c, ps, rt_):
    nc = p.nc
    with ExitStack() as es:
        T = lambda name, shape, dt=F32: p.tile(es, name, shape, dt)
        idb = T("idb_s", [128, 128], BF16)
        xss = Rot([T(f"xs_s{i}", [128, D], BF16) for i in range(2)])
        xTs = Rot([T(f"xT_s{i}", [128, NDC, 128], BF16) for i in range(2)])
        units = Rot([T(f"wU{i}", [128, 3, 4096], BF16) for i in range(5)])
        sils = Rot([T(f"silS{i}", [128, 256]) for i in range(2)])
        hms = Rot([T(f"hmS{i}", [128, 8, 128], BF16) for i in range(2)])
        yts = Rot([T(f"ytS{i}", [128, D]) for i in range(2)])
        ybanks = ps[0:4]
        psr = Rot(ps[4:8])
        p.dma("pool", idb[:], c.ident, writes=[idb])
        precast_step(p, c, 1000)
        for tk in c.precast_toks:
            p._wait("pool", tk)
        pending = []

        def down(i, fc, w, hm, yt):
            for dtile in range(4):
                dsl = slice(dtile * 512, (dtile + 1) * 512)
                for j in range(2):
                    p.op("pe", lambda: nc.tensor.matmul(ybanks[dtile][:], hm[:, fc * 2 + j, :], w[:, 2, :].rearrange("p (a b) -> p a b", a=2)[:, j, dsl],
                                                        start=(fc == 0 and j == 0), stop=(fc == 3 and j == 1)),
                         reads=[hm, w], writes=[ybanks[dtile]], mark=(dtile == 3 and j == 1))
            if fc == 3:
                for dtile in range(4):
                    dsl = slice(dtile * 512, (dtile + 1) * 512)
                    if dtile % 2 == 0:
                        p.op("act", lambda: nc.scalar.copy(yt[:, dsl], ybanks[dtile][:]), reads=[ybanks[dtile]], writes=[yt])
                    else:
                        p.op("dve", lambda: nc.vector.tensor_copy(yt[:, dsl], ybanks[dtile][:]), reads=[ybanks[dtile]], writes=[yt])
                p.dma("sp", c.YP_s[i * 128:(i + 1) * 128, :], yt[:], reads=[yt])

        for i in range(NSLOT):
            xs_ = xss.next(); xT = xTs.next(); hm = hms.next(); yt = yts.next()
            p.dma("sp", xs_[:], c.XS_s[i * 128:(i + 1) * 128, :], writes=[xs_])
            for half in range(2):
                bank = psr.next()
                pb = bank[:].bitcast(BF16)
                for k in range(8):
                    dc = half * 8 + k
                    p.op("pe", lambda: nc.tensor.transpose(pb[:, k * 128:(k + 1) * 128], xs_[:, dc * 128:(dc + 1) * 128], idb[:]),
                         reads=[xs_, idb], writes=[bank], mark=(k == 7))
                if half == 0:
                    p.op("act", lambda: nc.scalar.copy(xT[:, 0:8, :], pb.rearrange("p (a b) -> p a b", a=8)), reads=[bank], writes=[xT])
                else:
                    p.op("dve", lambda: nc.vector.tensor_copy(xT[:, 8:16, :], pb.rearrange("p (a b) -> p a b", a=8)), reads=[bank], writes=[xT])
            for fc in range(4):
                w = units.next(); sil = sils.next()
                off = bass.IndirectOffsetOnAxis(rt_.IDXI[:, i:i + 1], 0)
                p.idma(w[:].rearrange("p a b -> p (a b)"), None, c.WB_s[fc], off, reads=[rt_.IDXI], writes=[w])
                bg = psr.next(); bu = psr.next()
                for (bank, m_) in ((bg, 0), (bu, 1)):
                    for j in range(2):
                        for kc in range(NDC):
                            lhs = w[:, m_, :].rearrange("p (h a b) -> p h a b", h=2, a=8)[:, kc // 8, kc % 8, j * 128:(j + 1) * 128]
                            p.op("pe", lambda: nc.tensor.matmul(bank[:, j * 128:(j + 1) * 128], lhs, xT[:, kc, :], start=(kc == 0), stop=(kc == NDC - 1)),
                                 reads=[w, xT], writes=[bank], mark=(kc == NDC - 1 and j == 1))
                p.op("act", lambda: nc.scalar.activation(out=sil[:], in_=bg[:, 0:256], func=AF.Silu), reads=[bg], writes=[sil])
                p.op("dve", lambda: nc.vector.tensor_tensor(hm[:, fc * 2:fc * 2 + 2, :], sil[:].rearrange("p (a b) -> p a b", a=2),
                                                            bu[:, 0:256].rearrange("p (a b) -> p a b", a=2), ALU.mult), reads=[sil, bu], writes=[hm])
                for fn in pending:
                    fn()
                pending = [lambda i=i, fc=fc, w=w, hm=hm, yt=yt: down(i, fc, w, hm, yt)]
        for fn in pending:
            fn()
        p.barrier()


def phase_ec(p, c, ps, rt_):
    nc = p.nc
    with ExitStack() as es:
        T = lambda name, shape, dt=F32: p.tile(es, name, shape, dt)
        fnw = T("fnw", [128, D])
        y1s = Rot([T(f"y1_{i}", [128, D]) for i in range(2)])
        y2s = Rot([T(f"y2_{i}", [128, D]) for i in range(2)])
        x2s = Rot([T(f"x2c_{i}", [128, D]) for i in range(2)])
        junk = T("junk_c", [128, D], BF16)
        st4 = Rot([T(f"st4c_{i}", [128, 4]) for i in range(2)])
        p.dma("sp", fnw[:], c.final_norm_w.partition_broadcast(128), writes=[fnw])
        for tt in range(16):
            y1 = y1s.next(); y2 = y2s.next(); x2 = x2s.next(); s4 = st4.next()
            p.dma("sp", x2[:], c.X2_s[tt * 128:(tt + 1) * 128, :], writes=[x2])
            p.idma(y1[:], None, c.YP_s, bass.IndirectOffsetOnAxis(rt_.POSI[:, 0, tt:tt + 1], 0), reads=[rt_.POSI], writes=[y1])
            p.idma(y2[:], None, c.YP_s, bass.IndirectOffsetOnAxis(rt_.POSI[:, 1, tt:tt + 1], 0), reads=[rt_.POSI], writes=[y2])
            p.op("dve", lambda: nc.vector.scalar_tensor_tensor(x2[:], y1[:], rt_.PG[:, tt, 0:1], x2[:], ALU.mult, ALU.add), reads=[y1, rt_.PG, x2], writes=[x2])
            p.op("dve", lambda: nc.vector.scalar_tensor_tensor(x2[:], y2[:], rt_.PG[:, tt, 1:2], x2[:], ALU.mult, ALU.add), reads=[y2, rt_.PG, x2], writes=[x2])
            p.op("act", lambda: nc.scalar.activation(out=junk[:], in_=x2[:], func=AF.Square, accum_out=s4[:, 0:1]), reads=[x2], writes=[junk, s4])
            p.op("dve", lambda: nc.vector.tensor_scalar(s4[:, 1:2], s4[:, 0:1], 1.0 / D, EPS, ALU.mult, ALU.add), reads=[s4], writes=[s4])
            p.op("act", lambda: nc.scalar.activation(out=s4[:, 2:3], in_=s4[:, 1:2], func=AF.Ln), reads=[s4], writes=[s4])
            p.op("act", lambda: nc.scalar.activation(out=s4[:, 3:4], in_=s4[:, 2:3], func=AF.Exp, scale=-0.5), reads=[s4], writes=[s4])
            p.op("dve", lambda: nc.vector.scalar_tensor_tensor(y1[:], x2[:], s4[:, 3:4], fnw[:], ALU.mult, ALU.mult), reads=[x2, s4, fnw], writes=[y1])
            p.dma("sp", c.out[tt * 128:(tt + 1) * 128, :], y1[:], reads=[y1])
        p.barrier()


def host_common_s(inp, m):
    for k in ("wg_t", "wu_t", "wd_t"):
        m.pop(k, None)
    wg = np.asarray(inp["w_exp_gate"])[0]
    wu = np.asarray(inp["w_exp_up"])[0]
    wd = np.asarray(inp["w_exp_down"])[0]
    tg = lambda w: np.ascontiguousarray(w.reshape(NE, 2, 8, 128, 4, 256).transpose(4, 0, 3, 1, 2, 5)).reshape(4, NE * 128, 4096)
    m["wg_r"] = tg(wg)
    m["wu_r"] = tg(wu)
    m["wd_r"] = np.ascontiguousarray(wd.reshape(NE, 4, 2, 128, D).transpose(1, 0, 3, 2, 4)).reshape(4, NE * 128, 4096)
    l = np.arange(128)
    sutr = (l[:, None] < l[None, :]).astype(np.float32)
    ones = np.ones((128, 128), np.float32)
    thr = np.tile((128.0 * np.arange(NSLOT, dtype=np.float32))[None, :], (128, 1))
    c8 = (np.arange(8, dtype=np.float32)[None, :] * 128.0 + l[:, None].astype(np.float32))
    m["cstE"] = np.ascontiguousarray(np.concatenate([sutr, ones, thr, c8], axis=1).astype(np.float32))
    return m
```
